# Optimizing a Trainium2 kernel written in Bass

```python
import jax, jax.numpy as jnp
from jax import lax
import numpy as np

D_MODEL = 4096
BATCH = 4
SEQ = 2048
DEPTH = 1
DEC_BATCH = 4
DEC_SEQ = 4096
PAST_LEN = 128

HEAD_DIM = 128
ATTN_PATTERNS = ((128, 1), (512, 4), (2048, 16))
N_ATT_GROUPS = 3
HEADS_PER_GROUP = 8
N_ATT_HEADS = N_ATT_GROUPS * HEADS_PER_GROUP
ATT_WIDTH = N_ATT_HEADS * HEAD_DIM
ATT_OUT_WIDTH = HEADS_PER_GROUP * HEAD_DIM
QUERY_BLOCK = 64
N_FOURIER_GROUPS = 4
FOURIER_GROUP_WIDTH = D_MODEL // 8
FOURIER_WIDTH = N_FOURIER_GROUPS * FOURIER_GROUP_WIDTH
N_BRANCHES = 2
IN_WIDTH = 3 * ATT_WIDTH + FOURIER_WIDTH + N_BRANCHES * D_MODEL
N_EXPERT_GROUPS = 4
EXPERTS_PER_GROUP = 4
N_EXPERTS = N_EXPERT_GROUPS * EXPERTS_PER_GROUP
EXPERT_TOP_K = 2
D_FF_EXPERT = D_MODEL // 4
RMS_EPS = 1e-6
NEG_INF = -1e30

kernel_name = 'dilated_fourier_hmoe_encoder'


def rms_norm(x, g):
    xf = x.astype(jnp.float32)
    y = xf * lax.rsqrt(jnp.mean(xf * xf, axis=-1, keepdims=True) + RMS_EPS) * g.astype(jnp.float32)
    return y.astype(x.dtype)


def alibi_slopes():
    s = 2.0 ** (-8.0 * np.arange(1, N_ATT_HEADS + 1) / N_ATT_HEADS)
    return jnp.asarray(s, dtype=jnp.float32).reshape(N_ATT_GROUPS, HEADS_PER_GROUP)


def dilated_band_attention(q, k, v, window, dilation, slopes):
    B, S, H, Dh = q.shape
    half = window // (2 * dilation)
    L = S // dilation
    nb = -(-L // QUERY_BLOCK)
    Lp = nb * QUERY_BLOCK
    kb_len = QUERY_BLOCK + 2 * half

    def to_classes(t):
        return t.reshape(B, L, dilation, H, Dh).transpose(0, 2, 1, 3, 4)

    qc = jnp.pad(to_classes(q), ((0, 0), (0, 0), (0, Lp - L), (0, 0), (0, 0)))
    kpad = ((0, 0), (0, 0), (half, Lp - L + half), (0, 0), (0, 0))
    kc = jnp.pad(to_classes(k), kpad)
    vc = jnp.pad(to_classes(v), kpad)
    key_idx = jnp.arange(nb)[:, None] * QUERY_BLOCK + jnp.arange(kb_len)[None, :]
    kb = kc[:, :, key_idx]
    vb = vc[:, :, key_idx]
    qb = qc.reshape(B, dilation, nb, QUERY_BLOCK, H, Dh)
    s = jnp.einsum('bcnqhe,bcnkhe->bcnhqk', qb, kb).astype(jnp.float32) * (Dh ** -0.5)
    rel = jnp.arange(kb_len)[None, :] - half - jnp.arange(QUERY_BLOCK)[:, None]
    key_pos = key_idx - half
    valid = (jnp.abs(rel) <= half)[None] & ((key_pos >= 0) & (key_pos < L))[:, None, :]
    alibi = -slopes[:, None, None] * (jnp.abs(rel) * dilation).astype(jnp.float32)[None]
    s = jnp.where(valid[None, None, :, None], s + alibi[None, None, None], NEG_INF)
    m = jnp.max(s, axis=-1, keepdims=True)
    p = jnp.exp(s - m)
    den = jnp.sum(p, axis=-1, keepdims=True)
    o = jnp.einsum('bcnhqk,bcnkhe->bcnqhe', (p / den).astype(v.dtype), vb)
    lse = (m + jnp.log(den))[..., 0]
    o = o.reshape(B, dilation, Lp, H, Dh)[:, :, :L].transpose(0, 2, 1, 3, 4).reshape(B, S, H, Dh)
    lse = lse.transpose(0, 1, 2, 4, 3).reshape(B, dilation, Lp, H)[:, :, :L]
    lse = lse.transpose(0, 2, 1, 3).reshape(B, S, H)
    return o, lse


def mixer_branches(h, w_in, w_branch_attn, w_branch_fourier, b_gate, slopes):
    B, S, _ = h.shape
    proj = h @ w_in
    cuts = [ATT_WIDTH, 2 * ATT_WIDTH, 3 * ATT_WIDTH, 3 * ATT_WIDTH + FOURIER_WIDTH]
    q, k, v, f, gate_pre = jnp.split(proj, cuts, axis=-1)
    q = q.reshape(B, S, N_ATT_GROUPS, HEADS_PER_GROUP, HEAD_DIM)
    k = k.reshape(B, S, N_ATT_GROUPS, HEADS_PER_GROUP, HEAD_DIM)
    v = v.reshape(B, S, N_ATT_GROUPS, HEADS_PER_GROUP, HEAD_DIM)
    outs, lses = [], []
    for g, (window, dilation) in enumerate(ATTN_PATTERNS):
        o_g, lse_g = dilated_band_attention(q[:, :, g], k[:, :, g], v[:, :, g], window, dilation, slopes[g])
        outs.append(o_g)
        lses.append(lse_g)
    w_grp = jax.nn.softmax(jnp.stack(lses, axis=0), axis=0)
    o = jnp.einsum('gbsh,gbshe->bshe', w_grp, jnp.stack(outs, axis=0).astype(jnp.float32))
    attn = o.astype(h.dtype).reshape(B, S, ATT_OUT_WIDTH) @ w_branch_attn
    fg = f.astype(jnp.float32).reshape(B, S, N_FOURIER_GROUPS, FOURIER_GROUP_WIDTH)
    fr = jnp.fft.fft2(fg, axes=(1, 3), norm='ortho').real.astype(h.dtype).reshape(B, S, FOURIER_WIDTH)
    four = fr @ w_branch_fourier
    gates = jax.nn.sigmoid(gate_pre.astype(jnp.float32) + b_gate.astype(jnp.float32)).astype(h.dtype)
    return gates[..., :D_MODEL] * attn + gates[..., D_MODEL:] * four


def hierarchical_moe(h, w_router_group, b_router_group, w_router_expert, b_router_expert,
                     w_expert_gate, w_expert_up, w_expert_down):
    lg = (h @ w_router_group).astype(jnp.float32) + b_router_group.astype(jnp.float32)
    pg = jax.nn.softmax(lg, axis=-1)
    gsel = jnp.argmax(lg, axis=-1)
    p_group = jnp.take_along_axis(pg, gsel[..., None], axis=-1)
    le = jnp.einsum('bsd,gde->bsge', h, w_router_expert).astype(jnp.float32) + b_router_expert.astype(jnp.float32)
    le_sel = jnp.take_along_axis(le, gsel[..., None, None], axis=2)[:, :, 0]
    top_vals, top_idx = lax.top_k(le_sel, EXPERT_TOP_K)
    p_exp = jax.nn.softmax(top_vals, axis=-1)
    expert_id = gsel[..., None] * EXPERTS_PER_GROUP + top_idx
    combine = jnp.sum(jax.nn.one_hot(expert_id, N_EXPERTS, dtype=jnp.float32) * (p_group * p_exp)[..., None], axis=-2)
    combine = combine.astype(h.dtype)
    out = jnp.zeros_like(h)
    for e in range(N_EXPERTS):
        a = jax.nn.silu(h @ w_expert_gate[e]) * (h @ w_expert_up[e])
        out = out + combine[..., e:e + 1] * (a @ w_expert_down[e])
    return out


def encoder_trunk(x, attn_norm_g, w_in, w_branch_attn, w_branch_fourier, b_gate, w_out, ffn_norm_g,
                  w_router_group, b_router_group, w_router_expert, b_router_expert,
                  w_expert_gate, w_expert_up, w_expert_down, final_norm_g):
    slopes = alibi_slopes()
    for l in range(DEPTH):
        h = rms_norm(x, attn_norm_g[l])
        merged = mixer_branches(h, w_in[l], w_branch_attn[l], w_branch_fourier[l], b_gate[l], slopes)
        x = x + merged @ w_out[l]
        h2 = rms_norm(x, ffn_norm_g[l])
        x = x + hierarchical_moe(h2, w_router_group[l], b_router_group[l], w_router_expert[l], b_router_expert[l],
                                 w_expert_gate[l], w_expert_up[l], w_expert_down[l])
    return rms_norm(x, final_norm_g)


def setup_inputs(seed: int = 0) -> dict:
    key = jax.random.key(seed)
    ks = jax.random.split(key, 18)
    f32 = jnp.float32

    def nrm(k, shape, scale):
        return jax.random.normal(k, shape, f32) * scale

    return {
        'x_prompt': nrm(ks[0], (BATCH, SEQ, D_MODEL), 1.0),
        'x_sample': nrm(ks[1], (DEC_BATCH, DEC_SEQ, D_MODEL), 1.0),
        'attn_norm_g': 1.0 + nrm(ks[2], (DEPTH, D_MODEL), 0.01),
        'w_in': nrm(ks[3], (DEPTH, D_MODEL, IN_WIDTH), D_MODEL ** -0.5),
        'w_branch_attn': nrm(ks[4], (DEPTH, ATT_OUT_WIDTH, D_MODEL), ATT_OUT_WIDTH ** -0.5),
        'w_branch_fourier': nrm(ks[5], (DEPTH, FOURIER_WIDTH, D_MODEL), FOURIER_WIDTH ** -0.5),
        'b_gate': nrm(ks[6], (DEPTH, N_BRANCHES * D_MODEL), 0.01),
        'w_out': nrm(ks[7], (DEPTH, D_MODEL, D_MODEL), D_MODEL ** -0.5),
        'ffn_norm_g': 1.0 + nrm(ks[8], (DEPTH, D_MODEL), 0.01),
        'w_router_group': nrm(ks[9], (DEPTH, D_MODEL, N_EXPERT_GROUPS), D_MODEL ** -0.5),
        'b_router_group': nrm(ks[10], (DEPTH, N_EXPERT_GROUPS), 0.01),
        'w_router_expert': nrm(ks[11], (DEPTH, N_EXPERT_GROUPS, D_MODEL, EXPERTS_PER_GROUP), D_MODEL ** -0.5),
        'b_router_expert': nrm(ks[12], (DEPTH, N_EXPERT_GROUPS, EXPERTS_PER_GROUP), 0.01),
        'w_expert_gate': nrm(ks[13], (DEPTH, N_EXPERTS, D_MODEL, D_FF_EXPERT), D_MODEL ** -0.5),
        'w_expert_up': nrm(ks[14], (DEPTH, N_EXPERTS, D_MODEL, D_FF_EXPERT), D_MODEL ** -0.5),
        'w_expert_down': nrm(ks[15], (DEPTH, N_EXPERTS, D_FF_EXPERT, D_MODEL), D_FF_EXPERT ** -0.5),
        'final_norm_g': 1.0 + nrm(ks[16], (D_MODEL,), 0.01),
    }


def reference(x_prompt, x_sample, attn_norm_g, w_in, w_branch_attn, w_branch_fourier, b_gate, w_out,
              ffn_norm_g, w_router_group, b_router_group, w_router_expert, b_router_expert,
              w_expert_gate, w_expert_up, w_expert_down, final_norm_g):
    y_prompt = encoder_trunk(x_prompt, attn_norm_g, w_in, w_branch_attn, w_branch_fourier, b_gate, w_out,
                             ffn_norm_g, w_router_group, b_router_group, w_router_expert, b_router_expert,
                             w_expert_gate, w_expert_up, w_expert_down, final_norm_g)
    y_sample = encoder_trunk(x_sample, attn_norm_g, w_in, w_branch_attn, w_branch_fourier, b_gate, w_out,
                             ffn_norm_g, w_router_group, b_router_group, w_router_expert, b_router_expert,
                             w_expert_gate, w_expert_up, w_expert_down, final_norm_g)
    return (y_prompt, y_sample)
```

```python
import math
from contextlib import ExitStack

import numpy as np
import concourse.bass as bass
import concourse.mybir as mybir
from concourse.bass_utils import run_bass_kernel_spmd

F32 = mybir.dt.float32
BF16 = mybir.dt.bfloat16
AF = mybir.ActivationFunctionType
ALU = mybir.AluOpType
AX = mybir.AxisListType

FULL_CFG = dict(D=4096, HPG=8, FGW=512, DFF=1024, S_S=4096, S_P=2048)
NGRP = 3
DIL = (1, 4, 16)
NFG = 4
NEG = 4
EPG = 4
NE = NEG * EPG
NR = NEG + NE
T = 512
EPS = 1e-6
ARENA_WORDS = 48 * 1024


class Buf:
    __slots__ = ("name", "dram", "w", "r", "cnt", "sid")

    def __init__(self, name, dram=False):
        self.name = name
        self.dram = dram
        self.w = {}
        self.r = {}
        self.cnt = 0
        self.sid = None


def _merge(dst, src):
    for k, v in src.items():
        if dst.get(k, -1) < v:
            dst[k] = v


class Op:
    __slots__ = ("emit", "deps", "seq", "dma")


class Prog:
    ENGS = ("pe", "act", "dve", "pool", "sp")

    def __init__(self):
        self.q = {e: [] for e in self.ENGS}
        self.needed = {e: set() for e in self.ENGS}
        self.barrier_deps = {}
        self.slots = []
        self.last_compute = {}

    def op(self, eng, emit, reads=(), writes=(), dma=None):
        o = Op()
        o.emit = emit
        deps = dict(self.barrier_deps)
        for b in reads:
            _merge(deps, b.w)
        for b in writes:
            _merge(deps, b.w)
            _merge(deps, b.r)
        o.seq = len(self.q[eng])
        if dma is not None:
            if dma.sid is None:
                dma.sid = len(self.slots)
                self.slots.append(dma)
            dma.cnt += 1
            key, val = ("dma", dma.sid), dma.cnt * 16
            o.dma = dma.sid
        else:
            key, val = ("eng", eng), o.seq
            o.dma = None
            self.last_compute[eng] = o.seq
        for k, v in deps.items():
            if k[0] == "eng" and not (k[1] == "pe" and eng == "pe"):
                self.needed[k[1]].add(v)
        for b in reads:
            if not b.dram and b.r.get(key, -1) < val:
                b.r[key] = val
        for b in writes:
            if b.dram:
                if b.w.get(key, -1) < val:
                    b.w[key] = val
            elif b.r:
                b.w = {key: val}
                b.r = {}
            else:
                if b.w.get(key, -1) < val:
                    b.w[key] = val
        o.deps = deps
        self.q[eng].append(o)
        return o

    def barrier(self):
        d = {}
        for e, v in self.last_compute.items():
            d[("eng", e)] = v
        for s in self.slots:
            d[("dma", s.sid)] = s.cnt * 16
        self.barrier_deps = d

    def emit_all(self, nc):
        ranks = {}
        for e in self.ENGS:
            ranks[e] = {s: i + 1 for i, s in enumerate(sorted(self.needed[e]))}
        with ExitStack() as st:
            esem = {e: st.enter_context(nc.semaphore("sem_" + e)) for e in self.ENGS}
            dsem = [st.enter_context(nc.semaphore("dsem%d" % i)) for i in range(len(self.slots))]
            block = st.enter_context(nc.Block())

            def run(ename, eng):
                known = {}
                for o in self.q[ename]:
                    for k, v in o.deps.items():
                        if k[0] == "eng":
                            if k[1] == "pe" and ename == "pe":
                                continue
                            sem, val = esem[k[1]], ranks[k[1]][v]
                        else:
                            sem, val = dsem[k[1]], v
                        if known.get(k, -1) >= val:
                            continue
                        eng.wait_ge(sem, val)
                        known[k] = val
                    inst = o.emit(eng)
                    if o.dma is not None:
                        inst.then_inc(dsem[o.dma], 16)
                    elif o.seq in ranks[ename]:
                        inst.then_inc(esem[ename], 1)

            @block.tensor
            def _(e):
                run("pe", e)

            @block.scalar
            def _(e):
                run("act", e)

            @block.vector
            def _(e):
                run("dve", e)

            @block.gpsimd
            def _(e):
                run("pool", e)

            @block.sync
            def _(e):
                run("sp", e)


class Tile:
    __slots__ = ("ap", "buf")

    def __init__(self, ap, buf):
        self.ap = ap
        self.buf = buf


class Arena:
    def __init__(self, ap, nwords):
        self.base = ap
        self.n = nwords
        self.off = 0

    def reset(self):
        self.off = 0

    def alloc(self, name, free_shape, dtype=F32):
        n = 1
        for s in free_shape:
            n *= s
        words = n if dtype == F32 else (n + 1) // 2
        words = (words + 7) // 8 * 8
        assert self.off + words <= self.n, "SBUF arena overflow at %s: %d + %d > %d" % (name, self.off, words, self.n)
        a = self.base[:, self.off:self.off + words]
        self.off += words
        if dtype != F32:
            a = a.bitcast(dtype)
        a = a[:, 0:n]
        if len(free_shape) == 2:
            a = a.rearrange("p (a b) -> p a b", a=free_shape[0])
        elif len(free_shape) == 3:
            a = a.rearrange("p (a b c) -> p a b c", a=free_shape[0], b=free_shape[1])
        return Tile(a, Buf(name))


def build_nc(cfg):
    D, HPG, FGW, DFF = cfg["D"], cfg["HPG"], cfg["FGW"], cfg["DFF"]
    KC = D // 128
    AW = NGRP * HPG * 128
    GW = HPG * 128
    AO = HPG * 128
    FW = NFG * FGW
    FCG = FGW // 128
    IN_W = 3 * AW + FW + 2 * D
    FFC = DFF // 128
    parts = [dict(name="s", S=cfg["S_S"], own=cfg["S_S"] // 2), dict(name="p", S=cfg["S_P"], own=cfg["S_P"] // 2)]
    for p in parts:
        p["ext"] = p["own"] + 1024
        assert p["ext"] <= p["S"]
    scale = 1.0 / math.sqrt(128.0)

    nc = bass.Bass("TRN2", target_bir_lowering=False)

    def din(name, shape, dt=F32):
        return nc.dram_tensor(name, list(shape), dt, kind="ExternalInput").ap()

    def dscr(name, shape, dt=BF16):
        return nc.dram_tensor(name, list(shape), dt, kind="Internal").ap()

    for p in parts:
        n = p["name"]
        p["x"] = din("x_" + n, [p["S"], D])
        p["cs"] = din("cs_" + n, [p["S"], p["own"]])
        p["ss"] = din("ss_" + n, [p["S"], p["own"]])
        p["y"] = nc.dram_tensor("y_" + n, [p["own"], D], F32, kind="ExternalOutput").ap()
        p["qT"] = dscr("qT_" + n, [AW, p["own"]])
        p["kT"] = dscr("kT_" + n, [AW, p["ext"]])
        p["v"] = dscr("v_" + n, [p["ext"], AW])
        p["f"] = dscr("f_" + n, [p["S"], FW])
        p["oT"] = dscr("oT_" + n, [AO, p["own"]])
        p["frT"] = dscr("frT_" + n, [FW, p["own"]])
        p["x1"] = dscr("x1_" + n, [p["own"], D], F32)
        for k in ("qT", "kT", "v", "f", "oT", "frT", "x1", "y"):
            p["b_" + k] = Buf(k + "_" + n, dram=True)
    w_in = din("w_in", [D, IN_W])
    w_ba = din("w_ba", [AO, D])
    w_bf = din("w_bf", [FW, D])
    w_out = din("w_out", [D, D])
    w_r = din("w_r", [D, NR])
    w_eg = din("w_eg", [NE, D, DFF])
    w_eu = din("w_eu", [NE, D, DFF])
    w_ed = din("w_ed", [NE, DFF, D])
    g1T_d = din("g1T", [128, KC])
    g2T_d = din("g2T", [128, KC])
    bgT_d = din("bgT", [128, 2 * KC])
    gf_d = din("gf_b", [128, D])
    br_d = din("br_b", [128, NR])
    ident_d = din("ident", [128, 128])
    cc_d = din("cc", [FGW, FGW])
    sc_d = din("scn", [FGW, FGW])
    ea_d = din("etA", [128, NGRP * HPG, 128])
    eb_d = din("etB", [128, NGRP * HPG, 128])
    ea0_d = din("etA0", [64, NGRP * HPG, 128])

    P = Prog()
    st = ExitStack()
    arena_t = st.enter_context(nc.sbuf_tensor("arena", [128, ARENA_WORDS], F32))
    AR = Arena(arena_t[:], ARENA_WORDS)
    pss = []
    for i in range(8):
        pt = st.enter_context(nc.psum_tensor("ps%d" % i, [128, 512], F32))
        pss.append(Tile(pt[:], Buf("ps%d" % i)))
    psi = [0]

    def PS():
        t = pss[psi[0] % 8]
        psi[0] += 1
        return t

    def MM(ps, out, lhsT, rhs, start, stop, reads):
        P.op("pe", lambda e: e.matmul(out, lhsT, rhs, start=start, stop=stop), reads=reads, writes=[ps.buf])

    def TR(ps, out, in_, ident, reads):
        P.op("pe", lambda e: e.transpose(out, in_, ident), reads=reads, writes=[ps.buf])

    def DMA(q, out, in_, reads, writes, slot):
        P.op(q, lambda e: e.dma_start(out=out, in_=in_), reads=reads, writes=writes, dma=slot)

    def ACT(out, in_, func, reads, writes, bias=None, scale=None, accum=None):
        kw = {}
        if bias is not None:
            kw["bias"] = bias
        if scale is not None:
            kw["scale"] = scale
        if accum is not None:
            kw["accum_out"] = accum
        P.op("act", lambda e: e.activation(out, in_, func, **kw), reads=reads, writes=writes)

    def TT(out, in0, in1, op, reads, writes, eng="dve"):
        P.op(eng, lambda e: e.tensor_tensor(out, in0, in1, op), reads=reads, writes=writes)

    def TS(out, in0, s1, s2, op0, op1, reads, writes, eng="dve"):
        if op1 is None:
            P.op(eng, lambda e: e.tensor_scalar(out, in0, s1, None, op0), reads=reads, writes=writes)
        else:
            P.op(eng, lambda e: e.tensor_scalar(out, in0, s1, s2, op0, op1), reads=reads, writes=writes)

    def STT(out, in0, scalar, in1, op0, op1, reads, writes, eng="dve"):
        P.op(eng, lambda e: e.scalar_tensor_tensor(out, in0, scalar, in1, op0, op1), reads=reads, writes=writes)

    def CP(out, in_, reads, writes, eng="dve"):
        P.op(eng, lambda e: e.tensor_copy(out, in_), reads=reads, writes=writes)

    def wblock(slot, src2d, kchunks, ncols):
        v = slot.ap[:, 0:kchunks, 0:ncols]
        DMA("pool", v, src2d.rearrange("(kc p) n -> p kc n", p=128), [], [slot.buf], slot.buf)
        return v

    def rms_to_T(xt, dstT, col0, gT, junk, small, xs):
        ssq = small.ap[:, 0:1]
        t1 = small.ap[:, 1:2]
        t2 = small.ap[:, 2:3]
        rstd = small.ap[:, 3:4]
        ACT(junk.ap, xt.ap, AF.Square, [xt.buf], [junk.buf, small.buf], accum=ssq)
        TS(t1, ssq, 1.0 / D, EPS, ALU.mult, ALU.add, [small.buf], [small.buf])
        ACT(t2, t1, AF.Sqrt, [small.buf], [small.buf])
        P.op("dve", lambda e: e.reciprocal(rstd, t2), reads=[small.buf], writes=[small.buf])
        TS(xs.ap, xt.ap, rstd, None, ALU.mult, None, [xt.buf, small.buf], [xs.buf])
        for kg in range(KC // 4):
            ps = PS()
            for j in range(4):
                kc = kg * 4 + j
                TR(ps, ps.ap[:, j * 128:(j + 1) * 128], xs.ap[:, kc * 128:(kc + 1) * 128], ident.ap, [xs.buf, ident.buf])
            gb = gT.ap[:, kg * 4:kg * 4 + 4].unsqueeze(2).to_broadcast([128, 4, 128])
            TT(dstT.ap[:, kg * 4:kg * 4 + 4, col0:col0 + 128], ps.ap.rearrange("p (a b) -> p a b", a=4), gb, ALU.mult,
               [ps.buf, gT.buf], [dstT.buf])

    ident = AR.alloc("ident", [128])
    g1T = AR.alloc("g1T", [KC])
    g2T = AR.alloc("g2T", [KC])
    DMA("sp", ident.ap, ident_d, [], [ident.buf], ident.buf)
    DMA("sp", g1T.ap, g1T_d, [], [g1T.buf], g1T.buf)
    DMA("sp", g2T.ap, g2T_d, [], [g2T.buf], g2T.buf)
    const_off = AR.off

    def phase_reset():
        P.barrier()
        AR.off = const_off

    def phase_A():
        phase_reset()
        hT = AR.alloc("hT", [KC, T], BF16)
        xts = [AR.alloc("xtA%d" % i, [D]) for i in range(2)]
        xs = AR.alloc("xsA", [D])
        junk = AR.alloc("junkA", [D], BF16)
        smalls = [AR.alloc("smallA%d" % i, [4]) for i in range(2)]
        wsl = [AR.alloc("wA%d" % i, [KC, 512], BF16) for i in range(2)]
        stg = [AR.alloc("stgA%d" % i, [512], BF16) for i in range(4)]
        wi = [0]
        si = [0]
        xi = [0]
        for p in parts:
            ntile = p["S"] // T
            for tt in range(ntile):
                t0 = tt * T
                if t0 < p["own"]:
                    kind = "own"
                elif t0 < p["ext"]:
                    kind = "halo"
                else:
                    kind = "far"
                for s4 in range(4):
                    xt = xts[xi[0] % 2]
                    sm = smalls[xi[0] % 2]
                    xi[0] += 1
                    DMA("sp", xt.ap, p["x"][t0 + s4 * 128:t0 + (s4 + 1) * 128, :], [], [xt.buf], xt.buf)
                    rms_to_T(xt, hT, s4 * 128, g1T, junk, sm, xs)
                secs = []
                if kind == "own":
                    secs.append((0, AW, "fm", p["qT"], p["b_qT"], 0))
                    secs.append((AW, AW, "fm", p["kT"], p["b_kT"], 0))
                    secs.append((2 * AW, AW, "tm", p["v"], p["b_v"], 0))
                elif kind == "halo":
                    g_lo = 0 if t0 < p["own"] + T else 2
                    secs.append((AW + g_lo * GW, AW - g_lo * GW, "fm", p["kT"], p["b_kT"], g_lo * GW))
                    secs.append((2 * AW + g_lo * GW, AW - g_lo * GW, "tm", p["v"], p["b_v"], g_lo * GW))
                secs.append((3 * AW, FW, "tm", p["f"], p["b_f"], 0))
                for (c0, ncols, mode, dst, dstb, dc0) in secs:
                    bw = 512 if ncols % 512 == 0 else 128
                    for blk in range(ncols // bw):
                        ws = wsl[wi[0] % 2]
                        wi[0] += 1
                        wv = wblock(ws, w_in[:, c0 + blk * bw:c0 + (blk + 1) * bw], KC, bw)
                        if mode == "fm":
                            for j in range(bw // 128):
                                ps = PS()
                                for kc in range(KC):
                                    MM(ps, ps.ap, wv[:, kc, j * 128:(j + 1) * 128], hT.ap[:, kc, :], kc == 0, kc == KC - 1,
                                       [ws.buf, hT.buf])
                                sg = stg[si[0] % 4]
                                si[0] += 1
                                ACT(sg.ap, ps.ap, AF.Copy, [ps.buf], [sg.buf])
                                r0 = dc0 + blk * bw + j * 128
                                DMA("sp", dst[r0:r0 + 128, t0:t0 + T], sg.ap, [sg.buf], [dstb], sg.buf)
                        else:
                            pst = [PS() for _ in range(4)]
                            for kc in range(KC):
                                for s4 in range(4):
                                    MM(pst[s4], pst[s4].ap[:, 0:bw], hT.ap[:, kc, s4 * 128:(s4 + 1) * 128], wv[:, kc, :],
                                       kc == 0, kc == KC - 1, [ws.buf, hT.buf])
                            for s4 in range(4):
                                sg = stg[si[0] % 4]
                                si[0] += 1
                                if s4 % 2 == 0:
                                    ACT(sg.ap[:, 0:bw], pst[s4].ap[:, 0:bw], AF.Copy, [pst[s4].buf], [sg.buf])
                                else:
                                    CP(sg.ap[:, 0:bw], pst[s4].ap[:, 0:bw], [pst[s4].buf], [sg.buf])
                                cc0 = dc0 + blk * bw
                                DMA("sp", dst[t0 + s4 * 128:t0 + (s4 + 1) * 128, cc0:cc0 + bw], sg.ap[:, 0:bw], [sg.buf], [dstb],
                                    sg.buf)

    def phase_B1():
        phase_reset()
        etA = AR.alloc("etA", [NGRP * HPG, 128])
        etB = AR.alloc("etB", [NGRP * HPG, 128])
        etA0 = AR.alloc("etA0", [NGRP * HPG, 128])
        ones = AR.alloc("ones", [128], BF16)
        DMA("sp", etA.ap, ea_d, [], [etA.buf], etA.buf)
        DMA("sp", etB.ap, eb_d, [], [etB.buf], etB.buf)
        DMA("sp", etA0.ap[0:64], ea0_d, [], [etA0.buf], etA0.buf)
        P.op("dve", lambda e: e.memset(ones.ap, 1.0), writes=[ones.buf])
        maxown = max(p["own"] for p in parts)
        ksz = [max(p["own"] + 64 * DIL[g] for p in parts) for g in range(NGRP)]
        vsz = [max((max(1, p["own"] // DIL[g] // 128) + 1) * DIL[g] * 128 for p in parts) for g in range(NGRP)]
        qs = [[AR.alloc("q%d_%d" % (i, g), [maxown], BF16) for g in range(NGRP)] for i in range(2)]
        ks = [[AR.alloc("k%d_%d" % (i, g), [ksz[g]], BF16) for g in range(NGRP)] for i in range(2)]
        vs = [[AR.alloc("v%d_%d" % (i, g), [vsz[g]], BF16) for g in range(NGRP)] for i in range(2)]
        oacc = [AR.alloc("oacc%d" % i, [maxown]) for i in range(2)]
        dacc = [AR.alloc("dacc%d" % i, [maxown]) for i in range(2)]
        ost = [AR.alloc("ost%d" % i, [maxown], BF16) for i in range(2)]
        pfs = [AR.alloc("pf%d" % i, [128]) for i in range(4)]
        pbs = [AR.alloc("pb%d" % i, [128], BF16) for i in range(4)]
        hi = [0]
        bi = [0]
        for p in parts:
            own, ext = p["own"], p["ext"]
            for h in range(HPG):
                par = hi[0] % 2
                hi[0] += 1
                oa, da, os_ = oacc[par], dacc[par], ost[par]
                for g in range(NGRP):
                    d = DIL[g]
                    Lo = own // d
                    eg = own + 64 * d
                    row0 = (g * HPG + h) * 128
                    qt, kt, vt = qs[par][g], ks[par][g], vs[par][g]
                    DMA("sp", qt.ap[:, 0:own], p["qT"][row0:row0 + 128, 0:own], [p["b_qT"]], [qt.buf], qt.buf)
                    DMA("sp", kt.ap[:, 0:eg], p["kT"][row0:row0 + 128, 0:eg], [p["b_kT"]], [kt.buf], kt.buf)
                    nq = max(1, Lo // 128)
                    Q = min(128, Lo)
                    vcol = slice(row0, row0 + 128)
                    MT = nq + 1
                    vv = vt.ap[:, 0:MT * d * 128].rearrange("p (m r c) -> p m r c", m=MT, r=d)
                    DMA("sp", vv[0:64, 0], p["v"][0:64 * d, vcol].rearrange("(p r) c -> p r c", r=d), [p["b_v"]], [vt.buf], vt.buf)
                    if Lo >= 128:
                        for m in range(1, MT):
                            tok0 = (128 * m - 64) * d
                            DMA("sp", vv[:, m], p["v"][tok0:tok0 + 128 * d, vcol].rearrange("(p r) c -> p r c", r=d), [p["b_v"]],
                                [vt.buf], vt.buf)
                    else:
                        DMA("sp", vv[0:Q, 1], p["v"][64 * d:(64 + Q) * d, vcol].rearrange("(p r) c -> p r c", r=d), [p["b_v"]],
                            [vt.buf], vt.buf)
                    eidx = g * HPG + h
                    for r in range(d):
                        for iq in range(nq):
                            i0 = iq * 128
                            qv = qt.ap[:, i0 * d + r:(i0 + Q) * d:d]
                            psO = PS()
                            psD = PS()
                            tiles = []
                            if iq == 0:
                                tiles.append((0, 64, 0, etA0.ap[0:64, eidx, 0:Q], etA0.buf))
                            else:
                                tiles.append((i0 - 64, 128, iq, etA.ap[:, eidx, 0:Q], etA.buf))
                            KB = Q
                            tiles.append((i0 + 64, KB, iq + 1, etB.ap[0:KB, eidx, 0:Q], etB.buf))
                            for ti, (lo, K, m, E, Eb) in enumerate(tiles):
                                kv = kt.ap[:, lo * d + r:(lo + K) * d:d]
                                psS = PS()
                                MM(psS, psS.ap[0:K, 0:Q], kv, qv, True, True, [kt.buf, qt.buf])
                                pf = pfs[bi[0] % 4]
                                pb = pbs[bi[0] % 4]
                                bi[0] += 1
                                ACT(pf.ap[0:K, 0:Q], psS.ap[0:K, 0:Q], AF.Exp, [psS.buf], [pf.buf], scale=scale)
                                TT(pb.ap[0:K, 0:Q], pf.ap[0:K, 0:Q], E, ALU.mult, [pf.buf, Eb], [pb.buf])
                                MM(psO, psO.ap[:, 0:Q], vv[0:K, m, r, :], pb.ap[0:K, 0:Q], ti == 0, ti == 1, [vt.buf, pb.buf])
                                MM(psD, psD.ap[:, 0:Q], ones.ap[0:K, :], pb.ap[0:K, 0:Q], ti == 0, ti == 1, [ones.buf, pb.buf])
                            ov = oa.ap[:, i0 * d + r:(i0 + Q) * d:d]
                            dv = da.ap[:, i0 * d + r:(i0 + Q) * d:d]
                            if g == 0:
                                ACT(ov, psO.ap[:, 0:Q], AF.Copy, [psO.buf], [oa.buf])
                                CP(dv, psD.ap[:, 0:Q], [psD.buf], [da.buf])
                            else:
                                TT(ov, ov, psO.ap[:, 0:Q], ALU.add, [psO.buf, oa.buf], [oa.buf])
                                TT(dv, dv, psD.ap[:, 0:Q], ALU.add, [psD.buf, da.buf], [da.buf])
                P.op("dve", lambda e, a=da.ap[:, 0:own]: e.reciprocal(a, a), reads=[da.buf], writes=[da.buf])
                TT(os_.ap[:, 0:own], oa.ap[:, 0:own], da.ap[:, 0:own], ALU.mult, [oa.buf, da.buf], [os_.buf], eng="pool")
                DMA("sp", p["oT"][h * 128:(h + 1) * 128, 0:own], os_.ap[:, 0:own], [os_.buf], [p["b_oT"]], os_.buf)

    def phase_B2():
        phase_reset()
        ccs = AR.alloc("ccs", [FCG, FGW], BF16)
        scs = AR.alloc("scs", [FCG, FGW], BF16)
        wblock(ccs, cc_d, FCG, FGW)
        wblock(scs, sc_d, FCG, FGW)
        maxS = max(p["S"] for p in parts)
        csb = AR.alloc("csb", [maxS // 128, T], BF16)
        ssb = AR.alloc("ssb", [maxS // 128, T], BF16)
        xf = [AR.alloc("xf%d" % i, [maxS // 128, FGW], BF16) for i in range(2)]
        abT = [AR.alloc("abT%d" % i, [2 * FCG, T], BF16) for i in range(2)]
        ystg = [AR.alloc("ystg%d" % i, [T], BF16) for i in range(4)]
        xi = [0]
        yi = [0]
        for p in parts:
            NC_ = p["S"] // 128
            for kb in range(p["own"] // T):
                k0 = kb * T
                csv = wblock(csb, p["cs"][:, k0:k0 + T], NC_, T)
                ssv = wblock(ssb, p["ss"][:, k0:k0 + T], NC_, T)
                for fg in range(NFG):
                    x_ = xf[xi[0] % 2]
                    ab = abT[xi[0] % 2]
                    xi[0] += 1
                    xv = x_.ap[:, 0:NC_, :]
                    DMA("sp", xv, p["f"][:, fg * FGW:(fg + 1) * FGW].rearrange("(n p) c -> p n c", p=128), [p["b_f"]], [x_.buf], x_.buf)
                    for fc in range(FCG):
                        for which, mat, matb in ((0, csv, csb.buf), (1, ssv, ssb.buf)):
                            ps = PS()
                            for n in range(NC_):
                                MM(ps, ps.ap, xv[:, n, fc * 128:(fc + 1) * 128], mat[:, n, :], n == 0, n == NC_ - 1, [x_.buf, matb])
                            if which == 0:
                                ACT(ab.ap[:, fc, :], ps.ap, AF.Copy, [ps.buf], [ab.buf])
                            else:
                                CP(ab.ap[:, FCG + fc, :], ps.ap, [ps.buf], [ab.buf])
                    for oc in range(FCG):
                        ps = PS()
                        for c in range(2 * FCG):
                            m_ = ccs if c < FCG else scs
                            MM(ps, ps.ap, m_.ap[:, c % FCG, oc * 128:(oc + 1) * 128], ab.ap[:, c, :], c == 0, c == 2 * FCG - 1,
                               [m_.buf, ab.buf])
                        ys = ystg[yi[0] % 4]
                        yi[0] += 1
                        ACT(ys.ap, ps.ap, AF.Copy, [ps.buf], [ys.buf])
                        r0 = fg * FGW + oc * 128
                        DMA("sp", p["frT"][r0:r0 + 128, k0:k0 + T], ys.ap, [ys.buf], [p["b_frT"]], ys.buf)

    def phase_C1():
        phase_reset()
        bgT = AR.alloc("bgT", [2 * KC])
        DMA("sp", bgT.ap, bgT_d, [], [bgT.buf], bgT.buf)
        hT = AR.alloc("hTc", [KC, T], BF16)
        mT = AR.alloc("mTc", [KC, T], BF16)
        oTt = AR.alloc("oTt", [AO // 128, T], BF16)
        frt = AR.alloc("frt", [FW // 128, T], BF16)
        xts = [AR.alloc("xtC%d" % i, [D]) for i in range(1)]
        junk = AR.alloc("junkC", [D], BF16)
        smalls = [AR.alloc("smallC%d" % i, [4]) for i in range(2)]
        WB = 256
        WKC = max(KC, AO // 128 + FW // 128)
        wsl = [AR.alloc("wC%d" % i, [WKC, WB], BF16) for i in range(3)]
        gts = [AR.alloc("gt%d" % i, [T]) for i in range(4)]
        tmp = [AR.alloc("tmpC%d" % i, [T]) for i in range(4)]
        xr = [AR.alloc("xr%d" % i, [WB]) for i in range(4)]
        wi = [0]
        gi = [0]
        xi = [0]
        ri = [0]

        def nextw():
            w = wsl[wi[0] % 3]
            wi[0] += 1
            return w

        for p in parts:
            for tt in range(p["own"] // T):
                t0 = tt * T
                DMA("sp", oTt.ap, p["oT"][:, t0:t0 + T].rearrange("(c p) t -> p c t", p=128), [p["b_oT"]], [oTt.buf], oTt.buf)
                DMA("sp", frt.ap, p["frT"][:, t0:t0 + T].rearrange("(c p) t -> p c t", p=128), [p["b_frT"]], [frt.buf], frt.buf)
                for s4 in range(4):
                    xt = xts[0]
                    sm = smalls[xi[0] % 2]
                    xi[0] += 1
                    DMA("sp", xt.ap, p["x"][t0 + s4 * 128:t0 + (s4 + 1) * 128, :], [], [xt.buf], xt.buf)
                    rms_to_T(xt, hT, s4 * 128, g1T, junk, sm, xt)
                for cb in range(D // WB):
                    wga = nextw()
                    wgav = wblock(wga, w_in[:, 3 * AW + FW + cb * WB:3 * AW + FW + (cb + 1) * WB], KC, WB)
                    wgf = nextw()
                    wgfv = wblock(wgf, w_in[:, 3 * AW + FW + D + cb * WB:3 * AW + FW + D + (cb + 1) * WB], KC, WB)
                    wbr = nextw()
                    wbav = wbr.ap[:, 0:AO // 128, 0:WB]
                    DMA("pool", wbav, w_ba[:, cb * WB:(cb + 1) * WB].rearrange("(kc p) n -> p kc n", p=128), [], [wbr.buf], wbr.buf)
                    wbfv = wbr.ap[:, AO // 128:AO // 128 + FW // 128, 0:WB]
                    DMA("pool", wbfv, w_bf[:, cb * WB:(cb + 1) * WB].rearrange("(kc p) n -> p kc n", p=128), [], [wbr.buf], wbr.buf)
                    for j in range(WB // 128):
                        c = cb * (WB // 128) + j
                        cs_ = slice(j * 128, (j + 1) * 128)
                        psA, psF, psa, psf = PS(), PS(), PS(), PS()
                        for kc in range(KC):
                            MM(psA, psA.ap, wgav[:, kc, cs_], hT.ap[:, kc, :], kc == 0, kc == KC - 1, [wga.buf, hT.buf])
                        for kc in range(KC):
                            MM(psF, psF.ap, wgfv[:, kc, cs_], hT.ap[:, kc, :], kc == 0, kc == KC - 1, [wgf.buf, hT.buf])
                        na = AO // 128
                        for kc in range(na):
                            MM(psa, psa.ap, wbav[:, kc, cs_], oTt.ap[:, kc, :], kc == 0, kc == na - 1, [wbr.buf, oTt.buf])
                        nf = FW // 128
                        for kc in range(nf):
                            MM(psf, psf.ap, wbfv[:, kc, cs_], frt.ap[:, kc, :], kc == 0, kc == nf - 1, [wbr.buf, frt.buf])
                        gA = gts[gi[0] % 4]
                        gF = gts[(gi[0] + 1) % 4]
                        t1 = tmp[gi[0] % 4]
                        t2 = tmp[(gi[0] + 1) % 4]
                        gi[0] += 2
                        ACT(gA.ap, psA.ap, AF.Sigmoid, [psA.buf, bgT.buf], [gA.buf], bias=bgT.ap[:, c:c + 1])
                        ACT(gF.ap, psF.ap, AF.Sigmoid, [psF.buf, bgT.buf], [gF.buf], bias=bgT.ap[:, KC + c:KC + c + 1])
                        TT(t1.ap, gA.ap, psa.ap, ALU.mult, [gA.buf, psa.buf], [t1.buf])
                        TT(t2.ap, gF.ap, psf.ap, ALU.mult, [gF.buf, psf.buf], [t2.buf])
                        TT(mT.ap[:, c, :], t1.ap, t2.ap, ALU.add, [t1.buf, t2.buf], [mT.buf], eng="pool")
                for cb in range(D // WB):
                    ws = nextw()
                    wv = wblock(ws, w_out[:, cb * WB:(cb + 1) * WB], KC, WB)
                    pst = [PS() for _ in range(4)]
                    for kc in range(KC):
                        for s4 in range(4):
                            MM(pst[s4], pst[s4].ap[:, 0:WB], mT.ap[:, kc, s4 * 128:(s4 + 1) * 128], wv[:, kc, :], kc == 0, kc == KC - 1,
                               [ws.buf, mT.buf])
                    for s4 in range(4):
                        x_ = xr[ri[0] % 4]
                        ri[0] += 1
                        rows = slice(t0 + s4 * 128, t0 + (s4 + 1) * 128)
                        DMA("sp", x_.ap, p["x"][rows, cb * WB:(cb + 1) * WB], [], [x_.buf], x_.buf)
                        TT(x_.ap, x_.ap, pst[s4].ap[:, 0:WB], ALU.add, [x_.buf, pst[s4].buf], [x_.buf])
                        DMA("sp", p["x1"][rows, cb * WB:(cb + 1) * WB], x_.ap, [x_.buf], [p["b_x1"]], x_.buf)

    def phase_C2():
        phase_reset()
        gfb = AR.alloc("gfb", [D])
        brb = AR.alloc("brb", [NR])
        wrs = AR.alloc("wrs", [KC, NR], BF16)
        DMA("sp", gfb.ap, gf_d, [], [gfb.buf], gfb.buf)
        DMA("sp", brb.ap, br_d, [], [brb.buf], brb.buf)
        wblock(wrs, w_r, KC, NR)
        xt = AR.alloc("x1t", [4, D])
        hT = AR.alloc("h2T", [KC, T], BF16)
        xs = AR.alloc("xsD", [D])
        junk = xs
        smalls = [AR.alloc("smallD%d" % i, [4]) for i in range(2)]
        comb = AR.alloc("comb", [4, NE])
        rt = AR.alloc("rt", [4, 64])
        WB = 128
        DB = min(512, D)
        WKC = max(KC, (FFC * DB + WB - 1) // WB)
        NWS = 4
        wsl = [AR.alloc("wD%d" % i, [WKC, WB], BF16) for i in range(NWS)]
        aT = [AR.alloc("aT%d" % i, [FFC, T], BF16) for i in range(2)]
        sil = [AR.alloc("sil%d" % i, [T]) for i in range(3)]
        wi = [0]
        ai = [0]
        li = [0]
        xi = [0]

        def nextw():
            w = wsl[wi[0] % NWS]
            wi[0] += 1
            return w

        for p in parts:
            for tt in range(p["own"] // T):
                t0 = tt * T
                for s4 in range(4):
                    xv = Tile(xt.ap[:, s4, :], xt.buf)
                    sm = smalls[xi[0] % 2]
                    xi[0] += 1
                    DMA("sp", xv.ap, p["x1"][t0 + s4 * 128:t0 + (s4 + 1) * 128, :], [p["b_x1"]], [xt.buf], xt.buf)
                    rms_to_T(xv, hT, s4 * 128, g2T, junk, sm, xs)
                for s4 in range(4):
                    ps = PS()
                    for kc in range(KC):
                        MM(ps, ps.ap[:, 0:NR], hT.ap[:, kc, s4 * 128:(s4 + 1) * 128], wrs.ap[:, kc, :], kc == 0, kc == KC - 1,
                           [hT.buf, wrs.buf])
                    R = rt.ap[:, s4, :]
                    rb = [rt.buf]
                    lg = R[:, 0:NR]
                    TT(lg, ps.ap[:, 0:NR], brb.ap, ALU.add, [ps.buf, brb.buf], rb)
                    gmax = R[:, 20:21]
                    P.op("dve", lambda e, o=gmax, i=lg[:, 0:NEG]: e.reduce_max(o, i, AX.X), reads=rb, writes=rb)
                    oh = R[:, 21:25]
                    TS(oh, lg[:, 0:NEG], gmax, None, ALU.is_equal, None, rb, rb)
                    ngm = R[:, 25:26]
                    TS(ngm, gmax, -1.0, None, ALU.mult, None, rb, rb)
                    eg_ = R[:, 26:30]
                    sg_ = R[:, 30:31]
                    ACT(eg_, lg[:, 0:NEG], AF.Exp, rb, rb, bias=ngm, accum=sg_)
                    pg = R[:, 31:32]
                    P.op("dve", lambda e, o=pg, i=sg_: e.reciprocal(o, i), reads=rb, writes=rb)
                    les = R[:, 32:36]
                    TS(les, lg[:, NEG:NEG + EPG], oh[:, 0:1], None, ALU.mult, None, rb, rb)
                    for g in range(1, NEG):
                        STT(les, lg[:, NEG + g * EPG:NEG + (g + 1) * EPG], oh[:, g:g + 1], les, ALU.mult, ALU.add, rb, rb)
                    m1 = R[:, 36:37]
                    P.op("dve", lambda e, o=m1, i=les: e.reduce_max(o, i, AX.X), reads=rb, writes=rb)
                    k1 = R[:, 37:41]
                    TS(k1, les, m1, None, ALU.is_equal, None, rb, rb)
                    le2 = R[:, 41:45]
                    STT(le2, k1, -1e30, les, ALU.mult, ALU.add, rb, rb)
                    m2 = R[:, 45:46]
                    P.op("dve", lambda e, o=m2, i=le2: e.reduce_max(o, i, AX.X), reads=rb, writes=rb)
                    k2 = R[:, 46:50]
                    TS(k2, le2, m2, None, ALU.is_equal, None, rb, rb)
                    dm = R[:, 50:51]
                    TT(dm, m2, m1, ALU.subtract, rb, rb)
                    ex = R[:, 51:52]
                    ACT(ex, dm, AF.Exp, rb, rb)
                    den = R[:, 52:53]
                    TS(den, ex, 1.0, None, ALU.add, None, rb, rb)
                    p1 = R[:, 53:54]
                    P.op("dve", lambda e, o=p1, i=den: e.reciprocal(o, i), reads=rb, writes=rb)
                    p2 = R[:, 54:55]
                    TT(p2, ex, p1, ALU.mult, rb, rb)
                    TT(p1, p1, pg, ALU.mult, rb, rb)
                    TT(p2, p2, pg, ALU.mult, rb, rb)
                    cw = R[:, 55:59]
                    TS(cw, k1, p1, None, ALU.mult, None, rb, rb)
                    STT(cw, k2, p2, cw, ALU.mult, ALU.add, rb, rb)
                    for g in range(NEG):
                        TS(comb.ap[:, s4, g * EPG:(g + 1) * EPG], cw, oh[:, g:g + 1], None, ALU.mult, None, rb, [comb.buf])
                for ex_ in range(NE):
                    a_ = aT[ai[0] % 2]
                    ai[0] += 1
                    for fb in range(DFF // WB):
                        wg = nextw()
                        wgv = wblock(wg, w_eg[ex_, :, fb * WB:(fb + 1) * WB], KC, WB)
                        wu = nextw()
                        wuv = wblock(wu, w_eu[ex_, :, fb * WB:(fb + 1) * WB], KC, WB)
                        for j in range(WB // 128):
                            fc = fb * (WB // 128) + j
                            cs_ = slice(j * 128, (j + 1) * 128)
                            psG, psU = PS(), PS()
                            for kc in range(KC):
                                MM(psG, psG.ap, wgv[:, kc, cs_], hT.ap[:, kc, :], kc == 0, kc == KC - 1, [wg.buf, hT.buf])
                            for kc in range(KC):
                                MM(psU, psU.ap, wuv[:, kc, cs_], hT.ap[:, kc, :], kc == 0, kc == KC - 1, [wu.buf, hT.buf])
                            s_ = sil[li[0] % 3]
                            li[0] += 1
                            ACT(s_.ap, psG.ap, AF.Silu, [psG.buf], [s_.buf])
                            TT(a_.ap[:, fc, :], s_.ap, psU.ap, ALU.mult, [s_.buf, psU.buf], [a_.buf])
                    for cb in range(D // DB):
                        wd = nextw()
                        wdv = wd.ap.rearrange("p a b -> p (a b)")[:, 0:FFC * DB].rearrange("p (a b) -> p a b", a=FFC)
                        DMA("pool", wdv, w_ed[ex_, :, cb * DB:(cb + 1) * DB].rearrange("(kc p) n -> p kc n", p=128), [], [wd.buf], wd.buf)
                        pst = [PS() for _ in range(4)]
                        for kc in range(FFC):
                            for s4 in range(4):
                                MM(pst[s4], pst[s4].ap[:, 0:DB], a_.ap[:, kc, s4 * 128:(s4 + 1) * 128], wdv[:, kc, :], kc == 0, kc == FFC - 1,
                                   [wd.buf, a_.buf])
                        for s4 in range(4):
                            xv = xt.ap[:, s4, cb * DB:(cb + 1) * DB]
                            STT(xv, pst[s4].ap[:, 0:DB], comb.ap[:, s4, ex_:ex_ + 1], xv, ALU.mult, ALU.add, [pst[s4].buf, comb.buf, xt.buf], [xt.buf])
                for s4 in range(4):
                    sm = smalls[xi[0] % 2]
                    xi[0] += 1
                    xv = xt.ap[:, s4, :]
                    ssq, t1, t2, rstd = sm.ap[:, 0:1], sm.ap[:, 1:2], sm.ap[:, 2:3], sm.ap[:, 3:4]
                    ACT(junk.ap, xv, AF.Square, [xt.buf], [junk.buf, sm.buf], accum=ssq)
                    TS(t1, ssq, 1.0 / D, EPS, ALU.mult, ALU.add, [sm.buf], [sm.buf])
                    ACT(t2, t1, AF.Sqrt, [sm.buf], [sm.buf])
                    P.op("dve", lambda e, o=rstd, i=t2: e.reciprocal(o, i), reads=[sm.buf], writes=[sm.buf])
                    STT(xs.ap, xv, rstd, gfb.ap, ALU.mult, ALU.mult, [xt.buf, sm.buf, gfb.buf], [xs.buf])
                    DMA("sp", p["y"][t0 + s4 * 128:t0 + (s4 + 1) * 128, :], xs.ap, [xs.buf], [p["b_y"]], xs.buf)

    phase_A()
    phase_B1()
    phase_B2()
    phase_C1()
    phase_C2()
    P.op("sp", lambda e: e.nop(), reads=[p["b_y"] for p in parts])
    P.emit_all(nc)
    st.close()
    return nc


def _slopes(HPG):
    nh = NGRP * HPG
    s = 2.0 ** (-8.0 * np.arange(1, nh + 1) / nh)
    return s.astype(np.float32).reshape(NGRP, HPG)


def _const_tables(cfg):
    HPG, FGW = cfg["HPG"], cfg["FGW"]
    sl = _slopes(HPG).astype(np.float64)
    a = np.arange(128)[:, None]
    b = np.arange(128)[None, :]
    etA = np.zeros((128, NGRP * HPG, 128), np.float32)
    etB = np.zeros((128, NGRP * HPG, 128), np.float32)
    for g in range(NGRP):
        for h in range(HPG):
            s = np.float64(np.float32(sl[g, h])) * DIL[g]
            ea = np.where(a >= b, np.exp(-s * np.abs(a - b - 64)), 0.0)
            eb = np.where(a <= b, np.exp(-s * np.abs(a - b + 64)), 0.0)
            etA[:, g * HPG + h, :] = ea
            etB[:, g * HPG + h, :] = eb
    etA0 = np.ascontiguousarray(etA[64:128])
    c = np.arange(FGW)
    ang = 2.0 * np.pi * np.outer(c, c) / FGW
    cc = (np.cos(ang) / np.sqrt(FGW)).astype(np.float32)
    scn = (-np.sin(ang) / np.sqrt(FGW)).astype(np.float32)
    return dict(etA=etA, etB=etB, etA0=etA0, cc=cc, scn=scn, ident=np.eye(128, dtype=np.float32))


def _dft_local(S, own, half):
    loc = np.arange(S, dtype=np.int64)
    glob = loc if half == 0 else S - 1 - loc
    prod = np.outer(glob, glob[:own]) % S
    ang = 2.0 * np.pi * prod / S
    return (np.cos(ang) / np.sqrt(S)).astype(np.float32), (np.sin(ang) / np.sqrt(S)).astype(np.float32)


def _run(cfg, x_prompt, x_sample, attn_norm_g, w_in, w_branch_attn, w_branch_fourier, b_gate, w_out, ffn_norm_g,
         w_router_group, b_router_group, w_router_expert, b_router_expert, w_expert_gate, w_expert_up, w_expert_down,
         final_norm_g, n_cores=8):
    D = cfg["D"]
    KC = D // 128
    f = lambda a: np.ascontiguousarray(np.asarray(a, dtype=np.float32))
    x_prompt, x_sample = f(x_prompt), f(x_sample)
    shared = dict(
        w_in=f(w_in)[0], w_ba=f(w_branch_attn)[0], w_bf=f(w_branch_fourier)[0], w_out=f(w_out)[0],
        w_r=np.ascontiguousarray(np.concatenate(
            [f(w_router_group)[0], f(w_router_expert)[0].transpose(1, 0, 2).reshape(D, NE)], axis=1)),
        w_eg=f(w_expert_gate)[0], w_eu=f(w_expert_up)[0], w_ed=f(w_expert_down)[0],
        g1T=np.ascontiguousarray(f(attn_norm_g)[0].reshape(KC, 128).T),
        g2T=np.ascontiguousarray(f(ffn_norm_g)[0].reshape(KC, 128).T),
        bgT=np.ascontiguousarray(f(b_gate)[0].reshape(2 * KC, 128).T),
        gf_b=np.ascontiguousarray(np.broadcast_to(f(final_norm_g)[None, :], (128, D))),
        br_b=np.ascontiguousarray(np.broadcast_to(
            np.concatenate([f(b_router_group)[0], f(b_router_expert)[0].reshape(NE)])[None, :], (128, NR))),
    )
    shared.update(_const_tables(cfg))
    dft = {}
    for nm, S in (("s", cfg["S_S"]), ("p", cfg["S_P"])):
        for half in (0, 1):
            dft[(nm, half)] = _dft_local(S, S // 2, half)
    in_maps = []
    for c in range(n_cores):
        b, half = c // 2, c % 2
        m = dict(shared)
        xs = x_sample[b] if half == 0 else x_sample[b, ::-1]
        xp = x_prompt[b] if half == 0 else x_prompt[b, ::-1]
        m["x_s"] = np.ascontiguousarray(xs)
        m["x_p"] = np.ascontiguousarray(xp)
        m["cs_s"], m["ss_s"] = dft[("s", half)]
        m["cs_p"], m["ss_p"] = dft[("p", half)]
        in_maps.append(m)
    nc = build_nc(cfg)
    res = run_bass_kernel_spmd(nc, in_maps, core_ids=list(range(n_cores)))
    nb = n_cores // 2
    y_p = np.zeros((nb, cfg["S_P"], D), np.float32)
    y_s = np.zeros((nb, cfg["S_S"], D), np.float32)
    for c in range(n_cores):
        b, half = c // 2, c % 2
        r = res.results[c]
        for nm, S, dst in (("p", cfg["S_P"], y_p), ("s", cfg["S_S"], y_s)):
            o = np.asarray(r["y_" + nm], dtype=np.float32)
            if half == 0:
                dst[b, :S // 2] = o
            else:
                dst[b, S // 2:] = o[::-1]
    return y_p, y_s


def kernel(**inputs):
    return _run(FULL_CFG, **inputs)
```

```python
import math
from contextlib import ExitStack

import numpy as np
import concourse.bass as bass
import concourse.mybir as mybir
from concourse.bass_utils import run_bass_kernel_spmd

F32 = mybir.dt.float32
BF16 = mybir.dt.bfloat16
I32 = mybir.dt.int32
AF = mybir.ActivationFunctionType
ALU = mybir.AluOpType
AX = mybir.AxisListType

FULL_CFG = dict(D=4096, HPG=8, FGW=512, DFF=1024, S_S=4096, S_P=2048)
NGRP = 3
DIL = (1, 4, 16)
NFG = 4
NEG = 4
EPG = 4
NE = NEG * EPG
NR = NEG + NE
T = 512
EPS = 1e-6
ARENA_WORDS = 48 * 1024


class Buf:
    __slots__ = ("name", "dram", "w", "r", "cnt", "sid")

    def __init__(self, name, dram=False):
        self.name = name
        self.dram = dram
        self.w = {}
        self.r = {}
        self.cnt = 0
        self.sid = None


def _merge(dst, src):
    for k, v in src.items():
        if dst.get(k, -1) < v:
            dst[k] = v


class Op:
    __slots__ = ("emit", "deps", "seq", "dma")


class Prog:
    ENGS = ("pe", "act", "dve", "pool", "sp")

    def __init__(self):
        self.q = {e: [] for e in self.ENGS}
        self.needed = {e: set() for e in self.ENGS}
        self.barrier_deps = {}
        self.slots = []
        self.last_compute = {}

    def op(self, eng, emit, reads=(), writes=(), dma=None):
        o = Op()
        o.emit = emit
        deps = dict(self.barrier_deps)
        for b in reads:
            _merge(deps, b.w)
        for b in writes:
            _merge(deps, b.w)
            _merge(deps, b.r)
        o.seq = len(self.q[eng])
        if dma is not None:
            if dma.sid is None:
                dma.sid = len(self.slots)
                self.slots.append(dma)
            dma.cnt += 1
            key, val = ("dma", dma.sid), dma.cnt * 16
            o.dma = dma.sid
        else:
            key, val = ("eng", eng), o.seq
            o.dma = None
            self.last_compute[eng] = o.seq
        for k, v in deps.items():
            if k[0] == "eng" and not (k[1] == "pe" and eng == "pe"):
                self.needed[k[1]].add(v)
        for b in reads:
            if not b.dram and b.r.get(key, -1) < val:
                b.r[key] = val
        for b in writes:
            if b.dram:
                if b.w.get(key, -1) < val:
                    b.w[key] = val
            elif b.r:
                b.w = {key: val}
                b.r = {}
            else:
                if b.w.get(key, -1) < val:
                    b.w[key] = val
        o.deps = deps
        self.q[eng].append(o)
        return o

    def barrier(self):
        d = {}
        for e, v in self.last_compute.items():
            d[("eng", e)] = v
        for s in self.slots:
            d[("dma", s.sid)] = s.cnt * 16
        self.barrier_deps = d

    def emit_all(self, nc):
        ranks = {}
        for e in self.ENGS:
            ranks[e] = {s: i + 1 for i, s in enumerate(sorted(self.needed[e]))}
        with ExitStack() as st:
            esem = {e: st.enter_context(nc.semaphore("sem_" + e)) for e in self.ENGS}
            dsem = [st.enter_context(nc.semaphore("dsem%d" % i)) for i in range(len(self.slots))]
            block = st.enter_context(nc.Block())

            def run(ename, eng):
                known = {}
                for o in self.q[ename]:
                    for k, v in o.deps.items():
                        if k[0] == "eng":
                            if k[1] == "pe" and ename == "pe":
                                continue
                            sem, val = esem[k[1]], ranks[k[1]][v]
                        else:
                            sem, val = dsem[k[1]], v
                        if known.get(k, -1) >= val:
                            continue
                        eng.wait_ge(sem, val)
                        known[k] = val
                    inst = o.emit(eng)
                    if o.dma is not None:
                        inst.then_inc(dsem[o.dma], 16)
                    elif o.seq in ranks[ename]:
                        inst.then_inc(esem[ename], 1)

            @block.tensor
            def _(e):
                run("pe", e)

            @block.scalar
            def _(e):
                run("act", e)

            @block.vector
            def _(e):
                run("dve", e)

            @block.gpsimd
            def _(e):
                run("pool", e)

            @block.sync
            def _(e):
                run("sp", e)


class Tile:
    __slots__ = ("ap", "buf")

    def __init__(self, ap, buf):
        self.ap = ap
        self.buf = buf


class Arena:
    def __init__(self, ap, nwords):
        self.base = ap
        self.n = nwords
        self.off = 0

    def reset(self):
        self.off = 0

    def alloc(self, name, free_shape, dtype=F32):
        n = 1
        for s in free_shape:
            n *= s
        words = n if dtype in (F32, I32) else (n + 1) // 2
        words = (words + 7) // 8 * 8
        assert self.off + words <= self.n, "SBUF arena overflow at %s: %d + %d > %d" % (name, self.off, words, self.n)
        a = self.base[:, self.off:self.off + words]
        self.off += words
        if dtype != F32:
            a = a.bitcast(dtype)
        a = a[:, 0:n]
        if len(free_shape) == 2:
            a = a.rearrange("p (a b) -> p a b", a=free_shape[0])
        elif len(free_shape) == 3:
            a = a.rearrange("p (a b c) -> p a b c", a=free_shape[0], b=free_shape[1])
        return Tile(a, Buf(name))


def build_nc(cfg):
    D, HPG, FGW, DFF = cfg["D"], cfg["HPG"], cfg["FGW"], cfg["DFF"]
    KC = D // 128
    AW = NGRP * HPG * 128
    GW = HPG * 128
    AO = HPG * 128
    FW = NFG * FGW
    FCG = FGW // 128
    IN_W = 3 * AW + FW + 2 * D
    FFC = DFF // 128
    parts = [dict(name="s", S=cfg["S_S"], own=cfg["S_S"] // 2), dict(name="p", S=cfg["S_P"], own=cfg["S_P"] // 2)]
    for p in parts:
        p["ext"] = p["own"] + 1024
        assert p["ext"] <= p["S"]
    scale = 1.0 / math.sqrt(128.0)

    nc = bass.Bass("TRN2", target_bir_lowering=False)

    def din(name, shape, dt=F32):
        return nc.dram_tensor(name, list(shape), dt, kind="ExternalInput").ap()

    def dscr(name, shape, dt=BF16):
        return nc.dram_tensor(name, list(shape), dt, kind="Internal").ap()

    for p in parts:
        n = p["name"]
        p["x"] = din("x_" + n, [p["S"], D])
        p["cs"] = din("cs_" + n, [p["S"], p["own"]])
        p["ss"] = din("ss_" + n, [p["S"], p["own"]])
        p["y"] = nc.dram_tensor("y_" + n, [p["own"], D], F32, kind="ExternalOutput").ap()
        p["qT"] = dscr("qT_" + n, [AW, p["own"]])
        p["kT"] = dscr("kT_" + n, [AW, p["ext"]])
        p["v"] = dscr("v_" + n, [p["ext"], AW])
        p["f"] = dscr("f_" + n, [p["S"], FW])
        p["oT"] = dscr("oT_" + n, [AO, p["own"]])
        p["frT"] = dscr("frT_" + n, [FW, p["own"]])
        p["x1"] = dscr("x1_" + n, [p["own"], D], F32)
        for k in ("qT", "kT", "v", "f", "oT", "frT", "x1", "y"):
            p["b_" + k] = Buf(k + "_" + n, dram=True)
    w_in = din("w_in", [D, IN_W])
    w_ba = din("w_ba", [AO, D])
    w_bf = din("w_bf", [FW, D])
    w_out = din("w_out", [D, D])
    w_r = din("w_r", [D, NR])
    DBX = min(512, D)
    w_eg = din("w_eg", [FFC, NE * 128, KC * 128])
    w_eu = din("w_eu", [FFC, NE * 128, KC * 128])
    w_ed = din("w_ed", [D // DBX, NE * 128, FFC * DBX])
    pidx_d = din("pidx", [128, 1])
    wb_eg = dscr("wb_eg", [FFC * NE * 128, KC * 128])
    wb_eu = dscr("wb_eu", [FFC * NE * 128, KC * 128])
    wb_ed = dscr("wb_ed", [(D // DBX) * NE * 128, FFC * DBX])
    b_wb = Buf("wb", dram=True)
    conv_jobs = []
    for (src3, dst2) in ((w_eg, wb_eg), (w_eu, wb_eu), (w_ed, wb_ed)):
        src2 = src3.rearrange("f r n -> (f r) n")
        for r0 in range(0, dst2.shape[0], 128):
            conv_jobs.append((src2, dst2, r0))
    conv_state = dict(i=0, slots=None)

    def emit_conv(n):
        for _ in range(n):
            if conv_state["i"] >= len(conv_jobs):
                return
            src2, dst2, r0 = conv_jobs[conv_state["i"]]
            sl = conv_state["slots"][conv_state["i"] % len(conv_state["slots"])]
            conv_state["i"] += 1
            ncol = dst2.shape[1]
            CW = 1024 if ncol % 1024 == 0 else ncol
            v = sl.ap[:, 0:ncol]
            DMA("pool", v.rearrange("p (a b) -> p a b", b=CW), src2[r0:r0 + 128, :].rearrange("p (a b) -> p a b", b=CW), [], [sl.buf], sl.buf)
            DMA("sp", dst2[r0:r0 + 128, :], v, [sl.buf], [b_wb], conv_state["ssem"][(conv_state["i"] - 1) % len(conv_state["slots"])])
    g1T_d = din("g1T", [128, KC])
    g2T_d = din("g2T", [128, KC])
    bgT_d = din("bgT", [128, 2 * KC])
    gf_d = din("gf_b", [128, D])
    br_d = din("br_b", [128, NR])
    ident_d = din("ident", [128, 128])
    cc_d = din("cc", [FGW, FGW])
    sc_d = din("scn", [FGW, FGW])
    ea_d = din("etA", [128, NGRP * HPG, 128])
    eb_d = din("etB", [128, NGRP * HPG, 128])
    ea0_d = din("etA0", [64, NGRP * HPG, 128])
    TB = cfg.get("TB", 256)
    NTOK = sum(p["own"] for p in parts)
    NT = NTOK // 128
    JMAX = 2 * NTOK // TB + NE
    NS = JMAX * TB
    g2b_d = din("g2_b", [128, D])
    jidx_d = din("jidx", [128, JMAX])
    tri_d = din("tri", [128, 128])
    H_d = dscr("H_scr", [NTOK, D])
    Hs_d = dscr("Hs_scr", [NS, D])
    Ys_d = dscr("Ys_scr", [NS, D], F32)
    b_H, b_Hs, b_Ys = Buf("H", dram=True), Buf("Hs", dram=True), Buf("Ys", dram=True)

    P = Prog()
    st = ExitStack()
    arena_t = st.enter_context(nc.sbuf_tensor("arena", [128, ARENA_WORDS], F32))
    AR = Arena(arena_t[:], ARENA_WORDS)
    pss = []
    for i in range(8):
        pt = st.enter_context(nc.psum_tensor("ps%d" % i, [128, 512], F32))
        pss.append(Tile(pt[:], Buf("ps%d" % i)))
    psi = [0]

    def PS():
        t = pss[psi[0] % 8]
        psi[0] += 1
        return t

    def MM(ps, out, lhsT, rhs, start, stop, reads):
        P.op("pe", lambda e: e.matmul(out, lhsT, rhs, start=start, stop=stop), reads=reads, writes=[ps.buf])

    def TR(ps, out, in_, ident, reads):
        P.op("pe", lambda e: e.transpose(out, in_, ident), reads=reads, writes=[ps.buf])

    def DMA(q, out, in_, reads, writes, slot):
        P.op(q, lambda e: e.dma_start(out=out, in_=in_), reads=reads, writes=writes, dma=slot)

    def ACT(out, in_, func, reads, writes, bias=None, scale=None, accum=None):
        kw = {}
        if bias is not None:
            kw["bias"] = bias
        if scale is not None:
            kw["scale"] = scale
        if accum is not None:
            kw["accum_out"] = accum
        P.op("act", lambda e: e.activation(out, in_, func, **kw), reads=reads, writes=writes)

    def TT(out, in0, in1, op, reads, writes, eng="dve"):
        P.op(eng, lambda e: e.tensor_tensor(out, in0, in1, op), reads=reads, writes=writes)

    def TS(out, in0, s1, s2, op0, op1, reads, writes, eng="dve"):
        if op1 is None:
            P.op(eng, lambda e: e.tensor_scalar(out, in0, s1, None, op0), reads=reads, writes=writes)
        else:
            P.op(eng, lambda e: e.tensor_scalar(out, in0, s1, s2, op0, op1), reads=reads, writes=writes)

    def STT(out, in0, scalar, in1, op0, op1, reads, writes, eng="dve"):
        P.op(eng, lambda e: e.scalar_tensor_tensor(out, in0, scalar, in1, op0, op1), reads=reads, writes=writes)

    def CP(out, in_, reads, writes, eng="dve"):
        P.op(eng, lambda e: e.tensor_copy(out, in_), reads=reads, writes=writes)

    def wblock(slot, src2d, kchunks, ncols):
        v = slot.ap[:, 0:kchunks, 0:ncols]
        DMA("pool", v, src2d.rearrange("(kc p) n -> p kc n", p=128), [], [slot.buf], slot.buf)
        return v

    def rms_to_T(xt, dstT, col0, gT, junk, small, xs):
        ssq = small.ap[:, 0:1]
        t1 = small.ap[:, 1:2]
        t2 = small.ap[:, 2:3]
        rstd = small.ap[:, 3:4]
        ACT(junk.ap, xt.ap, AF.Square, [xt.buf], [junk.buf, small.buf], accum=ssq)
        TS(t1, ssq, 1.0 / D, EPS, ALU.mult, ALU.add, [small.buf], [small.buf])
        ACT(t2, t1, AF.Sqrt, [small.buf], [small.buf])
        P.op("dve", lambda e: e.reciprocal(rstd, t2), reads=[small.buf], writes=[small.buf])
        TS(xs.ap, xt.ap, rstd, None, ALU.mult, None, [xt.buf, small.buf], [xs.buf])
        for kg in range(KC // 4):
            ps = PS()
            for j in range(4):
                kc = kg * 4 + j
                TR(ps, ps.ap[:, j * 128:(j + 1) * 128], xs.ap[:, kc * 128:(kc + 1) * 128], ident.ap, [xs.buf, ident.buf])
            gb = gT.ap[:, kg * 4:kg * 4 + 4].unsqueeze(2).to_broadcast([128, 4, 128])
            TT(dstT.ap[:, kg * 4:kg * 4 + 4, col0:col0 + 128], ps.ap.rearrange("p (a b) -> p a b", a=4), gb, ALU.mult,
               [ps.buf, gT.buf], [dstT.buf])

    ident = AR.alloc("ident", [128])
    g1T = AR.alloc("g1T", [KC])
    g2T = AR.alloc("g2T", [KC])
    DMA("sp", ident.ap, ident_d, [], [ident.buf], ident.buf)
    DMA("sp", g1T.ap, g1T_d, [], [g1T.buf], g1T.buf)
    DMA("sp", g2T.ap, g2T_d, [], [g2T.buf], g2T.buf)
    rm1 = AR.alloc("rm1", [NT, NE])
    rm2 = AR.alloc("rm2", [NT, NE])
    rc12 = AR.alloc("rc12", [NT, 2])
    rpos = AR.alloc("rpos", [NT, 2], I32)
    reid = AR.alloc("reid", [JMAX], I32)
    const_off = AR.off

    def phase_reset():
        P.barrier()
        AR.off = const_off

    def phase_A():
        phase_reset()
        hT = AR.alloc("hT", [KC, T], BF16)
        xts = [AR.alloc("xtA%d" % i, [D]) for i in range(2)]
        xs = AR.alloc("xsA", [D])
        junk = AR.alloc("junkA", [D], BF16)
        smalls = [AR.alloc("smallA%d" % i, [4]) for i in range(2)]
        wsl = [AR.alloc("wA%d" % i, [KC, 512], BF16) for i in range(2)]
        stg = [AR.alloc("stgA%d" % i, [512], BF16) for i in range(4)]
        wi = [0]
        si = [0]
        xi = [0]
        for p in parts:
            ntile = p["S"] // T
            for tt in range(ntile):
                t0 = tt * T
                if t0 < p["own"]:
                    kind = "own"
                elif t0 < p["ext"]:
                    kind = "halo"
                else:
                    kind = "far"
                for s4 in range(4):
                    xt = xts[xi[0] % 2]
                    sm = smalls[xi[0] % 2]
                    xi[0] += 1
                    DMA("sp", xt.ap, p["x"][t0 + s4 * 128:t0 + (s4 + 1) * 128, :], [], [xt.buf], xt.buf)
                    rms_to_T(xt, hT, s4 * 128, g1T, junk, sm, xs)
                secs = []
                if kind == "own":
                    secs.append((0, AW, "fm", p["qT"], p["b_qT"], 0))
                    secs.append((AW, AW, "fm", p["kT"], p["b_kT"], 0))
                    secs.append((2 * AW, AW, "tm", p["v"], p["b_v"], 0))
                elif kind == "halo":
                    g_lo = 0 if t0 < p["own"] + T else 2
                    secs.append((AW + g_lo * GW, AW - g_lo * GW, "fm", p["kT"], p["b_kT"], g_lo * GW))
                    secs.append((2 * AW + g_lo * GW, AW - g_lo * GW, "tm", p["v"], p["b_v"], g_lo * GW))
                secs.append((3 * AW, FW, "tm", p["f"], p["b_f"], 0))
                for (c0, ncols, mode, dst, dstb, dc0) in secs:
                    bw = 512 if ncols % 512 == 0 else 128
                    for blk in range(ncols // bw):
                        ws = wsl[wi[0] % 2]
                        wi[0] += 1
                        wv = wblock(ws, w_in[:, c0 + blk * bw:c0 + (blk + 1) * bw], KC, bw)
                        if mode == "fm":
                            for j in range(bw // 128):
                                ps = PS()
                                for kc in range(KC):
                                    MM(ps, ps.ap, wv[:, kc, j * 128:(j + 1) * 128], hT.ap[:, kc, :], kc == 0, kc == KC - 1,
                                       [ws.buf, hT.buf])
                                sg = stg[si[0] % 4]
                                si[0] += 1
                                ACT(sg.ap, ps.ap, AF.Copy, [ps.buf], [sg.buf])
                                r0 = dc0 + blk * bw + j * 128
                                DMA("sp", dst[r0:r0 + 128, t0:t0 + T], sg.ap, [sg.buf], [dstb], sg.buf)
                        else:
                            pst = [PS() for _ in range(4)]
                            for kc in range(KC):
                                for s4 in range(4):
                                    MM(pst[s4], pst[s4].ap[:, 0:bw], hT.ap[:, kc, s4 * 128:(s4 + 1) * 128], wv[:, kc, :],
                                       kc == 0, kc == KC - 1, [ws.buf, hT.buf])
                            for s4 in range(4):
                                sg = stg[si[0] % 4]
                                si[0] += 1
                                if s4 % 2 == 0:
                                    ACT(sg.ap[:, 0:bw], pst[s4].ap[:, 0:bw], AF.Copy, [pst[s4].buf], [sg.buf])
                                else:
                                    CP(sg.ap[:, 0:bw], pst[s4].ap[:, 0:bw], [pst[s4].buf], [sg.buf])
                                cc0 = dc0 + blk * bw
                                DMA("sp", dst[t0 + s4 * 128:t0 + (s4 + 1) * 128, cc0:cc0 + bw], sg.ap[:, 0:bw], [sg.buf], [dstb],
                                    sg.buf)

    def phase_B1():
        phase_reset()
        etA = AR.alloc("etA", [NGRP * HPG, 128])
        etB = AR.alloc("etB", [NGRP * HPG, 128])
        etA0 = AR.alloc("etA0", [NGRP * HPG, 128])
        ones = AR.alloc("ones", [128], BF16)
        DMA("sp", etA.ap, ea_d, [], [etA.buf], etA.buf)
        DMA("sp", etB.ap, eb_d, [], [etB.buf], etB.buf)
        DMA("sp", etA0.ap[0:64], ea0_d, [], [etA0.buf], etA0.buf)
        P.op("dve", lambda e: e.memset(ones.ap, 1.0), writes=[ones.buf])
        maxown = max(p["own"] for p in parts)
        ksz = [max(p["own"] + 64 * DIL[g] for p in parts) for g in range(NGRP)]
        vsz = [max((max(1, p["own"] // DIL[g] // 128) + 1) * DIL[g] * 128 for p in parts) for g in range(NGRP)]
        qs = [[AR.alloc("q%d_%d" % (i, g), [maxown], BF16) for g in range(NGRP)] for i in range(2)]
        ks = [[AR.alloc("k%d_%d" % (i, g), [ksz[g]], BF16) for g in range(NGRP)] for i in range(2)]
        vs = [[AR.alloc("v%d_%d" % (i, g), [vsz[g]], BF16) for g in range(NGRP)] for i in range(2)]
        oacc = [AR.alloc("oacc%d" % i, [maxown]) for i in range(2)]
        dacc = [AR.alloc("dacc%d" % i, [maxown]) for i in range(2)]
        ost = [AR.alloc("ost%d" % i, [maxown], BF16) for i in range(2)]
        pfs = [AR.alloc("pf%d" % i, [128]) for i in range(4)]
        pbs = [AR.alloc("pb%d" % i, [128], BF16) for i in range(4)]
        hi = [0]
        bi = [0]
        cvw = max(KC * 128, FFC * DBX)
        conv_state["slots"] = [AR.alloc("cv%d" % i, [cvw], BF16) for i in range(2)]
        conv_state["ssem"] = [Buf("cvs%d" % i) for i in range(2)]
        per_head = (len(conv_jobs) + 2 * HPG - 1) // (2 * HPG)
        for p in parts:
            own, ext = p["own"], p["ext"]
            for h in range(HPG):
                if not cfg.get("dense_moe", False):
                    emit_conv(per_head)
                par = hi[0] % 2
                hi[0] += 1
                oa, da, os_ = oacc[par], dacc[par], ost[par]
                for g in range(NGRP):
                    d = DIL[g]
                    Lo = own // d
                    eg = own + 64 * d
                    row0 = (g * HPG + h) * 128
                    qt, kt, vt = qs[par][g], ks[par][g], vs[par][g]
                    DMA("sp", qt.ap[:, 0:own], p["qT"][row0:row0 + 128, 0:own], [p["b_qT"]], [qt.buf], qt.buf)
                    DMA("sp", kt.ap[:, 0:eg], p["kT"][row0:row0 + 128, 0:eg], [p["b_kT"]], [kt.buf], kt.buf)
                    nq = max(1, Lo // 128)
                    Q = min(128, Lo)
                    vcol = slice(row0, row0 + 128)
                    MT = nq + 1
                    vv = vt.ap[:, 0:MT * d * 128].rearrange("p (m r c) -> p m r c", m=MT, r=d)
                    DMA("sp", vv[0:64, 0], p["v"][0:64 * d, vcol].rearrange("(p r) c -> p r c", r=d), [p["b_v"]], [vt.buf], vt.buf)
                    if Lo >= 128:
                        for m in range(1, MT):
                            tok0 = (128 * m - 64) * d
                            DMA("sp", vv[:, m], p["v"][tok0:tok0 + 128 * d, vcol].rearrange("(p r) c -> p r c", r=d), [p["b_v"]],
                                [vt.buf], vt.buf)
                    else:
                        DMA("sp", vv[0:Q, 1], p["v"][64 * d:(64 + Q) * d, vcol].rearrange("(p r) c -> p r c", r=d), [p["b_v"]],
                            [vt.buf], vt.buf)
                    eidx = g * HPG + h
                    for r in range(d):
                        for iq in range(nq):
                            i0 = iq * 128
                            qv = qt.ap[:, i0 * d + r:(i0 + Q) * d:d]
                            psO = PS()
                            psD = PS()
                            tiles = []
                            if iq == 0:
                                tiles.append((0, 64, 0, etA0.ap[0:64, eidx, 0:Q], etA0.buf))
                            else:
                                tiles.append((i0 - 64, 128, iq, etA.ap[:, eidx, 0:Q], etA.buf))
                            KB = Q
                            tiles.append((i0 + 64, KB, iq + 1, etB.ap[0:KB, eidx, 0:Q], etB.buf))
                            for ti, (lo, K, m, E, Eb) in enumerate(tiles):
                                kv = kt.ap[:, lo * d + r:(lo + K) * d:d]
                                psS = PS()
                                MM(psS, psS.ap[0:K, 0:Q], kv, qv, True, True, [kt.buf, qt.buf])
                                pf = pfs[bi[0] % 4]
                                pb = pbs[bi[0] % 4]
                                bi[0] += 1
                                ACT(pf.ap[0:K, 0:Q], psS.ap[0:K, 0:Q], AF.Exp, [psS.buf], [pf.buf], scale=scale)
                                TT(pb.ap[0:K, 0:Q], pf.ap[0:K, 0:Q], E, ALU.mult, [pf.buf, Eb], [pb.buf])
                                MM(psO, psO.ap[:, 0:Q], vv[0:K, m, r, :], pb.ap[0:K, 0:Q], ti == 0, ti == 1, [vt.buf, pb.buf])
                                MM(psD, psD.ap[:, 0:Q], ones.ap[0:K, :], pb.ap[0:K, 0:Q], ti == 0, ti == 1, [ones.buf, pb.buf])
                            ov = oa.ap[:, i0 * d + r:(i0 + Q) * d:d]
                            dv = da.ap[:, i0 * d + r:(i0 + Q) * d:d]
                            if g == 0:
                                ACT(ov, psO.ap[:, 0:Q], AF.Copy, [psO.buf], [oa.buf])
                                CP(dv, psD.ap[:, 0:Q], [psD.buf], [da.buf])
                            else:
                                TT(ov, ov, psO.ap[:, 0:Q], ALU.add, [psO.buf, oa.buf], [oa.buf])
                                TT(dv, dv, psD.ap[:, 0:Q], ALU.add, [psD.buf, da.buf], [da.buf])
                P.op("dve", lambda e, a=da.ap[:, 0:own]: e.reciprocal(a, a), reads=[da.buf], writes=[da.buf])
                TT(os_.ap[:, 0:own], oa.ap[:, 0:own], da.ap[:, 0:own], ALU.mult, [oa.buf, da.buf], [os_.buf], eng="pool")
                DMA("sp", p["oT"][h * 128:(h + 1) * 128, 0:own], os_.ap[:, 0:own], [os_.buf], [p["b_oT"]], os_.buf)

    def phase_B2():
        phase_reset()
        ccs = AR.alloc("ccs", [FCG, FGW], BF16)
        scs = AR.alloc("scs", [FCG, FGW], BF16)
        wblock(ccs, cc_d, FCG, FGW)
        wblock(scs, sc_d, FCG, FGW)
        maxS = max(p["S"] for p in parts)
        csb = AR.alloc("csb", [maxS // 128, T], BF16)
        ssb = AR.alloc("ssb", [maxS // 128, T], BF16)
        xf = [AR.alloc("xf%d" % i, [maxS // 128, FGW], BF16) for i in range(2)]
        abT = [AR.alloc("abT%d" % i, [2 * FCG, T], BF16) for i in range(2)]
        ystg = [AR.alloc("ystg%d" % i, [T], BF16) for i in range(4)]
        xi = [0]
        yi = [0]
        for p in parts:
            NC_ = p["S"] // 128
            for kb in range(p["own"] // T):
                k0 = kb * T
                csv = wblock(csb, p["cs"][:, k0:k0 + T], NC_, T)
                ssv = wblock(ssb, p["ss"][:, k0:k0 + T], NC_, T)
                for fg in range(NFG):
                    x_ = xf[xi[0] % 2]
                    ab = abT[xi[0] % 2]
                    xi[0] += 1
                    xv = x_.ap[:, 0:NC_, :]
                    DMA("sp", xv, p["f"][:, fg * FGW:(fg + 1) * FGW].rearrange("(n p) c -> p n c", p=128), [p["b_f"]], [x_.buf], x_.buf)
                    for fc in range(FCG):
                        for which, mat, matb in ((0, csv, csb.buf), (1, ssv, ssb.buf)):
                            ps = PS()
                            for n in range(NC_):
                                MM(ps, ps.ap, xv[:, n, fc * 128:(fc + 1) * 128], mat[:, n, :], n == 0, n == NC_ - 1, [x_.buf, matb])
                            if which == 0:
                                ACT(ab.ap[:, fc, :], ps.ap, AF.Copy, [ps.buf], [ab.buf])
                            else:
                                CP(ab.ap[:, FCG + fc, :], ps.ap, [ps.buf], [ab.buf])
                    for oc in range(FCG):
                        ps = PS()
                        for c in range(2 * FCG):
                            m_ = ccs if c < FCG else scs
                            MM(ps, ps.ap, m_.ap[:, c % FCG, oc * 128:(oc + 1) * 128], ab.ap[:, c, :], c == 0, c == 2 * FCG - 1,
                               [m_.buf, ab.buf])
                        ys = ystg[yi[0] % 4]
                        yi[0] += 1
                        ACT(ys.ap, ps.ap, AF.Copy, [ps.buf], [ys.buf])
                        r0 = fg * FGW + oc * 128
                        DMA("sp", p["frT"][r0:r0 + 128, k0:k0 + T], ys.ap, [ys.buf], [p["b_frT"]], ys.buf)

    def phase_C1():
        phase_reset()
        bgT = AR.alloc("bgT", [2 * KC])
        DMA("sp", bgT.ap, bgT_d, [], [bgT.buf], bgT.buf)
        hT = AR.alloc("hTc", [KC, T], BF16)
        mT = AR.alloc("mTc", [KC, T], BF16)
        oTt = AR.alloc("oTt", [AO // 128, T], BF16)
        frt = AR.alloc("frt", [FW // 128, T], BF16)
        xts = [AR.alloc("xtC%d" % i, [D]) for i in range(1)]
        junk = AR.alloc("junkC", [D], BF16)
        smalls = [AR.alloc("smallC%d" % i, [4]) for i in range(2)]
        WB = 256
        WKC = max(KC, AO // 128 + FW // 128)
        wsl = [AR.alloc("wC%d" % i, [WKC, WB], BF16) for i in range(3)]
        gts = [AR.alloc("gt%d" % i, [T]) for i in range(4)]
        tmp = [AR.alloc("tmpC%d" % i, [T]) for i in range(4)]
        xr = [AR.alloc("xr%d" % i, [WB]) for i in range(4)]
        wi = [0]
        gi = [0]
        xi = [0]
        ri = [0]

        def nextw():
            w = wsl[wi[0] % 3]
            wi[0] += 1
            return w

        for p in parts:
            for tt in range(p["own"] // T):
                t0 = tt * T
                DMA("sp", oTt.ap, p["oT"][:, t0:t0 + T].rearrange("(c p) t -> p c t", p=128), [p["b_oT"]], [oTt.buf], oTt.buf)
                DMA("sp", frt.ap, p["frT"][:, t0:t0 + T].rearrange("(c p) t -> p c t", p=128), [p["b_frT"]], [frt.buf], frt.buf)
                for s4 in range(4):
                    xt = xts[0]
                    sm = smalls[xi[0] % 2]
                    xi[0] += 1
                    DMA("sp", xt.ap, p["x"][t0 + s4 * 128:t0 + (s4 + 1) * 128, :], [], [xt.buf], xt.buf)
                    rms_to_T(xt, hT, s4 * 128, g1T, junk, sm, xt)
                for cb in range(D // WB):
                    wga = nextw()
                    wgav = wblock(wga, w_in[:, 3 * AW + FW + cb * WB:3 * AW + FW + (cb + 1) * WB], KC, WB)
                    wgf = nextw()
                    wgfv = wblock(wgf, w_in[:, 3 * AW + FW + D + cb * WB:3 * AW + FW + D + (cb + 1) * WB], KC, WB)
                    wbr = nextw()
                    wbav = wbr.ap[:, 0:AO // 128, 0:WB]
                    DMA("pool", wbav, w_ba[:, cb * WB:(cb + 1) * WB].rearrange("(kc p) n -> p kc n", p=128), [], [wbr.buf], wbr.buf)
                    wbfv = wbr.ap[:, AO // 128:AO // 128 + FW // 128, 0:WB]
                    DMA("pool", wbfv, w_bf[:, cb * WB:(cb + 1) * WB].rearrange("(kc p) n -> p kc n", p=128), [], [wbr.buf], wbr.buf)
                    for j in range(WB // 128):
                        c = cb * (WB // 128) + j
                        cs_ = slice(j * 128, (j + 1) * 128)
                        psA, psF, psa, psf = PS(), PS(), PS(), PS()
                        for kc in range(KC):
                            MM(psA, psA.ap, wgav[:, kc, cs_], hT.ap[:, kc, :], kc == 0, kc == KC - 1, [wga.buf, hT.buf])
                        for kc in range(KC):
                            MM(psF, psF.ap, wgfv[:, kc, cs_], hT.ap[:, kc, :], kc == 0, kc == KC - 1, [wgf.buf, hT.buf])
                        na = AO // 128
                        for kc in range(na):
                            MM(psa, psa.ap, wbav[:, kc, cs_], oTt.ap[:, kc, :], kc == 0, kc == na - 1, [wbr.buf, oTt.buf])
                        nf = FW // 128
                        for kc in range(nf):
                            MM(psf, psf.ap, wbfv[:, kc, cs_], frt.ap[:, kc, :], kc == 0, kc == nf - 1, [wbr.buf, frt.buf])
                        gA = gts[gi[0] % 4]
                        gF = gts[(gi[0] + 1) % 4]
                        t1 = tmp[gi[0] % 4]
                        t2 = tmp[(gi[0] + 1) % 4]
                        gi[0] += 2
                        ACT(gA.ap, psA.ap, AF.Sigmoid, [psA.buf, bgT.buf], [gA.buf], bias=bgT.ap[:, c:c + 1])
                        ACT(gF.ap, psF.ap, AF.Sigmoid, [psF.buf, bgT.buf], [gF.buf], bias=bgT.ap[:, KC + c:KC + c + 1])
                        TT(t1.ap, gA.ap, psa.ap, ALU.mult, [gA.buf, psa.buf], [t1.buf])
                        TT(t2.ap, gF.ap, psf.ap, ALU.mult, [gF.buf, psf.buf], [t2.buf])
                        TT(mT.ap[:, c, :], t1.ap, t2.ap, ALU.add, [t1.buf, t2.buf], [mT.buf], eng="pool")
                for cb in range(D // WB):
                    ws = nextw()
                    wv = wblock(ws, w_out[:, cb * WB:(cb + 1) * WB], KC, WB)
                    pst = [PS() for _ in range(4)]
                    for kc in range(KC):
                        for s4 in range(4):
                            MM(pst[s4], pst[s4].ap[:, 0:WB], mT.ap[:, kc, s4 * 128:(s4 + 1) * 128], wv[:, kc, :], kc == 0, kc == KC - 1,
                               [ws.buf, mT.buf])
                    for s4 in range(4):
                        x_ = xr[ri[0] % 4]
                        ri[0] += 1
                        rows = slice(t0 + s4 * 128, t0 + (s4 + 1) * 128)
                        DMA("sp", x_.ap, p["x"][rows, cb * WB:(cb + 1) * WB], [], [x_.buf], x_.buf)
                        TT(x_.ap, x_.ap, pst[s4].ap[:, 0:WB], ALU.add, [x_.buf, pst[s4].buf], [x_.buf])
                        DMA("sp", p["x1"][rows, cb * WB:(cb + 1) * WB], x_.ap, [x_.buf], [p["b_x1"]], x_.buf)

    def phase_C2():
        phase_reset()
        gfb = AR.alloc("gfb", [D])
        brb = AR.alloc("brb", [NR])
        wrs = AR.alloc("wrs", [KC, NR], BF16)
        DMA("sp", gfb.ap, gf_d, [], [gfb.buf], gfb.buf)
        DMA("sp", brb.ap, br_d, [], [brb.buf], brb.buf)
        wblock(wrs, w_r, KC, NR)
        xt = AR.alloc("x1t", [4, D])
        hT = AR.alloc("h2T", [KC, T], BF16)
        xs = AR.alloc("xsD", [D])
        junk = xs
        smalls = [AR.alloc("smallD%d" % i, [4]) for i in range(2)]
        comb = AR.alloc("comb", [4, NE])
        rt = AR.alloc("rt", [4, 64])
        WB = 128
        DB = min(512, D)
        WKC = max(KC, (FFC * DB + WB - 1) // WB)
        NWS = 4
        wsl = [AR.alloc("wD%d" % i, [WKC, WB], BF16) for i in range(NWS)]
        aT = [AR.alloc("aT%d" % i, [FFC, T], BF16) for i in range(2)]
        sil = [AR.alloc("sil%d" % i, [T]) for i in range(3)]
        wi = [0]
        ai = [0]
        li = [0]
        xi = [0]

        def nextw():
            w = wsl[wi[0] % NWS]
            wi[0] += 1
            return w

        for p in parts:
            for tt in range(p["own"] // T):
                t0 = tt * T
                for s4 in range(4):
                    xv = Tile(xt.ap[:, s4, :], xt.buf)
                    sm = smalls[xi[0] % 2]
                    xi[0] += 1
                    DMA("sp", xv.ap, p["x1"][t0 + s4 * 128:t0 + (s4 + 1) * 128, :], [p["b_x1"]], [xt.buf], xt.buf)
                    rms_to_T(xv, hT, s4 * 128, g2T, junk, sm, xs)
                for s4 in range(4):
                    ps = PS()
                    for kc in range(KC):
                        MM(ps, ps.ap[:, 0:NR], hT.ap[:, kc, s4 * 128:(s4 + 1) * 128], wrs.ap[:, kc, :], kc == 0, kc == KC - 1,
                           [hT.buf, wrs.buf])
                    R = rt.ap[:, s4, :]
                    rb = [rt.buf]
                    lg = R[:, 0:NR]
                    TT(lg, ps.ap[:, 0:NR], brb.ap, ALU.add, [ps.buf, brb.buf], rb)
                    gmax = R[:, 20:21]
                    P.op("dve", lambda e, o=gmax, i=lg[:, 0:NEG]: e.reduce_max(o, i, AX.X), reads=rb, writes=rb)
                    oh = R[:, 21:25]
                    TS(oh, lg[:, 0:NEG], gmax, None, ALU.is_equal, None, rb, rb)
                    ngm = R[:, 25:26]
                    TS(ngm, gmax, -1.0, None, ALU.mult, None, rb, rb)
                    eg_ = R[:, 26:30]
                    sg_ = R[:, 30:31]
                    ACT(eg_, lg[:, 0:NEG], AF.Exp, rb, rb, bias=ngm, accum=sg_)
                    pg = R[:, 31:32]
                    P.op("dve", lambda e, o=pg, i=sg_: e.reciprocal(o, i), reads=rb, writes=rb)
                    les = R[:, 32:36]
                    TS(les, lg[:, NEG:NEG + EPG], oh[:, 0:1], None, ALU.mult, None, rb, rb)
                    for g in range(1, NEG):
                        STT(les, lg[:, NEG + g * EPG:NEG + (g + 1) * EPG], oh[:, g:g + 1], les, ALU.mult, ALU.add, rb, rb)
                    m1 = R[:, 36:37]
                    P.op("dve", lambda e, o=m1, i=les: e.reduce_max(o, i, AX.X), reads=rb, writes=rb)
                    k1 = R[:, 37:41]
                    TS(k1, les, m1, None, ALU.is_equal, None, rb, rb)
                    le2 = R[:, 41:45]
                    STT(le2, k1, -1e30, les, ALU.mult, ALU.add, rb, rb)
                    m2 = R[:, 45:46]
                    P.op("dve", lambda e, o=m2, i=le2: e.reduce_max(o, i, AX.X), reads=rb, writes=rb)
                    k2 = R[:, 46:50]
                    TS(k2, le2, m2, None, ALU.is_equal, None, rb, rb)
                    dm = R[:, 50:51]
                    TT(dm, m2, m1, ALU.subtract, rb, rb)
                    ex = R[:, 51:52]
                    ACT(ex, dm, AF.Exp, rb, rb)
                    den = R[:, 52:53]
                    TS(den, ex, 1.0, None, ALU.add, None, rb, rb)
                    p1 = R[:, 53:54]
                    P.op("dve", lambda e, o=p1, i=den: e.reciprocal(o, i), reads=rb, writes=rb)
                    p2 = R[:, 54:55]
                    TT(p2, ex, p1, ALU.mult, rb, rb)
                    TT(p1, p1, pg, ALU.mult, rb, rb)
                    TT(p2, p2, pg, ALU.mult, rb, rb)
                    cw = R[:, 55:59]
                    TS(cw, k1, p1, None, ALU.mult, None, rb, rb)
                    STT(cw, k2, p2, cw, ALU.mult, ALU.add, rb, rb)
                    for g in range(NEG):
                        TS(comb.ap[:, s4, g * EPG:(g + 1) * EPG], cw, oh[:, g:g + 1], None, ALU.mult, None, rb, [comb.buf])
                for ex_ in range(NE):
                    a_ = aT[ai[0] % 2]
                    ai[0] += 1
                    for fb in range(DFF // WB):
                        wg = nextw()
                        wgv = wblock(wg, w_eg[ex_, :, fb * WB:(fb + 1) * WB], KC, WB)
                        wu = nextw()
                        wuv = wblock(wu, w_eu[ex_, :, fb * WB:(fb + 1) * WB], KC, WB)
                        for j in range(WB // 128):
                            fc = fb * (WB // 128) + j
                            cs_ = slice(j * 128, (j + 1) * 128)
                            psG, psU = PS(), PS()
                            for kc in range(KC):
                                MM(psG, psG.ap, wgv[:, kc, cs_], hT.ap[:, kc, :], kc == 0, kc == KC - 1, [wg.buf, hT.buf])
                            for kc in range(KC):
                                MM(psU, psU.ap, wuv[:, kc, cs_], hT.ap[:, kc, :], kc == 0, kc == KC - 1, [wu.buf, hT.buf])
                            s_ = sil[li[0] % 3]
                            li[0] += 1
                            ACT(s_.ap, psG.ap, AF.Silu, [psG.buf], [s_.buf])
                            TT(a_.ap[:, fc, :], s_.ap, psU.ap, ALU.mult, [s_.buf, psU.buf], [a_.buf])
                    for cb in range(D // DB):
                        wd = nextw()
                        wdv = wd.ap.rearrange("p a b -> p (a b)")[:, 0:FFC * DB].rearrange("p (a b) -> p a b", a=FFC)
                        DMA("pool", wdv, w_ed[ex_, :, cb * DB:(cb + 1) * DB].rearrange("(kc p) n -> p kc n", p=128), [], [wd.buf], wd.buf)
                        pst = [PS() for _ in range(4)]
                        for kc in range(FFC):
                            for s4 in range(4):
                                MM(pst[s4], pst[s4].ap[:, 0:DB], a_.ap[:, kc, s4 * 128:(s4 + 1) * 128], wdv[:, kc, :], kc == 0, kc == FFC - 1,
                                   [wd.buf, a_.buf])
                        for s4 in range(4):
                            xv = xt.ap[:, s4, cb * DB:(cb + 1) * DB]
                            STT(xv, pst[s4].ap[:, 0:DB], comb.ap[:, s4, ex_:ex_ + 1], xv, ALU.mult, ALU.add, [pst[s4].buf, comb.buf, xt.buf], [xt.buf])
                for s4 in range(4):
                    sm = smalls[xi[0] % 2]
                    xi[0] += 1
                    xv = xt.ap[:, s4, :]
                    ssq, t1, t2, rstd = sm.ap[:, 0:1], sm.ap[:, 1:2], sm.ap[:, 2:3], sm.ap[:, 3:4]
                    ACT(junk.ap, xv, AF.Square, [xt.buf], [junk.buf, sm.buf], accum=ssq)
                    TS(t1, ssq, 1.0 / D, EPS, ALU.mult, ALU.add, [sm.buf], [sm.buf])
                    ACT(t2, t1, AF.Sqrt, [sm.buf], [sm.buf])
                    P.op("dve", lambda e, o=rstd, i=t2: e.reciprocal(o, i), reads=[sm.buf], writes=[sm.buf])
                    STT(xs.ap, xv, rstd, gfb.ap, ALU.mult, ALU.mult, [xt.buf, sm.buf, gfb.buf], [xs.buf])
                    DMA("sp", p["y"][t0 + s4 * 128:t0 + (s4 + 1) * 128, :], xs.ap, [xs.buf], [p["b_y"]], xs.buf)


    def IDMA(out, out_off, in_, in_off, reads, writes, slot, eoff=0):
        def emit(e):
            oo = bass.IndirectOffsetOnAxis(ap=out_off, axis=0) if out_off is not None else None
            io = bass.IndirectOffsetOnAxis(ap=in_off, axis=0) if in_off is not None else None
            return e.indirect_dma_start(out=out, out_offset=oo, in_=in_, in_offset=io, element_offset=eoff)
        P.op("pool", emit, reads=reads, writes=writes, dma=slot)

    def phase_C2_sparse():
        subt = [(p, t0) for p in parts for t0 in range(0, p["own"], 128)]
        phase_reset()
        g2b = AR.alloc("g2b", [D])
        brb = AR.alloc("brb", [NR])
        wrs = AR.alloc("wrs", [KC, NR], BF16)
        DMA("sp", g2b.ap, g2b_d, [], [g2b.buf], g2b.buf)
        DMA("sp", brb.ap, br_d, [], [brb.buf], brb.buf)
        wblock(wrs, w_r, KC, NR)
        hT = AR.alloc("h2T", [KC, T], BF16)
        xts = [AR.alloc("x1a%d" % i, [D]) for i in range(2)]
        xs = AR.alloc("xsD", [D])
        htm = [AR.alloc("htm%d" % i, [D], BF16) for i in range(2)]
        smalls = [AR.alloc("smallD%d" % i, [4]) for i in range(2)]
        rt = AR.alloc("rt", [4, 64])
        xi = [0]
        for tb_ in range(NT // 4):
            for s4 in range(4):
                i = tb_ * 4 + s4
                p, t0 = subt[i]
                xt = xts[xi[0] % 2]
                sm = smalls[xi[0] % 2]
                hm = htm[xi[0] % 2]
                xi[0] += 1
                DMA("sp", xt.ap, p["x1"][t0:t0 + 128, :], [p["b_x1"]], [xt.buf], xt.buf)
                rms_to_T(xt, hT, s4 * 128, g2T, xs, sm, xs)
                TT(hm.ap, xs.ap, g2b.ap, ALU.mult, [xs.buf, g2b.buf], [hm.buf], eng="pool")
                DMA("sp", H_d[i * 128:(i + 1) * 128, :], hm.ap, [hm.buf], [b_H], hm.buf)
            for s4 in range(4):
                i = tb_ * 4 + s4
                ps = PS()
                for kc in range(KC):
                    MM(ps, ps.ap[:, 0:NR], hT.ap[:, kc, s4 * 128:(s4 + 1) * 128], wrs.ap[:, kc, :], kc == 0, kc == KC - 1,
                       [hT.buf, wrs.buf])
                R = rt.ap[:, s4, :]
                rb = [rt.buf]
                lg = R[:, 0:NR]
                TT(lg, ps.ap[:, 0:NR], brb.ap, ALU.add, [ps.buf, brb.buf], rb)
                gmax = R[:, 20:21]
                P.op("dve", lambda e, o=gmax, i_=lg[:, 0:NEG]: e.reduce_max(o, i_, AX.X), reads=rb, writes=rb)
                oh = R[:, 21:25]
                TS(oh, lg[:, 0:NEG], gmax, None, ALU.is_equal, None, rb, rb)
                ngm = R[:, 25:26]
                TS(ngm, gmax, -1.0, None, ALU.mult, None, rb, rb)
                eg_ = R[:, 26:30]
                sg_ = R[:, 30:31]
                ACT(eg_, lg[:, 0:NEG], AF.Exp, rb, rb, bias=ngm, accum=sg_)
                pg = R[:, 31:32]
                P.op("dve", lambda e, o=pg, i_=sg_: e.reciprocal(o, i_), reads=rb, writes=rb)
                les = R[:, 32:36]
                TS(les, lg[:, NEG:NEG + EPG], oh[:, 0:1], None, ALU.mult, None, rb, rb)
                for g in range(1, NEG):
                    STT(les, lg[:, NEG + g * EPG:NEG + (g + 1) * EPG], oh[:, g:g + 1], les, ALU.mult, ALU.add, rb, rb)
                m1 = R[:, 36:37]
                P.op("dve", lambda e, o=m1, i_=les: e.reduce_max(o, i_, AX.X), reads=rb, writes=rb)
                k1 = R[:, 37:41]
                TS(k1, les, m1, None, ALU.is_equal, None, rb, rb)
                le2 = R[:, 41:45]
                STT(le2, k1, -1e30, les, ALU.mult, ALU.add, rb, rb)
                m2 = R[:, 45:46]
                P.op("dve", lambda e, o=m2, i_=le2: e.reduce_max(o, i_, AX.X), reads=rb, writes=rb)
                k2 = R[:, 46:50]
                TS(k2, le2, m2, None, ALU.is_equal, None, rb, rb)
                dm = R[:, 50:51]
                TT(dm, m2, m1, ALU.subtract, rb, rb)
                ex = R[:, 51:52]
                ACT(ex, dm, AF.Exp, rb, rb)
                den = R[:, 52:53]
                TS(den, ex, 1.0, None, ALU.add, None, rb, rb)
                p1 = R[:, 53:54]
                P.op("dve", lambda e, o=p1, i_=den: e.reciprocal(o, i_), reads=rb, writes=rb)
                p2 = R[:, 54:55]
                TT(p2, ex, p1, ALU.mult, rb, rb)
                TT(rc12.ap[:, i, 0:1], p1, pg, ALU.mult, rb, [rc12.buf])
                TT(rc12.ap[:, i, 1:2], p2, pg, ALU.mult, rb, [rc12.buf])
                for g in range(NEG):
                    TS(rm1.ap[:, i, g * EPG:(g + 1) * EPG], k1, oh[:, g:g + 1], None, ALU.mult, None, rb, [rm1.buf])
                    TS(rm2.ap[:, i, g * EPG:(g + 1) * EPG], k2, oh[:, g:g + 1], None, ALU.mult, None, rb, [rm2.buf])
        Mbf = AR.alloc("Mbf", [NT, NE], BF16)
        onesb = AR.alloc("onesb", [128], BF16)
        trib = AR.alloc("trib", [128], BF16)
        Cs = AR.alloc("Cs", [NT, NE])
        posf = AR.alloc("posf", [NT, NE])
        prod = AR.alloc("prod", [NT, NE])
        pf12 = AR.alloc("pf12", [2, NT])
        tb = AR.alloc("tb", [96])
        jid = AR.alloc("jid", [JMAX])
        eacc = AR.alloc("eacc", [JMAX])
        DMA("pool", trib.ap, tri_d, [], [trib.buf], trib.buf)
        DMA("sp", jid.ap, jidx_d, [], [jid.buf], jid.buf)
        P.op("dve", lambda e: e.memset(onesb.ap, 1.0), writes=[onesb.buf])
        TT(Mbf.ap, rm1.ap, rm2.ap, ALU.add, [rm1.buf, rm2.buf], [Mbf.buf])
        for i in range(NT):
            ps = PS()
            for i2 in range(i):
                MM(ps, ps.ap[:, 0:NE], onesb.ap, Mbf.ap[:, i2, :], i2 == 0, False, [onesb.buf, Mbf.buf])
            MM(ps, ps.ap[:, 0:NE], trib.ap, Mbf.ap[:, i, :], i == 0, True, [trib.buf, Mbf.buf])
            CP(Cs.ap[:, i, :], ps.ap[:, 0:NE], [ps.buf], [Cs.buf])
        ps = PS()
        for i in range(NT):
            MM(ps, ps.ap[:, 0:NE], onesb.ap, Mbf.ap[:, i, :], i == 0, i == NT - 1, [onesb.buf, Mbf.buf])
        tbb = [tb.buf]
        n_ = tb.ap[:, 0:16]
        ntl = tb.ap[:, 16:32]
        cum = tb.ap[:, 32:48]
        bm1 = tb.ap[:, 48:64]
        tmp_ = tb.ap[:, 64:80]
        CP(n_, ps.ap[:, 0:NE], [ps.buf], tbb)
        P.op("dve", lambda e: e.memset(ntl, 0.0), reads=tbb, writes=tbb)
        for j in range(NTOK // TB):
            STT(ntl, n_, float(j * TB), ntl, ALU.is_gt, ALU.add, tbb, tbb)
        CP(cum[:, 0:1], ntl[:, 0:1], tbb, tbb)
        for e_ in range(1, NE):
            TT(cum[:, e_:e_ + 1], cum[:, e_ - 1:e_], ntl[:, e_:e_ + 1], ALU.add, tbb, tbb)
        TT(tmp_, cum, ntl, ALU.subtract, tbb, tbb)
        TS(bm1, tmp_, float(TB), -1.0, ALU.mult, ALU.add, tbb, tbb)
        TT(posf.ap, Cs.ap, bm1.unsqueeze(1).to_broadcast([128, NT, NE]), ALU.add, [Cs.buf] + tbb, [posf.buf])
        for k, rm in ((0, rm1), (1, rm2)):
            TT(prod.ap, posf.ap, rm.ap, ALU.mult, [posf.buf, rm.buf], [prod.buf])
            P.op("dve", lambda e, o=pf12.ap[:, k, :], i_=prod.ap: e.reduce_sum(o, i_, AX.X), reads=[prod.buf], writes=[pf12.buf])
            CP(rpos.ap[:, :, k], pf12.ap[:, k, :], [pf12.buf], [rpos.buf])
        P.op("dve", lambda e: e.memset(eacc.ap, 0.0), writes=[eacc.buf])
        for e_ in range(NE):
            STT(eacc.ap, jid.ap, cum[:, e_:e_ + 1], eacc.ap, ALU.is_ge, ALU.add, [jid.buf, eacc.buf] + tbb, [eacc.buf])
        TS(eacc.ap, eacc.ap, float(NE - 1), None, ALU.min, None, [eacc.buf], [eacc.buf])
        pidx = AR.alloc("pidx", [1])
        DMA("sp", pidx.ap, pidx_d, [], [pidx.buf], pidx.buf)
        TS(eacc.ap, eacc.ap, 128.0, pidx.ap[:, 0:1], ALU.mult, ALU.add, [eacc.buf, pidx.buf], [eacc.buf])
        CP(reid.ap, eacc.ap, [eacc.buf], [reid.buf])
        phase_reset()
        hb = [AR.alloc("hb%d" % i, [D], BF16) for i in range(2)]
        hbsem = [Buf("hbsem%d" % i) for i in range(2)]
        zt = AR.alloc("zt", [D], BF16)
        b_Hs0 = Buf("Hs0", dram=True)
        P.op("dve", lambda e: e.memset(zt.ap, 0.0), writes=[zt.buf])
        for r in range(NS // 128):
            DMA("sp", Hs_d[r * 128:(r + 1) * 128, :], zt.ap, [zt.buf], [b_Hs0], zt.buf)
        for i in range(NT):
            h = hb[i % 2]
            DMA("sp", h.ap, H_d[i * 128:(i + 1) * 128, :], [b_H], [h.buf], h.buf)
            for k in range(2):
                IDMA(Hs_d[:, :], rpos.ap[:, i, k:k + 1], h.ap, None, [h.buf, rpos.buf, b_Hs0], [b_Hs], hbsem[i % 2])
        NSUB = TB // 128
        hstm = [AR.alloc("hstm%d" % i, [NSUB, D], BF16) for i in range(2)]
        hsT = [AR.alloc("hsT%d" % i, [KC, TB], BF16) for i in range(2)]
        aT = [AR.alloc("aTs%d" % i, [FFC, TB], BF16) for i in range(2)]
        sil = [AR.alloc("sils%d" % i, [TB]) for i in range(3)]
        DB = min(512, D)
        WB = 128
        WKC = max(KC, (FFC * DB + WB - 1) // WB)
        NWS = 6
        wsl = [AR.alloc("wS%d" % i, [WKC, WB], BF16) for i in range(NWS)]
        ystg = [AR.alloc("ystg%d" % i, [DB]) for i in range(4)]
        identb = AR.alloc("identb", [128], BF16)
        CP(identb.ap, ident.ap, [ident.buf], [identb.buf])
        wi = [0]
        li = [0]
        yi = [0]
        regs = {}

        def wdyn(j, wb2, blk, n, view, slot):
            rowlen = wb2.shape[1]
            v2 = slot.ap.rearrange("p a b -> p (a b)")[:, 0:rowlen]
            IDMA(v2, None, wb2, reid.ap[:, j:j + 1], [reid.buf, b_wb], [slot.buf], slot.buf, eoff=blk * NE * 128 * rowlen)

        for j in range(JMAX):
            hm = hstm[j % 2]
            hT_ = hsT[j % 2]
            a_ = aT[j % 2]
            DMA("sp", hm.ap, Hs_d[j * TB:(j + 1) * TB, :].rearrange("(s p) d -> p s d", p=128), [b_Hs], [hm.buf], hm.buf)
            for st_ in range(NSUB):
                for kg in range(KC // 4):
                    ps = PS()
                    psb = ps.ap.bitcast(BF16)
                    for jj in range(4):
                        kc = kg * 4 + jj
                        TR(ps, psb[:, jj * 128:(jj + 1) * 128], hm.ap[:, st_, kc * 128:(kc + 1) * 128], identb.ap, [hm.buf, identb.buf])
                    src = psb[:, 0:512].rearrange("p (a b) -> p a b", a=4)
                    dst = hT_.ap[:, kg * 4:kg * 4 + 4, st_ * 128:(st_ + 1) * 128]
                    if kg % 2 == 0:
                        CP(dst, src, [ps.buf], [hT_.buf])
                    else:
                        ACT(dst, src, AF.Copy, [ps.buf], [hT_.buf])
            for fc in range(FFC):
                wg = wsl[wi[0] % NWS]
                wi[0] += 1
                wgv = wg.ap.rearrange("p a b -> p (a b)")[:, 0:KC * 128].rearrange("p (a b) -> p a b", a=KC)
                wdyn(j, wb_eg, fc, 128, wgv, wg)
                wu = wsl[wi[0] % NWS]
                wi[0] += 1
                wuv = wu.ap.rearrange("p a b -> p (a b)")[:, 0:KC * 128].rearrange("p (a b) -> p a b", a=KC)
                wdyn(j, wb_eu, fc, 128, wuv, wu)
                psG, psU = PS(), PS()
                for kc in range(KC):
                    MM(psG, psG.ap[:, 0:TB], wgv[:, kc, :], hT_.ap[:, kc, :], kc == 0, kc == KC - 1, [wg.buf, hT_.buf])
                for kc in range(KC):
                    MM(psU, psU.ap[:, 0:TB], wuv[:, kc, :], hT_.ap[:, kc, :], kc == 0, kc == KC - 1, [wu.buf, hT_.buf])
                s_ = sil[li[0] % 3]
                li[0] += 1
                ACT(s_.ap, psG.ap[:, 0:TB], AF.Silu, [psG.buf], [s_.buf])
                TT(a_.ap[:, fc, :], s_.ap, psU.ap[:, 0:TB], ALU.mult, [s_.buf, psU.buf], [a_.buf])
            for cb in range(D // DB):
                wd = wsl[wi[0] % NWS]
                wi[0] += 1
                wdv = wd.ap.rearrange("p a b -> p (a b)")[:, 0:FFC * DB].rearrange("p (a b) -> p a b", a=FFC)
                wdyn(j, wb_ed, cb, DB, wdv, wd)
                pst = [PS() for _ in range(NSUB)]
                for kc in range(FFC):
                    for st_ in range(NSUB):
                        MM(pst[st_], pst[st_].ap[:, 0:DB], a_.ap[:, kc, st_ * 128:(st_ + 1) * 128], wdv[:, kc, :], kc == 0, kc == FFC - 1,
                           [wd.buf, a_.buf])
                for st_ in range(NSUB):
                    ys = ystg[yi[0] % 4]
                    yi[0] += 1
                    if st_ % 2 == 0:
                        ACT(ys.ap, pst[st_].ap[:, 0:DB], AF.Copy, [pst[st_].buf], [ys.buf])
                    else:
                        CP(ys.ap, pst[st_].ap[:, 0:DB], [pst[st_].buf], [ys.buf])
                    r0 = j * TB + st_ * 128
                    DMA("sp", Ys_d[r0:r0 + 128, cb * DB:(cb + 1) * DB], ys.ap, [ys.buf], [b_Ys], ys.buf)
        phase_reset()
        gfb = AR.alloc("gfb", [D])
        DMA("sp", gfb.ap, gf_d, [], [gfb.buf], gfb.buf)
        x1b = [AR.alloc("x1e%d" % i, [D]) for i in range(2)]
        yab = [AR.alloc("ya%d" % i, [D]) for i in range(2)]
        ybb = [AR.alloc("yb%d" % i, [D]) for i in range(2)]
        xo = [AR.alloc("xo%d" % i, [D]) for i in range(2)]
        smalls = [AR.alloc("smallE%d" % i, [4]) for i in range(2)]
        for i in range(NT):
            p, t0 = subt[i]
            x = x1b[i % 2]
            ya, yb, o_ = yab[i % 2], ybb[i % 2], xo[i % 2]
            sm = smalls[i % 2]
            DMA("sp", x.ap, p["x1"][t0:t0 + 128, :], [p["b_x1"]], [x.buf], x.buf)
            IDMA(ya.ap, None, Ys_d[:, :], rpos.ap[:, i, 0:1], [b_Ys, rpos.buf], [ya.buf], ya.buf)
            IDMA(yb.ap, None, Ys_d[:, :], rpos.ap[:, i, 1:2], [b_Ys, rpos.buf], [yb.buf], yb.buf)
            STT(x.ap, ya.ap, rc12.ap[:, i, 0:1], x.ap, ALU.mult, ALU.add, [ya.buf, rc12.buf, x.buf], [x.buf])
            STT(x.ap, yb.ap, rc12.ap[:, i, 1:2], x.ap, ALU.mult, ALU.add, [yb.buf, rc12.buf, x.buf], [x.buf])
            ssq, t1, t2, rstd = sm.ap[:, 0:1], sm.ap[:, 1:2], sm.ap[:, 2:3], sm.ap[:, 3:4]
            ACT(o_.ap, x.ap, AF.Square, [x.buf], [o_.buf, sm.buf], accum=ssq)
            TS(t1, ssq, 1.0 / D, EPS, ALU.mult, ALU.add, [sm.buf], [sm.buf])
            ACT(t2, t1, AF.Sqrt, [sm.buf], [sm.buf])
            P.op("dve", lambda e, o=rstd, i_=t2: e.reciprocal(o, i_), reads=[sm.buf], writes=[sm.buf])
            STT(o_.ap, x.ap, rstd, gfb.ap, ALU.mult, ALU.mult, [x.buf, sm.buf, gfb.buf], [o_.buf])
            DMA("sp", p["y"][t0:t0 + 128, :], o_.ap, [o_.buf], [p["b_y"]], o_.buf)

    phase_A()
    phase_B1()
    phase_B2()
    phase_C1()
    if cfg.get("dense_moe", False):
        phase_C2()
    else:
        phase_C2_sparse()
    P.op("sp", lambda e: e.nop(), reads=[p["b_y"] for p in parts])
    P.emit_all(nc)
    st.close()
    return nc


def _slopes(HPG):
    nh = NGRP * HPG
    s = 2.0 ** (-8.0 * np.arange(1, nh + 1) / nh)
    return s.astype(np.float32).reshape(NGRP, HPG)


def _const_tables(cfg):
    HPG, FGW = cfg["HPG"], cfg["FGW"]
    sl = _slopes(HPG).astype(np.float64)
    a = np.arange(128)[:, None]
    b = np.arange(128)[None, :]
    etA = np.zeros((128, NGRP * HPG, 128), np.float32)
    etB = np.zeros((128, NGRP * HPG, 128), np.float32)
    for g in range(NGRP):
        for h in range(HPG):
            s = np.float64(np.float32(sl[g, h])) * DIL[g]
            ea = np.where(a >= b, np.exp(-s * np.abs(a - b - 64)), 0.0)
            eb = np.where(a <= b, np.exp(-s * np.abs(a - b + 64)), 0.0)
            etA[:, g * HPG + h, :] = ea
            etB[:, g * HPG + h, :] = eb
    etA0 = np.ascontiguousarray(etA[64:128])
    c = np.arange(FGW)
    ang = 2.0 * np.pi * np.outer(c, c) / FGW
    cc = (np.cos(ang) / np.sqrt(FGW)).astype(np.float32)
    scn = (-np.sin(ang) / np.sqrt(FGW)).astype(np.float32)
    return dict(etA=etA, etB=etB, etA0=etA0, cc=cc, scn=scn, ident=np.eye(128, dtype=np.float32))


def _dft_local(S, own, half):
    loc = np.arange(S, dtype=np.int64)
    glob = loc if half == 0 else S - 1 - loc
    prod = np.outer(glob, glob[:own]) % S
    ang = 2.0 * np.pi * prod / S
    return (np.cos(ang) / np.sqrt(S)).astype(np.float32), (np.sin(ang) / np.sqrt(S)).astype(np.float32)


def _relayout_up(w):
    ne, d, dff = w.shape
    kc, ffc = d // 128, dff // 128
    r = w.reshape(ne, kc, 128, ffc, 128).transpose(3, 0, 2, 1, 4)
    return np.ascontiguousarray(r).reshape(ffc, ne * 128, kc * 128)


def _relayout_down(w):
    ne, dff, d = w.shape
    ffc = dff // 128
    db = min(512, d)
    r = w.reshape(ne, ffc, 128, d // db, db).transpose(3, 0, 2, 1, 4)
    return np.ascontiguousarray(r).reshape(d // db, ne * 128, ffc * db)


def _run(cfg, x_prompt, x_sample, attn_norm_g, w_in, w_branch_attn, w_branch_fourier, b_gate, w_out, ffn_norm_g,
         w_router_group, b_router_group, w_router_expert, b_router_expert, w_expert_gate, w_expert_up, w_expert_down,
         final_norm_g, n_cores=8):
    D = cfg["D"]
    KC = D // 128
    f = lambda a: np.ascontiguousarray(np.asarray(a, dtype=np.float32))
    x_prompt, x_sample = f(x_prompt), f(x_sample)
    shared = dict(
        w_in=f(w_in)[0], w_ba=f(w_branch_attn)[0], w_bf=f(w_branch_fourier)[0], w_out=f(w_out)[0],
        w_r=np.ascontiguousarray(np.concatenate(
            [f(w_router_group)[0], f(w_router_expert)[0].transpose(1, 0, 2).reshape(D, NE)], axis=1)),
        w_eg=_relayout_up(f(w_expert_gate)[0]), w_eu=_relayout_up(f(w_expert_up)[0]),
        w_ed=_relayout_down(f(w_expert_down)[0]),
        pidx=np.arange(128, dtype=np.float32).reshape(128, 1),
        g1T=np.ascontiguousarray(f(attn_norm_g)[0].reshape(KC, 128).T),
        g2T=np.ascontiguousarray(f(ffn_norm_g)[0].reshape(KC, 128).T),
        bgT=np.ascontiguousarray(f(b_gate)[0].reshape(2 * KC, 128).T),
        gf_b=np.ascontiguousarray(np.broadcast_to(f(final_norm_g)[None, :], (128, D))),
        br_b=np.ascontiguousarray(np.broadcast_to(
            np.concatenate([f(b_router_group)[0], f(b_router_expert)[0].reshape(NE)])[None, :], (128, NR))),
    )
    shared.update(_const_tables(cfg))
    TB = cfg.get("TB", 256)
    ntok = (cfg["S_S"] + cfg["S_P"]) // 2
    jmax = 2 * ntok // TB + NE
    shared["g2_b"] = np.ascontiguousarray(np.broadcast_to(f(ffn_norm_g)[0][None, :], (128, D)))
    shared["jidx"] = np.ascontiguousarray(np.broadcast_to(np.arange(jmax, dtype=np.float32)[None, :], (128, jmax)))
    shared["tri"] = np.triu(np.ones((128, 128), np.float32))
    dft = {}
    for nm, S in (("s", cfg["S_S"]), ("p", cfg["S_P"])):
        for half in (0, 1):
            dft[(nm, half)] = _dft_local(S, S // 2, half)
    in_maps = []
    for c in range(n_cores):
        b, half = c // 2, c % 2
        m = dict(shared)
        xs = x_sample[b] if half == 0 else x_sample[b, ::-1]
        xp = x_prompt[b] if half == 0 else x_prompt[b, ::-1]
        m["x_s"] = np.ascontiguousarray(xs)
        m["x_p"] = np.ascontiguousarray(xp)
        m["cs_s"], m["ss_s"] = dft[("s", half)]
        m["cs_p"], m["ss_p"] = dft[("p", half)]
        in_maps.append(m)
    nc = build_nc(cfg)
    res = run_bass_kernel_spmd(nc, in_maps, core_ids=list(range(n_cores)))
    nb = n_cores // 2
    y_p = np.zeros((nb, cfg["S_P"], D), np.float32)
    y_s = np.zeros((nb, cfg["S_S"], D), np.float32)
    for c in range(n_cores):
        b, half = c // 2, c % 2
        r = res.results[c]
        for nm, S, dst in (("p", cfg["S_P"], y_p), ("s", cfg["S_S"], y_s)):
            o = np.asarray(r["y_" + nm], dtype=np.float32)
            if half == 0:
                dst[b, :S // 2] = o
            else:
                dst[b, S // 2:] = o[::-1]
    return y_p, y_s


def kernel(**inputs):
    return _run(FULL_CFG, **inputs)
```

```python
import math
from contextlib import ExitStack

import numpy as np
import concourse.bass as bass
import concourse.mybir as mybir
from concourse.bass_utils import run_bass_kernel_spmd

F32 = mybir.dt.float32
BF16 = mybir.dt.bfloat16
I32 = mybir.dt.int32
AF = mybir.ActivationFunctionType
ALU = mybir.AluOpType
AX = mybir.AxisListType

FULL_CFG = dict(D=4096, HPG=8, FGW=512, DFF=1024, S_S=4096, S_P=2048)
NGRP = 3
DIL = (1, 4, 16)
NFG = 4
NEG = 4
EPG = 4
NE = NEG * EPG
NR = NEG + NE
T = 512
EPS = 1e-6
ARENA_WORDS = 48 * 1024


class Buf:
    __slots__ = ("name", "dram", "w", "r", "cnt", "sid")

    def __init__(self, name, dram=False):
        self.name = name
        self.dram = dram
        self.w = {}
        self.r = {}
        self.cnt = 0
        self.sid = None


def _merge(dst, src):
    for k, v in src.items():
        if dst.get(k, -1) < v:
            dst[k] = v


class Op:
    __slots__ = ("emit", "deps", "seq", "dma")


class Prog:
    ENGS = ("pe", "act", "dve", "pool", "sp")

    def __init__(self):
        self.q = {e: [] for e in self.ENGS}
        self.needed = {e: set() for e in self.ENGS}
        self.barrier_deps = {}
        self.slots = []
        self.last_compute = {}

    def op(self, eng, emit, reads=(), writes=(), dma=None):
        o = Op()
        o.emit = emit
        deps = dict(self.barrier_deps)
        for b in reads:
            _merge(deps, b.w)
        for b in writes:
            _merge(deps, b.w)
            _merge(deps, b.r)
        o.seq = len(self.q[eng])
        if dma is not None:
            if dma.sid is None:
                dma.sid = len(self.slots)
                self.slots.append(dma)
            dma.cnt += 1
            key, val = ("dma", dma.sid), dma.cnt * 16
            o.dma = dma.sid
        else:
            key, val = ("eng", eng), o.seq
            o.dma = None
            self.last_compute[eng] = o.seq
        for k, v in deps.items():
            if k[0] == "eng" and not (k[1] == "pe" and eng == "pe"):
                self.needed[k[1]].add(v)
        for b in reads:
            if not b.dram and b.r.get(key, -1) < val:
                b.r[key] = val
        for b in writes:
            if b.dram:
                if b.w.get(key, -1) < val:
                    b.w[key] = val
            elif b.r:
                b.w = {key: val}
                b.r = {}
            else:
                if b.w.get(key, -1) < val:
                    b.w[key] = val
        o.deps = deps
        self.q[eng].append(o)
        return o

    def barrier(self):
        d = {}
        for e, v in self.last_compute.items():
            d[("eng", e)] = v
        for s in self.slots:
            d[("dma", s.sid)] = s.cnt * 16
        self.barrier_deps = d

    def emit_all(self, nc):
        ranks = {}
        for e in self.ENGS:
            ranks[e] = {s: i + 1 for i, s in enumerate(sorted(self.needed[e]))}
        with ExitStack() as st:
            esem = {e: st.enter_context(nc.semaphore("sem_" + e)) for e in self.ENGS}
            dsem = [st.enter_context(nc.semaphore("dsem%d" % i)) for i in range(len(self.slots))]
            block = st.enter_context(nc.Block())

            def run(ename, eng):
                known = {}
                for o in self.q[ename]:
                    for k, v in o.deps.items():
                        if k[0] == "eng":
                            if k[1] == "pe" and ename == "pe":
                                continue
                            sem, val = esem[k[1]], ranks[k[1]][v]
                        else:
                            sem, val = dsem[k[1]], v
                        if known.get(k, -1) >= val:
                            continue
                        eng.wait_ge(sem, val)
                        known[k] = val
                    inst = o.emit(eng)
                    if o.dma is not None:
                        inst.then_inc(dsem[o.dma], 16)
                    elif o.seq in ranks[ename]:
                        inst.then_inc(esem[ename], 1)

            @block.tensor
            def _(e):
                run("pe", e)

            @block.scalar
            def _(e):
                run("act", e)

            @block.vector
            def _(e):
                run("dve", e)

            @block.gpsimd
            def _(e):
                run("pool", e)

            @block.sync
            def _(e):
                run("sp", e)


class Tile:
    __slots__ = ("ap", "buf")

    def __init__(self, ap, buf):
        self.ap = ap
        self.buf = buf


class Arena:
    def __init__(self, ap, nwords):
        self.base = ap
        self.n = nwords
        self.off = 0

    def reset(self):
        self.off = 0

    def alloc(self, name, free_shape, dtype=F32):
        n = 1
        for s in free_shape:
            n *= s
        words = n if dtype in (F32, I32) else (n + 1) // 2
        words = (words + 7) // 8 * 8
        assert self.off + words <= self.n, "SBUF arena overflow at %s: %d + %d > %d" % (name, self.off, words, self.n)
        a = self.base[:, self.off:self.off + words]
        self.off += words
        if dtype != F32:
            a = a.bitcast(dtype)
        a = a[:, 0:n]
        if len(free_shape) == 2:
            a = a.rearrange("p (a b) -> p a b", a=free_shape[0])
        elif len(free_shape) == 3:
            a = a.rearrange("p (a b c) -> p a b c", a=free_shape[0], b=free_shape[1])
        return Tile(a, Buf(name))


def build_nc(cfg):
    D, HPG, FGW, DFF = cfg["D"], cfg["HPG"], cfg["FGW"], cfg["DFF"]
    KC = D // 128
    AW = NGRP * HPG * 128
    GW = HPG * 128
    AO = HPG * 128
    FW = NFG * FGW
    FCG = FGW // 128
    IN_W = 3 * AW + FW + 2 * D
    FFC = DFF // 128
    parts = [dict(name="s", S=cfg["S_S"], own=cfg["S_S"] // 2), dict(name="p", S=cfg["S_P"], own=cfg["S_P"] // 2)]
    for p in parts:
        p["ext"] = p["own"] + 1024
        assert p["ext"] <= p["S"]
    scale = 1.0 / math.sqrt(128.0)

    nc = bass.Bass("TRN2", target_bir_lowering=False)

    def din(name, shape, dt=F32):
        return nc.dram_tensor(name, list(shape), dt, kind="ExternalInput").ap()

    def dscr(name, shape, dt=BF16):
        return nc.dram_tensor(name, list(shape), dt, kind="Internal").ap()

    for p in parts:
        n = p["name"]
        p["x"] = din("x_" + n, [p["S"], D])
        p["cs"] = din("cs_" + n, [p["S"], p["own"]])
        p["ss"] = din("ss_" + n, [p["S"], p["own"]])
        p["y"] = nc.dram_tensor("y_" + n, [p["own"], D], F32, kind="ExternalOutput").ap()
        p["qT"] = dscr("qT_" + n, [AW, p["own"]])
        p["kT"] = dscr("kT_" + n, [AW, p["ext"]])
        p["v"] = dscr("v_" + n, [p["ext"], AW])
        p["f"] = dscr("f_" + n, [p["S"], FW])
        p["oT"] = dscr("oT_" + n, [AO, p["own"]])
        p["frT"] = dscr("frT_" + n, [FW, p["own"]])
        p["x1"] = dscr("x1_" + n, [p["own"], D], F32)
        for k in ("qT", "kT", "v", "f", "oT", "frT", "x1", "y"):
            p["b_" + k] = Buf(k + "_" + n, dram=True)
    w_in = din("w_in", [D, IN_W])
    w_ba = din("w_ba", [AO, D])
    w_bf = din("w_bf", [FW, D])
    w_out = din("w_out", [D, D])
    w_r = din("w_r", [D, NR])
    DBX = min(512, D)
    w_eg = din("w_eg", [FFC, NE * 128, KC * 128])
    w_eu = din("w_eu", [FFC, NE * 128, KC * 128])
    w_ed = din("w_ed", [D // DBX, NE * 128, FFC * DBX])
    pidx_d = din("pidx", [128, 1])
    wb_eg = dscr("wb_eg", [FFC * NE * 128, KC * 128])
    wb_eu = dscr("wb_eu", [FFC * NE * 128, KC * 128])
    wb_ed = dscr("wb_ed", [(D // DBX) * NE * 128, FFC * DBX])
    b_wb = Buf("wb", dram=True)
    conv_jobs = []
    for (src3, dst2) in ((w_eg, wb_eg), (w_eu, wb_eu), (w_ed, wb_ed)):
        src2 = src3.rearrange("f r n -> (f r) n")
        for r0 in range(0, dst2.shape[0], 128):
            conv_jobs.append((src2, dst2, r0))
    conv_state = dict(i=0, slots=None)

    def emit_conv(n):
        for _ in range(n):
            if conv_state["i"] >= len(conv_jobs):
                return
            src2, dst2, r0 = conv_jobs[conv_state["i"]]
            sem = conv_state["ssem"][conv_state["i"] % len(conv_state["ssem"])]
            conv_state["i"] += 1
            DMA("pool", dst2[r0:r0 + 128, :], src2[r0:r0 + 128, :], [], [b_wb], sem)
    g1T_d = din("g1T", [128, KC])
    g2T_d = din("g2T", [128, KC])
    bgT_d = din("bgT", [128, 2 * KC])
    gf_d = din("gf_b", [128, D])
    br_d = din("br_b", [128, NR])
    ident_d = din("ident", [128, 128])
    cc_d = din("cc", [FGW, FGW])
    sc_d = din("scn", [FGW, FGW])
    ea_d = din("etA", [128, NGRP * HPG, 128])
    eb_d = din("etB", [128, NGRP * HPG, 128])
    ea0_d = din("etA0", [64, NGRP * HPG, 128])
    TB = cfg.get("TB", 256)
    NTOK = sum(p["own"] for p in parts)
    NT = NTOK // 128
    JMAX = 2 * NTOK // TB + NE
    NS = JMAX * TB
    g2b_d = din("g2_b", [128, D])
    jidx_d = din("jidx", [128, JMAX])
    tri_d = din("tri", [128, 128])
    H_d = dscr("H_scr", [NTOK, D])
    Hs_d = dscr("Hs_scr", [NS, D])
    Ys_d = dscr("Ys_scr", [NS, D], F32)
    b_H, b_Hs, b_Ys = Buf("H", dram=True), Buf("Hs", dram=True), Buf("Ys", dram=True)

    P = Prog()
    st = ExitStack()
    arena_t = st.enter_context(nc.sbuf_tensor("arena", [128, ARENA_WORDS], F32))
    AR = Arena(arena_t[:], ARENA_WORDS)
    pss = []
    for i in range(8):
        pt = st.enter_context(nc.psum_tensor("ps%d" % i, [128, 512], F32))
        pss.append(Tile(pt[:], Buf("ps%d" % i)))
    psi = [0]

    def PS():
        t = pss[psi[0] % 8]
        psi[0] += 1
        return t

    def MM(ps, out, lhsT, rhs, start, stop, reads):
        P.op("pe", lambda e: e.matmul(out, lhsT, rhs, start=start, stop=stop), reads=reads, writes=[ps.buf])

    def TR(ps, out, in_, ident, reads):
        P.op("pe", lambda e: e.transpose(out, in_, ident), reads=reads, writes=[ps.buf])

    def DMA(q, out, in_, reads, writes, slot):
        P.op(q, lambda e: e.dma_start(out=out, in_=in_), reads=reads, writes=writes, dma=slot)

    def ACT(out, in_, func, reads, writes, bias=None, scale=None, accum=None):
        kw = {}
        if bias is not None:
            kw["bias"] = bias
        if scale is not None:
            kw["scale"] = scale
        if accum is not None:
            kw["accum_out"] = accum
        P.op("act", lambda e: e.activation(out, in_, func, **kw), reads=reads, writes=writes)

    def TT(out, in0, in1, op, reads, writes, eng="dve"):
        P.op(eng, lambda e: e.tensor_tensor(out, in0, in1, op), reads=reads, writes=writes)

    def TS(out, in0, s1, s2, op0, op1, reads, writes, eng="dve"):
        if op1 is None:
            P.op(eng, lambda e: e.tensor_scalar(out, in0, s1, None, op0), reads=reads, writes=writes)
        else:
            P.op(eng, lambda e: e.tensor_scalar(out, in0, s1, s2, op0, op1), reads=reads, writes=writes)

    def STT(out, in0, scalar, in1, op0, op1, reads, writes, eng="dve"):
        P.op(eng, lambda e: e.scalar_tensor_tensor(out, in0, scalar, in1, op0, op1), reads=reads, writes=writes)

    def CP(out, in_, reads, writes, eng="dve"):
        P.op(eng, lambda e: e.tensor_copy(out, in_), reads=reads, writes=writes)

    def wblock(slot, src2d, kchunks, ncols):
        v = slot.ap[:, 0:kchunks, 0:ncols]
        DMA("pool", v, src2d.rearrange("(kc p) n -> p kc n", p=128), [], [slot.buf], slot.buf)
        return v

    def rms_to_T(xt, dstT, col0, gT, junk, small, xs):
        ssq = small.ap[:, 0:1]
        t1 = small.ap[:, 1:2]
        t2 = small.ap[:, 2:3]
        rstd = small.ap[:, 3:4]
        ACT(junk.ap, xt.ap, AF.Square, [xt.buf], [junk.buf, small.buf], accum=ssq)
        TS(t1, ssq, 1.0 / D, EPS, ALU.mult, ALU.add, [small.buf], [small.buf])
        ACT(t2, t1, AF.Sqrt, [small.buf], [small.buf])
        P.op("dve", lambda e: e.reciprocal(rstd, t2), reads=[small.buf], writes=[small.buf])
        TS(xs.ap, xt.ap, rstd, None, ALU.mult, None, [xt.buf, small.buf], [xs.buf])
        for kg in range(KC // 4):
            ps = PS()
            for j in range(4):
                kc = kg * 4 + j
                TR(ps, ps.ap[:, j * 128:(j + 1) * 128], xs.ap[:, kc * 128:(kc + 1) * 128], ident.ap, [xs.buf, ident.buf])
            gb = gT.ap[:, kg * 4:kg * 4 + 4].unsqueeze(2).to_broadcast([128, 4, 128])
            TT(dstT.ap[:, kg * 4:kg * 4 + 4, col0:col0 + 128], ps.ap.rearrange("p (a b) -> p a b", a=4), gb, ALU.mult,
               [ps.buf, gT.buf], [dstT.buf])

    ident = AR.alloc("ident", [128])
    g1T = AR.alloc("g1T", [KC])
    g2T = AR.alloc("g2T", [KC])
    DMA("sp", ident.ap, ident_d, [], [ident.buf], ident.buf)
    DMA("sp", g1T.ap, g1T_d, [], [g1T.buf], g1T.buf)
    DMA("sp", g2T.ap, g2T_d, [], [g2T.buf], g2T.buf)
    rm1 = AR.alloc("rm1", [NT, NE])
    rm2 = AR.alloc("rm2", [NT, NE])
    rc12 = AR.alloc("rc12", [NT, 2])
    rpos = AR.alloc("rpos", [NT, 2], I32)
    reid = AR.alloc("reid", [JMAX], I32)
    const_off = AR.off

    def phase_reset():
        P.barrier()
        AR.off = const_off

    def phase_A():
        phase_reset()
        hT = AR.alloc("hT", [KC, T], BF16)
        xts = [AR.alloc("xtA%d" % i, [D]) for i in range(2)]
        xs = AR.alloc("xsA", [D])
        junk = AR.alloc("junkA", [D], BF16)
        smalls = [AR.alloc("smallA%d" % i, [4]) for i in range(2)]
        wsl = [AR.alloc("wA%d" % i, [KC, 512], BF16) for i in range(2)]
        stg = [AR.alloc("stgA%d" % i, [512], BF16) for i in range(4)]
        wi = [0]
        si = [0]
        xi = [0]
        for p in parts:
            ntile = p["S"] // T
            for tt in range(ntile):
                t0 = tt * T
                if t0 < p["own"]:
                    kind = "own"
                elif t0 < p["ext"]:
                    kind = "halo"
                else:
                    kind = "far"
                for s4 in range(4):
                    xt = xts[xi[0] % 2]
                    sm = smalls[xi[0] % 2]
                    xi[0] += 1
                    DMA("sp", xt.ap, p["x"][t0 + s4 * 128:t0 + (s4 + 1) * 128, :], [], [xt.buf], xt.buf)
                    rms_to_T(xt, hT, s4 * 128, g1T, junk, sm, xs)
                secs = []
                if kind == "own":
                    secs.append((0, AW, "fm", p["qT"], p["b_qT"], 0))
                    secs.append((AW, AW, "fm", p["kT"], p["b_kT"], 0))
                    secs.append((2 * AW, AW, "tm", p["v"], p["b_v"], 0))
                elif kind == "halo":
                    g_lo = 0 if t0 < p["own"] + T else 2
                    secs.append((AW + g_lo * GW, AW - g_lo * GW, "fm", p["kT"], p["b_kT"], g_lo * GW))
                    secs.append((2 * AW + g_lo * GW, AW - g_lo * GW, "tm", p["v"], p["b_v"], g_lo * GW))
                secs.append((3 * AW, FW, "tm", p["f"], p["b_f"], 0))
                for (c0, ncols, mode, dst, dstb, dc0) in secs:
                    bw = 512 if ncols % 512 == 0 else 128
                    for blk in range(ncols // bw):
                        ws = wsl[wi[0] % 2]
                        wi[0] += 1
                        wv = wblock(ws, w_in[:, c0 + blk * bw:c0 + (blk + 1) * bw], KC, bw)
                        if mode == "fm":
                            for j in range(bw // 128):
                                ps = PS()
                                for kc in range(KC):
                                    MM(ps, ps.ap, wv[:, kc, j * 128:(j + 1) * 128], hT.ap[:, kc, :], kc == 0, kc == KC - 1,
                                       [ws.buf, hT.buf])
                                sg = stg[si[0] % 4]
                                si[0] += 1
                                ACT(sg.ap, ps.ap, AF.Copy, [ps.buf], [sg.buf])
                                r0 = dc0 + blk * bw + j * 128
                                DMA("sp", dst[r0:r0 + 128, t0:t0 + T], sg.ap, [sg.buf], [dstb], sg.buf)
                        else:
                            pst = [PS() for _ in range(4)]
                            for kc in range(KC):
                                for s4 in range(4):
                                    MM(pst[s4], pst[s4].ap[:, 0:bw], hT.ap[:, kc, s4 * 128:(s4 + 1) * 128], wv[:, kc, :],
                                       kc == 0, kc == KC - 1, [ws.buf, hT.buf])
                            for s4 in range(4):
                                sg = stg[si[0] % 4]
                                si[0] += 1
                                if s4 % 2 == 0:
                                    ACT(sg.ap[:, 0:bw], pst[s4].ap[:, 0:bw], AF.Copy, [pst[s4].buf], [sg.buf])
                                else:
                                    CP(sg.ap[:, 0:bw], pst[s4].ap[:, 0:bw], [pst[s4].buf], [sg.buf])
                                cc0 = dc0 + blk * bw
                                DMA("sp", dst[t0 + s4 * 128:t0 + (s4 + 1) * 128, cc0:cc0 + bw], sg.ap[:, 0:bw], [sg.buf], [dstb],
                                    sg.buf)

    def phase_B1():
        phase_reset()
        etA = AR.alloc("etA", [NGRP * HPG, 128])
        etB = AR.alloc("etB", [NGRP * HPG, 128])
        etA0 = AR.alloc("etA0", [NGRP * HPG, 128])
        ones = AR.alloc("ones", [128], BF16)
        DMA("sp", etA.ap, ea_d, [], [etA.buf], etA.buf)
        DMA("sp", etB.ap, eb_d, [], [etB.buf], etB.buf)
        DMA("sp", etA0.ap[0:64], ea0_d, [], [etA0.buf], etA0.buf)
        P.op("dve", lambda e: e.memset(ones.ap, 1.0), writes=[ones.buf])
        maxown = max(p["own"] for p in parts)
        ksz = [max(p["own"] + 64 * DIL[g] for p in parts) for g in range(NGRP)]
        vsz = [max((max(1, p["own"] // DIL[g] // 128) + 1) * DIL[g] * 128 for p in parts) for g in range(NGRP)]
        qs = [[AR.alloc("q%d_%d" % (i, g), [maxown], BF16) for g in range(NGRP)] for i in range(2)]
        ks = [[AR.alloc("k%d_%d" % (i, g), [ksz[g]], BF16) for g in range(NGRP)] for i in range(2)]
        vs = [[AR.alloc("v%d_%d" % (i, g), [vsz[g]], BF16) for g in range(NGRP)] for i in range(2)]
        oacc = [AR.alloc("oacc%d" % i, [maxown]) for i in range(2)]
        dacc = [AR.alloc("dacc%d" % i, [maxown]) for i in range(2)]
        ost = [AR.alloc("ost%d" % i, [maxown], BF16) for i in range(2)]
        pfs = [AR.alloc("pf%d" % i, [128]) for i in range(4)]
        pbs = [AR.alloc("pb%d" % i, [128], BF16) for i in range(4)]
        hi = [0]
        bi = [0]
        conv_state["ssem"] = [Buf("cvs%d" % i) for i in range(4)]
        per_head = (len(conv_jobs) // 2 + 2 * HPG - 1) // (2 * HPG)
        for p in parts:
            own, ext = p["own"], p["ext"]
            for h in range(HPG):
                if not cfg.get("dense_moe", False):
                    emit_conv(per_head)
                par = hi[0] % 2
                hi[0] += 1
                oa, da, os_ = oacc[par], dacc[par], ost[par]
                for g in range(NGRP):
                    d = DIL[g]
                    Lo = own // d
                    eg = own + 64 * d
                    row0 = (g * HPG + h) * 128
                    qt, kt, vt = qs[par][g], ks[par][g], vs[par][g]
                    DMA("sp", qt.ap[:, 0:own], p["qT"][row0:row0 + 128, 0:own], [p["b_qT"]], [qt.buf], qt.buf)
                    DMA("sp", kt.ap[:, 0:eg], p["kT"][row0:row0 + 128, 0:eg], [p["b_kT"]], [kt.buf], kt.buf)
                    nq = max(1, Lo // 128)
                    Q = min(128, Lo)
                    vcol = slice(row0, row0 + 128)
                    MT = nq + 1
                    vv = vt.ap[:, 0:MT * d * 128].rearrange("p (m r c) -> p m r c", m=MT, r=d)
                    DMA("sp", vv[0:64, 0], p["v"][0:64 * d, vcol].rearrange("(p r) c -> p r c", r=d), [p["b_v"]], [vt.buf], vt.buf)
                    if Lo >= 128:
                        for m in range(1, MT):
                            tok0 = (128 * m - 64) * d
                            DMA("sp", vv[:, m], p["v"][tok0:tok0 + 128 * d, vcol].rearrange("(p r) c -> p r c", r=d), [p["b_v"]],
                                [vt.buf], vt.buf)
                    else:
                        DMA("sp", vv[0:Q, 1], p["v"][64 * d:(64 + Q) * d, vcol].rearrange("(p r) c -> p r c", r=d), [p["b_v"]],
                            [vt.buf], vt.buf)
                    eidx = g * HPG + h
                    for r in range(d):
                        for iq in range(nq):
                            i0 = iq * 128
                            qv = qt.ap[:, i0 * d + r:(i0 + Q) * d:d]
                            psO = PS()
                            psD = PS()
                            tiles = []
                            if iq == 0:
                                tiles.append((0, 64, 0, etA0.ap[0:64, eidx, 0:Q], etA0.buf))
                            else:
                                tiles.append((i0 - 64, 128, iq, etA.ap[:, eidx, 0:Q], etA.buf))
                            KB = Q
                            tiles.append((i0 + 64, KB, iq + 1, etB.ap[0:KB, eidx, 0:Q], etB.buf))
                            for ti, (lo, K, m, E, Eb) in enumerate(tiles):
                                kv = kt.ap[:, lo * d + r:(lo + K) * d:d]
                                psS = PS()
                                MM(psS, psS.ap[0:K, 0:Q], kv, qv, True, True, [kt.buf, qt.buf])
                                pf = pfs[bi[0] % 4]
                                pb = pbs[bi[0] % 4]
                                bi[0] += 1
                                ACT(pf.ap[0:K, 0:Q], psS.ap[0:K, 0:Q], AF.Exp, [psS.buf], [pf.buf], scale=scale)
                                TT(pb.ap[0:K, 0:Q], pf.ap[0:K, 0:Q], E, ALU.mult, [pf.buf, Eb], [pb.buf])
                                MM(psO, psO.ap[:, 0:Q], vv[0:K, m, r, :], pb.ap[0:K, 0:Q], ti == 0, ti == 1, [vt.buf, pb.buf])
                                MM(psD, psD.ap[:, 0:Q], ones.ap[0:K, :], pb.ap[0:K, 0:Q], ti == 0, ti == 1, [ones.buf, pb.buf])
                            ov = oa.ap[:, i0 * d + r:(i0 + Q) * d:d]
                            dv = da.ap[:, i0 * d + r:(i0 + Q) * d:d]
                            if g == 0:
                                ACT(ov, psO.ap[:, 0:Q], AF.Copy, [psO.buf], [oa.buf])
                                CP(dv, psD.ap[:, 0:Q], [psD.buf], [da.buf])
                            else:
                                TT(ov, ov, psO.ap[:, 0:Q], ALU.add, [psO.buf, oa.buf], [oa.buf])
                                TT(dv, dv, psD.ap[:, 0:Q], ALU.add, [psD.buf, da.buf], [da.buf])
                P.op("dve", lambda e, a=da.ap[:, 0:own]: e.reciprocal(a, a), reads=[da.buf], writes=[da.buf])
                TT(os_.ap[:, 0:own], oa.ap[:, 0:own], da.ap[:, 0:own], ALU.mult, [oa.buf, da.buf], [os_.buf], eng="pool")
                DMA("sp", p["oT"][h * 128:(h + 1) * 128, 0:own], os_.ap[:, 0:own], [os_.buf], [p["b_oT"]], os_.buf)

    def phase_B2():
        phase_reset()
        ccs = AR.alloc("ccs", [FCG, FGW], BF16)
        scs = AR.alloc("scs", [FCG, FGW], BF16)
        wblock(ccs, cc_d, FCG, FGW)
        wblock(scs, sc_d, FCG, FGW)
        maxS = max(p["S"] for p in parts)
        csb = AR.alloc("csb", [maxS // 128, T], BF16)
        ssb = AR.alloc("ssb", [maxS // 128, T], BF16)
        xf = [AR.alloc("xf%d" % i, [maxS // 128, FGW], BF16) for i in range(2)]
        abT = [AR.alloc("abT%d" % i, [2 * FCG, T], BF16) for i in range(2)]
        ystg = [AR.alloc("ystg%d" % i, [T], BF16) for i in range(4)]
        xi = [0]
        yi = [0]
        n_it = sum(p["own"] // T for p in parts) * NFG
        per_it = (len(conv_jobs) - conv_state["i"] + n_it - 1) // n_it
        for p in parts:
            NC_ = p["S"] // 128
            for kb in range(p["own"] // T):
                k0 = kb * T
                csv = wblock(csb, p["cs"][:, k0:k0 + T], NC_, T)
                ssv = wblock(ssb, p["ss"][:, k0:k0 + T], NC_, T)
                for fg in range(NFG):
                    if not cfg.get("dense_moe", False):
                        emit_conv(per_it)
                    x_ = xf[xi[0] % 2]
                    ab = abT[xi[0] % 2]
                    xi[0] += 1
                    xv = x_.ap[:, 0:NC_, :]
                    DMA("sp", xv, p["f"][:, fg * FGW:(fg + 1) * FGW].rearrange("(n p) c -> p n c", p=128), [p["b_f"]], [x_.buf], x_.buf)
                    for fc in range(FCG):
                        for which, mat, matb in ((0, csv, csb.buf), (1, ssv, ssb.buf)):
                            ps = PS()
                            for n in range(NC_):
                                MM(ps, ps.ap, xv[:, n, fc * 128:(fc + 1) * 128], mat[:, n, :], n == 0, n == NC_ - 1, [x_.buf, matb])
                            if which == 0:
                                ACT(ab.ap[:, fc, :], ps.ap, AF.Copy, [ps.buf], [ab.buf])
                            else:
                                CP(ab.ap[:, FCG + fc, :], ps.ap, [ps.buf], [ab.buf])
                    for oc in range(FCG):
                        ps = PS()
                        for c in range(2 * FCG):
                            m_ = ccs if c < FCG else scs
                            MM(ps, ps.ap, m_.ap[:, c % FCG, oc * 128:(oc + 1) * 128], ab.ap[:, c, :], c == 0, c == 2 * FCG - 1,
                               [m_.buf, ab.buf])
                        ys = ystg[yi[0] % 4]
                        yi[0] += 1
                        ACT(ys.ap, ps.ap, AF.Copy, [ps.buf], [ys.buf])
                        r0 = fg * FGW + oc * 128
                        DMA("sp", p["frT"][r0:r0 + 128, k0:k0 + T], ys.ap, [ys.buf], [p["b_frT"]], ys.buf)

    def phase_C1():
        phase_reset()
        bgT = AR.alloc("bgT", [2 * KC])
        DMA("sp", bgT.ap, bgT_d, [], [bgT.buf], bgT.buf)
        hT = AR.alloc("hTc", [KC, T], BF16)
        mT = AR.alloc("mTc", [KC, T], BF16)
        oTt = AR.alloc("oTt", [AO // 128, T], BF16)
        frt = AR.alloc("frt", [FW // 128, T], BF16)
        xts = [AR.alloc("xtC%d" % i, [D]) for i in range(1)]
        junk = AR.alloc("junkC", [D], BF16)
        smalls = [AR.alloc("smallC%d" % i, [4]) for i in range(2)]
        WB = 256
        WKC = max(KC, AO // 128 + FW // 128)
        wsl = [AR.alloc("wC%d" % i, [WKC, WB], BF16) for i in range(3)]
        gts = [AR.alloc("gt%d" % i, [T]) for i in range(4)]
        tmp = [AR.alloc("tmpC%d" % i, [T]) for i in range(4)]
        xr = [AR.alloc("xr%d" % i, [WB]) for i in range(4)]
        wi = [0]
        gi = [0]
        xi = [0]
        ri = [0]

        def nextw():
            w = wsl[wi[0] % 3]
            wi[0] += 1
            return w

        NBR = AO // 128 + FW // 128
        NCB = D // WB
        wc_ga = dscr("wc_ga", [NCB * 128, KC * WB])
        wc_gf = dscr("wc_gf", [NCB * 128, KC * WB])
        wc_br = dscr("wc_br", [NCB * 128, NBR * WB])
        wc_wo = dscr("wc_wo", [NCB * 128, KC * WB])
        b_wc = {k: Buf("wc_" + k, dram=True) for k in ("ga", "gf", "br", "wo")}

        def cache_store(kind, wc, cb, slot, nch):
            DMA("sp", wc[cb * 128:(cb + 1) * 128, :], slot.ap[:, 0:nch, :].rearrange("p a b -> p (a b)"), [slot.buf], [b_wc[kind]],
                slot.buf)

        def cache_load(kind, wc, cb, slot, nch):
            v = slot.ap[:, 0:nch, :]
            DMA("pool", v, wc[cb * 128:(cb + 1) * 128, :].rearrange("p (a b) -> p a b", b=WB), [b_wc[kind]], [slot.buf], slot.buf)
            return v

        first = [True]
        for p in parts:
            for tt in range(p["own"] // T):
                t0 = tt * T
                fill = first[0]
                first[0] = False
                DMA("sp", oTt.ap, p["oT"][:, t0:t0 + T].rearrange("(c p) t -> p c t", p=128), [p["b_oT"]], [oTt.buf], oTt.buf)
                DMA("sp", frt.ap, p["frT"][:, t0:t0 + T].rearrange("(c p) t -> p c t", p=128), [p["b_frT"]], [frt.buf], frt.buf)
                for s4 in range(4):
                    xt = xts[0]
                    sm = smalls[xi[0] % 2]
                    xi[0] += 1
                    DMA("sp", xt.ap, p["x"][t0 + s4 * 128:t0 + (s4 + 1) * 128, :], [], [xt.buf], xt.buf)
                    rms_to_T(xt, hT, s4 * 128, g1T, junk, sm, xt)
                for cb in range(D // WB):
                    wga = nextw()
                    wgf = nextw()
                    wbr = nextw()
                    wbav = wbr.ap[:, 0:AO // 128, 0:WB]
                    wbfv = wbr.ap[:, AO // 128:AO // 128 + FW // 128, 0:WB]
                    if fill:
                        wgav = wblock(wga, w_in[:, 3 * AW + FW + cb * WB:3 * AW + FW + (cb + 1) * WB], KC, WB)
                        wgfv = wblock(wgf, w_in[:, 3 * AW + FW + D + cb * WB:3 * AW + FW + D + (cb + 1) * WB], KC, WB)
                        DMA("pool", wbav, w_ba[:, cb * WB:(cb + 1) * WB].rearrange("(kc p) n -> p kc n", p=128), [], [wbr.buf], wbr.buf)
                        DMA("pool", wbfv, w_bf[:, cb * WB:(cb + 1) * WB].rearrange("(kc p) n -> p kc n", p=128), [], [wbr.buf], wbr.buf)
                    else:
                        wgav = cache_load("ga", wc_ga, cb, wga, KC)
                        wgfv = cache_load("gf", wc_gf, cb, wgf, KC)
                        cache_load("br", wc_br, cb, wbr, NBR)
                    for j in range(WB // 128):
                        c = cb * (WB // 128) + j
                        cs_ = slice(j * 128, (j + 1) * 128)
                        psA, psF, psa, psf = PS(), PS(), PS(), PS()
                        for kc in range(KC):
                            MM(psA, psA.ap, wgav[:, kc, cs_], hT.ap[:, kc, :], kc == 0, kc == KC - 1, [wga.buf, hT.buf])
                        for kc in range(KC):
                            MM(psF, psF.ap, wgfv[:, kc, cs_], hT.ap[:, kc, :], kc == 0, kc == KC - 1, [wgf.buf, hT.buf])
                        na = AO // 128
                        for kc in range(na):
                            MM(psa, psa.ap, wbav[:, kc, cs_], oTt.ap[:, kc, :], kc == 0, kc == na - 1, [wbr.buf, oTt.buf])
                        nf = FW // 128
                        for kc in range(nf):
                            MM(psf, psf.ap, wbfv[:, kc, cs_], frt.ap[:, kc, :], kc == 0, kc == nf - 1, [wbr.buf, frt.buf])
                        gA = gts[gi[0] % 4]
                        gF = gts[(gi[0] + 1) % 4]
                        t1 = tmp[gi[0] % 4]
                        t2 = tmp[(gi[0] + 1) % 4]
                        gi[0] += 2
                        ACT(gA.ap, psA.ap, AF.Sigmoid, [psA.buf, bgT.buf], [gA.buf], bias=bgT.ap[:, c:c + 1])
                        ACT(gF.ap, psF.ap, AF.Sigmoid, [psF.buf, bgT.buf], [gF.buf], bias=bgT.ap[:, KC + c:KC + c + 1])
                        TT(t1.ap, gA.ap, psa.ap, ALU.mult, [gA.buf, psa.buf], [t1.buf])
                        TT(t2.ap, gF.ap, psf.ap, ALU.mult, [gF.buf, psf.buf], [t2.buf])
                        TT(mT.ap[:, c, :], t1.ap, t2.ap, ALU.add, [t1.buf, t2.buf], [mT.buf], eng="pool")
                    if fill:
                        cache_store("ga", wc_ga, cb, wga, KC)
                        cache_store("gf", wc_gf, cb, wgf, KC)
                        cache_store("br", wc_br, cb, wbr, NBR)
                for cb in range(D // WB):
                    ws = nextw()
                    if fill:
                        wv = wblock(ws, w_out[:, cb * WB:(cb + 1) * WB], KC, WB)
                    else:
                        wv = cache_load("wo", wc_wo, cb, ws, KC)
                    pst = [PS() for _ in range(4)]
                    for kc in range(KC):
                        for s4 in range(4):
                            MM(pst[s4], pst[s4].ap[:, 0:WB], mT.ap[:, kc, s4 * 128:(s4 + 1) * 128], wv[:, kc, :], kc == 0, kc == KC - 1,
                               [ws.buf, mT.buf])
                    for s4 in range(4):
                        x_ = xr[ri[0] % 4]
                        ri[0] += 1
                        rows = slice(t0 + s4 * 128, t0 + (s4 + 1) * 128)
                        DMA("sp", x_.ap, p["x"][rows, cb * WB:(cb + 1) * WB], [], [x_.buf], x_.buf)
                        TT(x_.ap, x_.ap, pst[s4].ap[:, 0:WB], ALU.add, [x_.buf, pst[s4].buf], [x_.buf])
                        DMA("sp", p["x1"][rows, cb * WB:(cb + 1) * WB], x_.ap, [x_.buf], [p["b_x1"]], x_.buf)
                    if fill:
                        cache_store("wo", wc_wo, cb, ws, KC)

    def phase_C2():
        phase_reset()
        gfb = AR.alloc("gfb", [D])
        brb = AR.alloc("brb", [NR])
        wrs = AR.alloc("wrs", [KC, NR], BF16)
        DMA("sp", gfb.ap, gf_d, [], [gfb.buf], gfb.buf)
        DMA("sp", brb.ap, br_d, [], [brb.buf], brb.buf)
        wblock(wrs, w_r, KC, NR)
        xt = AR.alloc("x1t", [4, D])
        hT = AR.alloc("h2T", [KC, T], BF16)
        xs = AR.alloc("xsD", [D])
        junk = xs
        smalls = [AR.alloc("smallD%d" % i, [4]) for i in range(2)]
        comb = AR.alloc("comb", [4, NE])
        rt = AR.alloc("rt", [4, 64])
        WB = 128
        DB = min(512, D)
        WKC = max(KC, (FFC * DB + WB - 1) // WB)
        NWS = 4
        wsl = [AR.alloc("wD%d" % i, [WKC, WB], BF16) for i in range(NWS)]
        aT = [AR.alloc("aT%d" % i, [FFC, T], BF16) for i in range(2)]
        sil = [AR.alloc("sil%d" % i, [T]) for i in range(3)]
        wi = [0]
        ai = [0]
        li = [0]
        xi = [0]

        def nextw():
            w = wsl[wi[0] % NWS]
            wi[0] += 1
            return w

        for p in parts:
            for tt in range(p["own"] // T):
                t0 = tt * T
                for s4 in range(4):
                    xv = Tile(xt.ap[:, s4, :], xt.buf)
                    sm = smalls[xi[0] % 2]
                    xi[0] += 1
                    DMA("sp", xv.ap, p["x1"][t0 + s4 * 128:t0 + (s4 + 1) * 128, :], [p["b_x1"]], [xt.buf], xt.buf)
                    rms_to_T(xv, hT, s4 * 128, g2T, junk, sm, xs)
                for s4 in range(4):
                    ps = PS()
                    for kc in range(KC):
                        MM(ps, ps.ap[:, 0:NR], hT.ap[:, kc, s4 * 128:(s4 + 1) * 128], wrs.ap[:, kc, :], kc == 0, kc == KC - 1,
                           [hT.buf, wrs.buf])
                    R = rt.ap[:, s4, :]
                    rb = [rt.buf]
                    lg = R[:, 0:NR]
                    TT(lg, ps.ap[:, 0:NR], brb.ap, ALU.add, [ps.buf, brb.buf], rb)
                    gmax = R[:, 20:21]
                    P.op("dve", lambda e, o=gmax, i=lg[:, 0:NEG]: e.reduce_max(o, i, AX.X), reads=rb, writes=rb)
                    oh = R[:, 21:25]
                    TS(oh, lg[:, 0:NEG], gmax, None, ALU.is_equal, None, rb, rb)
                    ngm = R[:, 25:26]
                    TS(ngm, gmax, -1.0, None, ALU.mult, None, rb, rb)
                    eg_ = R[:, 26:30]
                    sg_ = R[:, 30:31]
                    ACT(eg_, lg[:, 0:NEG], AF.Exp, rb, rb, bias=ngm, accum=sg_)
                    pg = R[:, 31:32]
                    P.op("dve", lambda e, o=pg, i=sg_: e.reciprocal(o, i), reads=rb, writes=rb)
                    les = R[:, 32:36]
                    TS(les, lg[:, NEG:NEG + EPG], oh[:, 0:1], None, ALU.mult, None, rb, rb)
                    for g in range(1, NEG):
                        STT(les, lg[:, NEG + g * EPG:NEG + (g + 1) * EPG], oh[:, g:g + 1], les, ALU.mult, ALU.add, rb, rb)
                    m1 = R[:, 36:37]
                    P.op("dve", lambda e, o=m1, i=les: e.reduce_max(o, i, AX.X), reads=rb, writes=rb)
                    k1 = R[:, 37:41]
                    TS(k1, les, m1, None, ALU.is_equal, None, rb, rb)
                    le2 = R[:, 41:45]
                    STT(le2, k1, -1e30, les, ALU.mult, ALU.add, rb, rb)
                    m2 = R[:, 45:46]
                    P.op("dve", lambda e, o=m2, i=le2: e.reduce_max(o, i, AX.X), reads=rb, writes=rb)
                    k2 = R[:, 46:50]
                    TS(k2, le2, m2, None, ALU.is_equal, None, rb, rb)
                    dm = R[:, 50:51]
                    TT(dm, m2, m1, ALU.subtract, rb, rb)
                    ex = R[:, 51:52]
                    ACT(ex, dm, AF.Exp, rb, rb)
                    den = R[:, 52:53]
                    TS(den, ex, 1.0, None, ALU.add, None, rb, rb)
                    p1 = R[:, 53:54]
                    P.op("dve", lambda e, o=p1, i=den: e.reciprocal(o, i), reads=rb, writes=rb)
                    p2 = R[:, 54:55]
                    TT(p2, ex, p1, ALU.mult, rb, rb)
                    TT(p1, p1, pg, ALU.mult, rb, rb)
                    TT(p2, p2, pg, ALU.mult, rb, rb)
                    cw = R[:, 55:59]
                    TS(cw, k1, p1, None, ALU.mult, None, rb, rb)
                    STT(cw, k2, p2, cw, ALU.mult, ALU.add, rb, rb)
                    for g in range(NEG):
                        TS(comb.ap[:, s4, g * EPG:(g + 1) * EPG], cw, oh[:, g:g + 1], None, ALU.mult, None, rb, [comb.buf])
                for ex_ in range(NE):
                    a_ = aT[ai[0] % 2]
                    ai[0] += 1
                    for fb in range(DFF // WB):
                        wg = nextw()
                        wgv = wblock(wg, w_eg[ex_, :, fb * WB:(fb + 1) * WB], KC, WB)
                        wu = nextw()
                        wuv = wblock(wu, w_eu[ex_, :, fb * WB:(fb + 1) * WB], KC, WB)
                        for j in range(WB // 128):
                            fc = fb * (WB // 128) + j
                            cs_ = slice(j * 128, (j + 1) * 128)
                            psG, psU = PS(), PS()
                            for kc in range(KC):
                                MM(psG, psG.ap, wgv[:, kc, cs_], hT.ap[:, kc, :], kc == 0, kc == KC - 1, [wg.buf, hT.buf])
                            for kc in range(KC):
                                MM(psU, psU.ap, wuv[:, kc, cs_], hT.ap[:, kc, :], kc == 0, kc == KC - 1, [wu.buf, hT.buf])
                            s_ = sil[li[0] % 3]
                            li[0] += 1
                            ACT(s_.ap, psG.ap, AF.Silu, [psG.buf], [s_.buf])
                            TT(a_.ap[:, fc, :], s_.ap, psU.ap, ALU.mult, [s_.buf, psU.buf], [a_.buf])
                    for cb in range(D // DB):
                        wd = nextw()
                        wdv = wd.ap.rearrange("p a b -> p (a b)")[:, 0:FFC * DB].rearrange("p (a b) -> p a b", a=FFC)
                        DMA("pool", wdv, w_ed[ex_, :, cb * DB:(cb + 1) * DB].rearrange("(kc p) n -> p kc n", p=128), [], [wd.buf], wd.buf)
                        pst = [PS() for _ in range(4)]
                        for kc in range(FFC):
                            for s4 in range(4):
                                MM(pst[s4], pst[s4].ap[:, 0:DB], a_.ap[:, kc, s4 * 128:(s4 + 1) * 128], wdv[:, kc, :], kc == 0, kc == FFC - 1,
                                   [wd.buf, a_.buf])
                        for s4 in range(4):
                            xv = xt.ap[:, s4, cb * DB:(cb + 1) * DB]
                            STT(xv, pst[s4].ap[:, 0:DB], comb.ap[:, s4, ex_:ex_ + 1], xv, ALU.mult, ALU.add, [pst[s4].buf, comb.buf, xt.buf], [xt.buf])
                for s4 in range(4):
                    sm = smalls[xi[0] % 2]
                    xi[0] += 1
                    xv = xt.ap[:, s4, :]
                    ssq, t1, t2, rstd = sm.ap[:, 0:1], sm.ap[:, 1:2], sm.ap[:, 2:3], sm.ap[:, 3:4]
                    ACT(junk.ap, xv, AF.Square, [xt.buf], [junk.buf, sm.buf], accum=ssq)
                    TS(t1, ssq, 1.0 / D, EPS, ALU.mult, ALU.add, [sm.buf], [sm.buf])
                    ACT(t2, t1, AF.Sqrt, [sm.buf], [sm.buf])
                    P.op("dve", lambda e, o=rstd, i=t2: e.reciprocal(o, i), reads=[sm.buf], writes=[sm.buf])
                    STT(xs.ap, xv, rstd, gfb.ap, ALU.mult, ALU.mult, [xt.buf, sm.buf, gfb.buf], [xs.buf])
                    DMA("sp", p["y"][t0 + s4 * 128:t0 + (s4 + 1) * 128, :], xs.ap, [xs.buf], [p["b_y"]], xs.buf)


    def IDMA(out, out_off, in_, in_off, reads, writes, slot, eoff=0):
        def emit(e):
            oo = bass.IndirectOffsetOnAxis(ap=out_off, axis=0) if out_off is not None else None
            io = bass.IndirectOffsetOnAxis(ap=in_off, axis=0) if in_off is not None else None
            return e.indirect_dma_start(out=out, out_offset=oo, in_=in_, in_offset=io, element_offset=eoff)
        P.op("pool", emit, reads=reads, writes=writes, dma=slot)

    def phase_C2_sparse():
        subt = [(p, t0) for p in parts for t0 in range(0, p["own"], 128)]
        phase_reset()
        g2b = AR.alloc("g2b", [D])
        brb = AR.alloc("brb", [NR])
        wrs = AR.alloc("wrs", [KC, NR], BF16)
        DMA("sp", g2b.ap, g2b_d, [], [g2b.buf], g2b.buf)
        DMA("sp", brb.ap, br_d, [], [brb.buf], brb.buf)
        wblock(wrs, w_r, KC, NR)
        hT = AR.alloc("h2T", [KC, T], BF16)
        xts = [AR.alloc("x1a%d" % i, [D]) for i in range(2)]
        xs = AR.alloc("xsD", [D])
        htm = [AR.alloc("htm%d" % i, [D], BF16) for i in range(2)]
        smalls = [AR.alloc("smallD%d" % i, [4]) for i in range(2)]
        rt = AR.alloc("rt", [4, 64])
        xi = [0]
        for tb_ in range(NT // 4):
            for s4 in range(4):
                i = tb_ * 4 + s4
                p, t0 = subt[i]
                xt = xts[xi[0] % 2]
                sm = smalls[xi[0] % 2]
                hm = htm[xi[0] % 2]
                xi[0] += 1
                DMA("sp", xt.ap, p["x1"][t0:t0 + 128, :], [p["b_x1"]], [xt.buf], xt.buf)
                rms_to_T(xt, hT, s4 * 128, g2T, xs, sm, xs)
                TT(hm.ap, xs.ap, g2b.ap, ALU.mult, [xs.buf, g2b.buf], [hm.buf], eng="pool")
                DMA("sp", H_d[i * 128:(i + 1) * 128, :], hm.ap, [hm.buf], [b_H], hm.buf)
            for s4 in range(4):
                i = tb_ * 4 + s4
                ps = PS()
                for kc in range(KC):
                    MM(ps, ps.ap[:, 0:NR], hT.ap[:, kc, s4 * 128:(s4 + 1) * 128], wrs.ap[:, kc, :], kc == 0, kc == KC - 1,
                       [hT.buf, wrs.buf])
                R = rt.ap[:, s4, :]
                rb = [rt.buf]
                lg = R[:, 0:NR]
                TT(lg, ps.ap[:, 0:NR], brb.ap, ALU.add, [ps.buf, brb.buf], rb)
                gmax = R[:, 20:21]
                P.op("dve", lambda e, o=gmax, i_=lg[:, 0:NEG]: e.reduce_max(o, i_, AX.X), reads=rb, writes=rb)
                oh = R[:, 21:25]
                TS(oh, lg[:, 0:NEG], gmax, None, ALU.is_equal, None, rb, rb)
                ngm = R[:, 25:26]
                TS(ngm, gmax, -1.0, None, ALU.mult, None, rb, rb)
                eg_ = R[:, 26:30]
                sg_ = R[:, 30:31]
                ACT(eg_, lg[:, 0:NEG], AF.Exp, rb, rb, bias=ngm, accum=sg_)
                pg = R[:, 31:32]
                P.op("dve", lambda e, o=pg, i_=sg_: e.reciprocal(o, i_), reads=rb, writes=rb)
                les = R[:, 32:36]
                TS(les, lg[:, NEG:NEG + EPG], oh[:, 0:1], None, ALU.mult, None, rb, rb)
                for g in range(1, NEG):
                    STT(les, lg[:, NEG + g * EPG:NEG + (g + 1) * EPG], oh[:, g:g + 1], les, ALU.mult, ALU.add, rb, rb)
                m1 = R[:, 36:37]
                P.op("dve", lambda e, o=m1, i_=les: e.reduce_max(o, i_, AX.X), reads=rb, writes=rb)
                k1 = R[:, 37:41]
                TS(k1, les, m1, None, ALU.is_equal, None, rb, rb)
                le2 = R[:, 41:45]
                STT(le2, k1, -1e30, les, ALU.mult, ALU.add, rb, rb)
                m2 = R[:, 45:46]
                P.op("dve", lambda e, o=m2, i_=le2: e.reduce_max(o, i_, AX.X), reads=rb, writes=rb)
                k2 = R[:, 46:50]
                TS(k2, le2, m2, None, ALU.is_equal, None, rb, rb)
                dm = R[:, 50:51]
                TT(dm, m2, m1, ALU.subtract, rb, rb)
                ex = R[:, 51:52]
                ACT(ex, dm, AF.Exp, rb, rb)
                den = R[:, 52:53]
                TS(den, ex, 1.0, None, ALU.add, None, rb, rb)
                p1 = R[:, 53:54]
                P.op("dve", lambda e, o=p1, i_=den: e.reciprocal(o, i_), reads=rb, writes=rb)
                p2 = R[:, 54:55]
                TT(p2, ex, p1, ALU.mult, rb, rb)
                TT(rc12.ap[:, i, 0:1], p1, pg, ALU.mult, rb, [rc12.buf])
                TT(rc12.ap[:, i, 1:2], p2, pg, ALU.mult, rb, [rc12.buf])
                for g in range(NEG):
                    TS(rm1.ap[:, i, g * EPG:(g + 1) * EPG], k1, oh[:, g:g + 1], None, ALU.mult, None, rb, [rm1.buf])
                    TS(rm2.ap[:, i, g * EPG:(g + 1) * EPG], k2, oh[:, g:g + 1], None, ALU.mult, None, rb, [rm2.buf])
        Mbf = AR.alloc("Mbf", [NT, NE], BF16)
        onesb = AR.alloc("onesb", [128], BF16)
        trib = AR.alloc("trib", [128], BF16)
        Cs = AR.alloc("Cs", [NT, NE])
        posf = AR.alloc("posf", [NT, NE])
        prod = AR.alloc("prod", [NT, NE])
        pf12 = AR.alloc("pf12", [2, NT])
        tb = AR.alloc("tb", [96])
        jid = AR.alloc("jid", [JMAX])
        eacc = AR.alloc("eacc", [JMAX])
        DMA("pool", trib.ap, tri_d, [], [trib.buf], trib.buf)
        DMA("sp", jid.ap, jidx_d, [], [jid.buf], jid.buf)
        P.op("dve", lambda e: e.memset(onesb.ap, 1.0), writes=[onesb.buf])
        TT(Mbf.ap, rm1.ap, rm2.ap, ALU.add, [rm1.buf, rm2.buf], [Mbf.buf])
        for i in range(NT):
            ps = PS()
            for i2 in range(i):
                MM(ps, ps.ap[:, 0:NE], onesb.ap, Mbf.ap[:, i2, :], i2 == 0, False, [onesb.buf, Mbf.buf])
            MM(ps, ps.ap[:, 0:NE], trib.ap, Mbf.ap[:, i, :], i == 0, True, [trib.buf, Mbf.buf])
            CP(Cs.ap[:, i, :], ps.ap[:, 0:NE], [ps.buf], [Cs.buf])
        ps = PS()
        for i in range(NT):
            MM(ps, ps.ap[:, 0:NE], onesb.ap, Mbf.ap[:, i, :], i == 0, i == NT - 1, [onesb.buf, Mbf.buf])
        tbb = [tb.buf]
        n_ = tb.ap[:, 0:16]
        ntl = tb.ap[:, 16:32]
        cum = tb.ap[:, 32:48]
        bm1 = tb.ap[:, 48:64]
        tmp_ = tb.ap[:, 64:80]
        CP(n_, ps.ap[:, 0:NE], [ps.buf], tbb)
        P.op("dve", lambda e: e.memset(ntl, 0.0), reads=tbb, writes=tbb)
        for j in range(NTOK // TB):
            STT(ntl, n_, float(j * TB), ntl, ALU.is_gt, ALU.add, tbb, tbb)
        CP(cum[:, 0:1], ntl[:, 0:1], tbb, tbb)
        for e_ in range(1, NE):
            TT(cum[:, e_:e_ + 1], cum[:, e_ - 1:e_], ntl[:, e_:e_ + 1], ALU.add, tbb, tbb)
        TT(tmp_, cum, ntl, ALU.subtract, tbb, tbb)
        TS(bm1, tmp_, float(TB), -1.0, ALU.mult, ALU.add, tbb, tbb)
        TT(posf.ap, Cs.ap, bm1.unsqueeze(1).to_broadcast([128, NT, NE]), ALU.add, [Cs.buf] + tbb, [posf.buf])
        for k, rm in ((0, rm1), (1, rm2)):
            TT(prod.ap, posf.ap, rm.ap, ALU.mult, [posf.buf, rm.buf], [prod.buf])
            P.op("dve", lambda e, o=pf12.ap[:, k, :], i_=prod.ap: e.reduce_sum(o, i_, AX.X), reads=[prod.buf], writes=[pf12.buf])
            CP(rpos.ap[:, :, k], pf12.ap[:, k, :], [pf12.buf], [rpos.buf])
        P.op("dve", lambda e: e.memset(eacc.ap, 0.0), writes=[eacc.buf])
        for e_ in range(NE):
            STT(eacc.ap, jid.ap, cum[:, e_:e_ + 1], eacc.ap, ALU.is_ge, ALU.add, [jid.buf, eacc.buf] + tbb, [eacc.buf])
        TS(eacc.ap, eacc.ap, float(NE - 1), None, ALU.min, None, [eacc.buf], [eacc.buf])
        pidx = AR.alloc("pidx", [1])
        DMA("sp", pidx.ap, pidx_d, [], [pidx.buf], pidx.buf)
        TS(eacc.ap, eacc.ap, 128.0, pidx.ap[:, 0:1], ALU.mult, ALU.add, [eacc.buf, pidx.buf], [eacc.buf])
        CP(reid.ap, eacc.ap, [eacc.buf], [reid.buf])
        phase_reset()
        hb = [AR.alloc("hb%d" % i, [D], BF16) for i in range(2)]
        hbsem = [Buf("hbsem%d" % i) for i in range(2)]
        zt = AR.alloc("zt", [D], BF16)
        b_Hs0 = Buf("Hs0", dram=True)
        P.op("dve", lambda e: e.memset(zt.ap, 0.0), writes=[zt.buf])
        for r in range(NS // 128):
            DMA("sp", Hs_d[r * 128:(r + 1) * 128, :], zt.ap, [zt.buf], [b_Hs0], zt.buf)
        for i in range(NT):
            h = hb[i % 2]
            DMA("sp", h.ap, H_d[i * 128:(i + 1) * 128, :], [b_H], [h.buf], h.buf)
            for k in range(2):
                IDMA(Hs_d[:, :], rpos.ap[:, i, k:k + 1], h.ap, None, [h.buf, rpos.buf, b_Hs0], [b_Hs], hbsem[i % 2])
        NSUB = TB // 128
        hstm = [AR.alloc("hstm%d" % i, [NSUB, D], BF16) for i in range(2)]
        hsT = [AR.alloc("hsT%d" % i, [KC, TB], BF16) for i in range(2)]
        aT = [AR.alloc("aTs%d" % i, [FFC, TB], BF16) for i in range(2)]
        sil = [AR.alloc("sils%d" % i, [TB]) for i in range(3)]
        DB = min(512, D)
        WB = 128
        WKC = max(KC, (FFC * DB + WB - 1) // WB)
        NWS = 6
        wsl = [AR.alloc("wS%d" % i, [WKC, WB], BF16) for i in range(NWS)]
        ystg = [AR.alloc("ystg%d" % i, [DB]) for i in range(4)]
        identb = AR.alloc("identb", [128], BF16)
        CP(identb.ap, ident.ap, [ident.buf], [identb.buf])
        wi = [0]
        li = [0]
        yi = [0]
        regs = {}

        def wdyn(j, wb2, blk, n, view, slot):
            rowlen = wb2.shape[1]
            v2 = slot.ap.rearrange("p a b -> p (a b)")[:, 0:rowlen]
            IDMA(v2, None, wb2, reid.ap[:, j:j + 1], [reid.buf, b_wb], [slot.buf], slot.buf, eoff=blk * NE * 128 * rowlen)

        for j in range(JMAX):
            hm = hstm[j % 2]
            hT_ = hsT[j % 2]
            a_ = aT[j % 2]
            DMA("sp", hm.ap, Hs_d[j * TB:(j + 1) * TB, :].rearrange("(s p) d -> p s d", p=128), [b_Hs], [hm.buf], hm.buf)
            for st_ in range(NSUB):
                for kg in range(KC // 4):
                    ps = PS()
                    psb = ps.ap.bitcast(BF16)
                    for jj in range(4):
                        kc = kg * 4 + jj
                        TR(ps, psb[:, jj * 128:(jj + 1) * 128], hm.ap[:, st_, kc * 128:(kc + 1) * 128], identb.ap, [hm.buf, identb.buf])
                    src = psb[:, 0:512].rearrange("p (a b) -> p a b", a=4)
                    dst = hT_.ap[:, kg * 4:kg * 4 + 4, st_ * 128:(st_ + 1) * 128]
                    if kg % 2 == 0:
                        CP(dst, src, [ps.buf], [hT_.buf])
                    else:
                        ACT(dst, src, AF.Copy, [ps.buf], [hT_.buf])
            for fc in range(FFC):
                wg = wsl[wi[0] % NWS]
                wi[0] += 1
                wgv = wg.ap.rearrange("p a b -> p (a b)")[:, 0:KC * 128].rearrange("p (a b) -> p a b", a=KC)
                wdyn(j, wb_eg, fc, 128, wgv, wg)
                wu = wsl[wi[0] % NWS]
                wi[0] += 1
                wuv = wu.ap.rearrange("p a b -> p (a b)")[:, 0:KC * 128].rearrange("p (a b) -> p a b", a=KC)
                wdyn(j, wb_eu, fc, 128, wuv, wu)
                psG, psU = PS(), PS()
                for kc in range(KC):
                    MM(psG, psG.ap[:, 0:TB], wgv[:, kc, :], hT_.ap[:, kc, :], kc == 0, kc == KC - 1, [wg.buf, hT_.buf])
                for kc in range(KC):
                    MM(psU, psU.ap[:, 0:TB], wuv[:, kc, :], hT_.ap[:, kc, :], kc == 0, kc == KC - 1, [wu.buf, hT_.buf])
                s_ = sil[li[0] % 3]
                li[0] += 1
                ACT(s_.ap, psG.ap[:, 0:TB], AF.Silu, [psG.buf], [s_.buf])
                TT(a_.ap[:, fc, :], s_.ap, psU.ap[:, 0:TB], ALU.mult, [s_.buf, psU.buf], [a_.buf])
            for cb in range(D // DB):
                wd = wsl[wi[0] % NWS]
                wi[0] += 1
                wdv = wd.ap.rearrange("p a b -> p (a b)")[:, 0:FFC * DB].rearrange("p (a b) -> p a b", a=FFC)
                wdyn(j, wb_ed, cb, DB, wdv, wd)
                pst = [PS() for _ in range(NSUB)]
                for kc in range(FFC):
                    for st_ in range(NSUB):
                        MM(pst[st_], pst[st_].ap[:, 0:DB], a_.ap[:, kc, st_ * 128:(st_ + 1) * 128], wdv[:, kc, :], kc == 0, kc == FFC - 1,
                           [wd.buf, a_.buf])
                for st_ in range(NSUB):
                    ys = ystg[yi[0] % 4]
                    yi[0] += 1
                    if st_ % 2 == 0:
                        ACT(ys.ap, pst[st_].ap[:, 0:DB], AF.Copy, [pst[st_].buf], [ys.buf])
                    else:
                        CP(ys.ap, pst[st_].ap[:, 0:DB], [pst[st_].buf], [ys.buf])
                    r0 = j * TB + st_ * 128
                    DMA("sp", Ys_d[r0:r0 + 128, cb * DB:(cb + 1) * DB], ys.ap, [ys.buf], [b_Ys], ys.buf)
        phase_reset()
        gfb = AR.alloc("gfb", [D])
        DMA("sp", gfb.ap, gf_d, [], [gfb.buf], gfb.buf)
        x1b = [AR.alloc("x1e%d" % i, [D]) for i in range(2)]
        yab = [AR.alloc("ya%d" % i, [D]) for i in range(2)]
        ybb = [AR.alloc("yb%d" % i, [D]) for i in range(2)]
        xo = [AR.alloc("xo%d" % i, [D]) for i in range(2)]
        smalls = [AR.alloc("smallE%d" % i, [4]) for i in range(2)]
        for i in range(NT):
            p, t0 = subt[i]
            x = x1b[i % 2]
            ya, yb, o_ = yab[i % 2], ybb[i % 2], xo[i % 2]
            sm = smalls[i % 2]
            DMA("sp", x.ap, p["x1"][t0:t0 + 128, :], [p["b_x1"]], [x.buf], x.buf)
            IDMA(ya.ap, None, Ys_d[:, :], rpos.ap[:, i, 0:1], [b_Ys, rpos.buf], [ya.buf], ya.buf)
            IDMA(yb.ap, None, Ys_d[:, :], rpos.ap[:, i, 1:2], [b_Ys, rpos.buf], [yb.buf], yb.buf)
            STT(x.ap, ya.ap, rc12.ap[:, i, 0:1], x.ap, ALU.mult, ALU.add, [ya.buf, rc12.buf, x.buf], [x.buf])
            STT(x.ap, yb.ap, rc12.ap[:, i, 1:2], x.ap, ALU.mult, ALU.add, [yb.buf, rc12.buf, x.buf], [x.buf])
            ssq, t1, t2, rstd = sm.ap[:, 0:1], sm.ap[:, 1:2], sm.ap[:, 2:3], sm.ap[:, 3:4]
            ACT(o_.ap, x.ap, AF.Square, [x.buf], [o_.buf, sm.buf], accum=ssq)
            TS(t1, ssq, 1.0 / D, EPS, ALU.mult, ALU.add, [sm.buf], [sm.buf])
            ACT(t2, t1, AF.Sqrt, [sm.buf], [sm.buf])
            P.op("dve", lambda e, o=rstd, i_=t2: e.reciprocal(o, i_), reads=[sm.buf], writes=[sm.buf])
            STT(o_.ap, x.ap, rstd, gfb.ap, ALU.mult, ALU.mult, [x.buf, sm.buf, gfb.buf], [o_.buf])
            DMA("sp", p["y"][t0:t0 + 128, :], o_.ap, [o_.buf], [p["b_y"]], o_.buf)

    phase_A()
    phase_B1()
    phase_B2()
    phase_C1()
    if cfg.get("dense_moe", False):
        phase_C2()
    else:
        phase_C2_sparse()
    P.op("sp", lambda e: e.nop(), reads=[p["b_y"] for p in parts])
    P.emit_all(nc)
    st.close()
    return nc


def _slopes(HPG):
    nh = NGRP * HPG
    s = 2.0 ** (-8.0 * np.arange(1, nh + 1) / nh)
    return s.astype(np.float32).reshape(NGRP, HPG)


def _const_tables(cfg):
    HPG, FGW = cfg["HPG"], cfg["FGW"]
    sl = _slopes(HPG).astype(np.float64)
    a = np.arange(128)[:, None]
    b = np.arange(128)[None, :]
    etA = np.zeros((128, NGRP * HPG, 128), np.float32)
    etB = np.zeros((128, NGRP * HPG, 128), np.float32)
    for g in range(NGRP):
        for h in range(HPG):
            s = np.float64(np.float32(sl[g, h])) * DIL[g]
            ea = np.where(a >= b, np.exp(-s * np.abs(a - b - 64)), 0.0)
            eb = np.where(a <= b, np.exp(-s * np.abs(a - b + 64)), 0.0)
            etA[:, g * HPG + h, :] = ea
            etB[:, g * HPG + h, :] = eb
    etA0 = np.ascontiguousarray(etA[64:128])
    c = np.arange(FGW)
    ang = 2.0 * np.pi * np.outer(c, c) / FGW
    cc = (np.cos(ang) / np.sqrt(FGW)).astype(np.float32)
    scn = (-np.sin(ang) / np.sqrt(FGW)).astype(np.float32)
    return dict(etA=etA, etB=etB, etA0=etA0, cc=cc, scn=scn, ident=np.eye(128, dtype=np.float32))


def _dft_local(S, own, half):
    loc = np.arange(S, dtype=np.int64)
    glob = loc if half == 0 else S - 1 - loc
    prod = np.outer(glob, glob[:own]) % S
    ang = 2.0 * np.pi * prod / S
    return (np.cos(ang) / np.sqrt(S)).astype(np.float32), (np.sin(ang) / np.sqrt(S)).astype(np.float32)


def _relayout_up(w):
    ne, d, dff = w.shape
    kc, ffc = d // 128, dff // 128
    r = w.reshape(ne, kc, 128, ffc, 128).transpose(3, 0, 2, 1, 4)
    return np.ascontiguousarray(r).reshape(ffc, ne * 128, kc * 128)


def _relayout_down(w):
    ne, dff, d = w.shape
    ffc = dff // 128
    db = min(512, d)
    r = w.reshape(ne, ffc, 128, d // db, db).transpose(3, 0, 2, 1, 4)
    return np.ascontiguousarray(r).reshape(d // db, ne * 128, ffc * db)


def _run(cfg, x_prompt, x_sample, attn_norm_g, w_in, w_branch_attn, w_branch_fourier, b_gate, w_out, ffn_norm_g,
         w_router_group, b_router_group, w_router_expert, b_router_expert, w_expert_gate, w_expert_up, w_expert_down,
         final_norm_g, n_cores=8):
    D = cfg["D"]
    KC = D // 128
    f = lambda a: np.ascontiguousarray(np.asarray(a, dtype=np.float32))
    x_prompt, x_sample = f(x_prompt), f(x_sample)
    shared = dict(
        w_in=f(w_in)[0], w_ba=f(w_branch_attn)[0], w_bf=f(w_branch_fourier)[0], w_out=f(w_out)[0],
        w_r=np.ascontiguousarray(np.concatenate(
            [f(w_router_group)[0], f(w_router_expert)[0].transpose(1, 0, 2).reshape(D, NE)], axis=1)),
        w_eg=_relayout_up(f(w_expert_gate)[0]), w_eu=_relayout_up(f(w_expert_up)[0]),
        w_ed=_relayout_down(f(w_expert_down)[0]),
        pidx=np.arange(128, dtype=np.float32).reshape(128, 1),
        g1T=np.ascontiguousarray(f(attn_norm_g)[0].reshape(KC, 128).T),
        g2T=np.ascontiguousarray(f(ffn_norm_g)[0].reshape(KC, 128).T),
        bgT=np.ascontiguousarray(f(b_gate)[0].reshape(2 * KC, 128).T),
        gf_b=np.ascontiguousarray(np.broadcast_to(f(final_norm_g)[None, :], (128, D))),
        br_b=np.ascontiguousarray(np.broadcast_to(
            np.concatenate([f(b_router_group)[0], f(b_router_expert)[0].reshape(NE)])[None, :], (128, NR))),
    )
    shared.update(_const_tables(cfg))
    TB = cfg.get("TB", 256)
    ntok = (cfg["S_S"] + cfg["S_P"]) // 2
    jmax = 2 * ntok // TB + NE
    shared["g2_b"] = np.ascontiguousarray(np.broadcast_to(f(ffn_norm_g)[0][None, :], (128, D)))
    shared["jidx"] = np.ascontiguousarray(np.broadcast_to(np.arange(jmax, dtype=np.float32)[None, :], (128, jmax)))
    shared["tri"] = np.triu(np.ones((128, 128), np.float32))
    dft = {}
    for nm, S in (("s", cfg["S_S"]), ("p", cfg["S_P"])):
        for half in (0, 1):
            dft[(nm, half)] = _dft_local(S, S // 2, half)
    in_maps = []
    for c in range(n_cores):
        b, half = c // 2, c % 2
        m = dict(shared)
        xs = x_sample[b] if half == 0 else x_sample[b, ::-1]
        xp = x_prompt[b] if half == 0 else x_prompt[b, ::-1]
        m["x_s"] = np.ascontiguousarray(xs)
        m["x_p"] = np.ascontiguousarray(xp)
        m["cs_s"], m["ss_s"] = dft[("s", half)]
        m["cs_p"], m["ss_p"] = dft[("p", half)]
        in_maps.append(m)
    nc = build_nc(cfg)
    res = run_bass_kernel_spmd(nc, in_maps, core_ids=list(range(n_cores)))
    nb = n_cores // 2
    y_p = np.zeros((nb, cfg["S_P"], D), np.float32)
    y_s = np.zeros((nb, cfg["S_S"], D), np.float32)
    for c in range(n_cores):
        b, half = c // 2, c % 2
        r = res.results[c]
        for nm, S, dst in (("p", cfg["S_P"], y_p), ("s", cfg["S_S"], y_s)):
            o = np.asarray(r["y_" + nm], dtype=np.float32)
            if half == 0:
                dst[b, :S // 2] = o
            else:
                dst[b, S // 2:] = o[::-1]
    return y_p, y_s


def kernel(**inputs):
    return _run(FULL_CFG, **inputs)
```

```python
import math
from contextlib import ExitStack

import numpy as np
import concourse.bass as bass
import concourse.mybir as mybir
from concourse.bass_utils import run_bass_kernel_spmd

F32 = mybir.dt.float32
BF16 = mybir.dt.bfloat16
I32 = mybir.dt.int32
AF = mybir.ActivationFunctionType
ALU = mybir.AluOpType
AX = mybir.AxisListType

FULL_CFG = dict(D=4096, HPG=8, FGW=512, DFF=1024, S_S=4096, S_P=2048)
NGRP = 3
DIL = (1, 4, 16)
NFG = 4
NEG = 4
EPG = 4
NE = NEG * EPG
NR = NEG + NE
T = 512
EPS = 1e-6
ARENA_WORDS = 48 * 1024


class Buf:
    __slots__ = ("name", "dram", "w", "r", "cnt", "sid")

    def __init__(self, name, dram=False):
        self.name = name
        self.dram = dram
        self.w = {}
        self.r = {}
        self.cnt = 0
        self.sid = None


def _merge(dst, src):
    for k, v in src.items():
        if dst.get(k, -1) < v:
            dst[k] = v


class Op:
    __slots__ = ("emit", "deps", "seq", "dma")


class Prog:
    ENGS = ("pe", "act", "dve", "pool", "sp")

    def __init__(self):
        self.q = {e: [] for e in self.ENGS}
        self.needed = {e: set() for e in self.ENGS}
        self.barrier_deps = {}
        self.slots = []
        self.last_compute = {}

    def op(self, eng, emit, reads=(), writes=(), dma=None):
        o = Op()
        o.emit = emit
        deps = dict(self.barrier_deps)
        for b in reads:
            _merge(deps, b.w)
        for b in writes:
            _merge(deps, b.w)
            _merge(deps, b.r)
        o.seq = len(self.q[eng])
        if dma is not None:
            if dma.sid is None:
                dma.sid = len(self.slots)
                self.slots.append(dma)
            dma.cnt += 1
            key, val = ("dma", dma.sid), dma.cnt * 16
            o.dma = dma.sid
        else:
            key, val = ("eng", eng), o.seq
            o.dma = None
            self.last_compute[eng] = o.seq
        for k, v in deps.items():
            if k[0] == "eng" and not (k[1] == "pe" and eng == "pe"):
                self.needed[k[1]].add(v)
        for b in reads:
            if not b.dram and b.r.get(key, -1) < val:
                b.r[key] = val
        for b in writes:
            if b.dram:
                if b.w.get(key, -1) < val:
                    b.w[key] = val
            elif b.r:
                b.w = {key: val}
                b.r = {}
            else:
                if b.w.get(key, -1) < val:
                    b.w[key] = val
        o.deps = deps
        self.q[eng].append(o)
        return o

    def barrier(self):
        d = {}
        for e, v in self.last_compute.items():
            d[("eng", e)] = v
        for s in self.slots:
            d[("dma", s.sid)] = s.cnt * 16
        self.barrier_deps = d

    def emit_all(self, nc):
        ranks = {}
        for e in self.ENGS:
            ranks[e] = {s: i + 1 for i, s in enumerate(sorted(self.needed[e]))}
        with ExitStack() as st:
            esem = {e: st.enter_context(nc.semaphore("sem_" + e)) for e in self.ENGS}
            dsem = [st.enter_context(nc.semaphore("dsem%d" % i)) for i in range(len(self.slots))]
            block = st.enter_context(nc.Block())

            def run(ename, eng):
                known = {}
                for o in self.q[ename]:
                    for k, v in o.deps.items():
                        if k[0] == "eng":
                            if k[1] == "pe" and ename == "pe":
                                continue
                            sem, val = esem[k[1]], ranks[k[1]][v]
                        else:
                            sem, val = dsem[k[1]], v
                        if known.get(k, -1) >= val:
                            continue
                        eng.wait_ge(sem, val)
                        known[k] = val
                    inst = o.emit(eng)
                    if o.dma is not None:
                        inst.then_inc(dsem[o.dma], 16)
                    elif o.seq in ranks[ename]:
                        inst.then_inc(esem[ename], 1)

            @block.tensor
            def _(e):
                run("pe", e)

            @block.scalar
            def _(e):
                run("act", e)

            @block.vector
            def _(e):
                run("dve", e)

            @block.gpsimd
            def _(e):
                run("pool", e)

            @block.sync
            def _(e):
                run("sp", e)


class Tile:
    __slots__ = ("ap", "buf")

    def __init__(self, ap, buf):
        self.ap = ap
        self.buf = buf


class Arena:
    def __init__(self, ap, nwords):
        self.base = ap
        self.n = nwords
        self.off = 0

    def reset(self):
        self.off = 0

    def alloc(self, name, free_shape, dtype=F32):
        n = 1
        for s in free_shape:
            n *= s
        words = n if dtype in (F32, I32) else (n + 1) // 2
        words = (words + 7) // 8 * 8
        assert self.off + words <= self.n, "SBUF arena overflow at %s: %d + %d > %d" % (name, self.off, words, self.n)
        a = self.base[:, self.off:self.off + words]
        self.off += words
        if dtype != F32:
            a = a.bitcast(dtype)
        a = a[:, 0:n]
        if len(free_shape) == 2:
            a = a.rearrange("p (a b) -> p a b", a=free_shape[0])
        elif len(free_shape) == 3:
            a = a.rearrange("p (a b c) -> p a b c", a=free_shape[0], b=free_shape[1])
        return Tile(a, Buf(name))


def build_nc(cfg):
    D, HPG, FGW, DFF = cfg["D"], cfg["HPG"], cfg["FGW"], cfg["DFF"]
    KC = D // 128
    AW = NGRP * HPG * 128
    GW = HPG * 128
    AO = HPG * 128
    FW = NFG * FGW
    FCG = FGW // 128
    IN_W = 3 * AW + FW + 2 * D
    FFC = DFF // 128
    parts = [dict(name="s", S=cfg["S_S"], own=cfg["S_S"] // 2), dict(name="p", S=cfg["S_P"], own=cfg["S_P"] // 2)]
    for p in parts:
        p["ext"] = p["own"] + 1024
        assert p["ext"] <= p["S"]
    scale = 1.0 / math.sqrt(128.0)

    nc = bass.Bass("TRN2", target_bir_lowering=False)

    def din(name, shape, dt=F32):
        return nc.dram_tensor(name, list(shape), dt, kind="ExternalInput").ap()

    def dscr(name, shape, dt=BF16):
        return nc.dram_tensor(name, list(shape), dt, kind="Internal").ap()

    for p in parts:
        n = p["name"]
        p["x"] = din("x_" + n, [p["S"], D])
        p["cs"] = din("cs_" + n, [p["S"], p["own"]])
        p["ss"] = din("ss_" + n, [p["S"], p["own"]])
        p["y"] = nc.dram_tensor("y_" + n, [p["own"], D], F32, kind="ExternalOutput").ap()
        p["qT"] = dscr("qT_" + n, [AW, p["own"]])
        p["kT"] = dscr("kT_" + n, [AW, p["ext"]])
        p["v"] = dscr("v_" + n, [p["ext"], AW])
        p["f"] = dscr("f_" + n, [p["S"], FW])
        p["oT"] = dscr("oT_" + n, [AO, p["own"]])
        p["frT"] = dscr("frT_" + n, [FW, p["own"]])
        p["x1"] = dscr("x1_" + n, [p["own"], D], F32)
        for k in ("qT", "kT", "v", "f", "oT", "frT", "x1", "y"):
            p["b_" + k] = Buf(k + "_" + n, dram=True)
    w_in = din("w_in", [D, IN_W])
    w_ba = din("w_ba", [AO, D])
    w_bf = din("w_bf", [FW, D])
    w_out = din("w_out", [D, D])
    w_r = din("w_r", [D, NR])
    DBX = min(512, D)
    w_eg = din("w_eg", [FFC, NE * 128, KC * 128])
    w_eu = din("w_eu", [FFC, NE * 128, KC * 128])
    w_ed = din("w_ed", [D // DBX, NE * 128, FFC * DBX])
    pidx_d = din("pidx", [128, 1])
    wb_eg = dscr("wb_eg", [FFC * NE * 128, KC * 128])
    wb_eu = dscr("wb_eu", [FFC * NE * 128, KC * 128])
    wb_ed = dscr("wb_ed", [(D // DBX) * NE * 128, FFC * DBX])
    b_wb = Buf("wb", dram=True)
    conv_jobs = []
    for (src3, dst2) in ((w_eg, wb_eg), (w_eu, wb_eu), (w_ed, wb_ed)):
        src2 = src3.rearrange("f r n -> (f r) n")
        for r0 in range(0, dst2.shape[0], 128):
            conv_jobs.append((src2, dst2, r0))
    conv_state = dict(i=0, slots=None)

    def emit_conv(n):
        for _ in range(n):
            if conv_state["i"] >= len(conv_jobs):
                return
            src2, dst2, r0 = conv_jobs[conv_state["i"]]
            sem = conv_state["ssem"][conv_state["i"] % len(conv_state["ssem"])]
            conv_state["i"] += 1
            DMA("pool", dst2[r0:r0 + 128, :], src2[r0:r0 + 128, :], [], [b_wb], sem)
    g1T_d = din("g1T", [128, KC])
    g2T_d = din("g2T", [128, KC])
    bgT_d = din("bgT", [128, 2 * KC])
    gf_d = din("gf_b", [128, D])
    br_d = din("br_b", [128, NR])
    ident_d = din("ident", [128, 128])
    cc_d = din("cc", [FGW, FGW])
    sc_d = din("scn", [FGW, FGW])
    ea_d = din("etA", [128, NGRP * HPG, 128])
    eb_d = din("etB", [128, NGRP * HPG, 128])
    ea0_d = din("etA0", [64, NGRP * HPG, 128])
    TB = cfg.get("TB", 256)
    NTOK = sum(p["own"] for p in parts)
    NT = NTOK // 128
    JMAX = 2 * NTOK // TB + NE
    NS = JMAX * TB
    g2b_d = din("g2_b", [128, D])
    jidx_d = din("jidx", [128, JMAX])
    tri_d = din("tri", [128, 128])
    H_d = dscr("H_scr", [NTOK, D])
    Hs_d = dscr("Hs_scr", [NS, D])
    Ys_d = dscr("Ys_scr", [NS, D], F32)
    b_H, b_Hs, b_Ys = Buf("H", dram=True), Buf("Hs", dram=True), Buf("Ys", dram=True)

    P = Prog()
    st = ExitStack()
    arena_t = st.enter_context(nc.sbuf_tensor("arena", [128, ARENA_WORDS], F32))
    AR = Arena(arena_t[:], ARENA_WORDS)
    pss = []
    for i in range(8):
        pt = st.enter_context(nc.psum_tensor("ps%d" % i, [128, 512], F32))
        pss.append(Tile(pt[:], Buf("ps%d" % i)))
    psi = [0]

    def PS():
        t = pss[psi[0] % 8]
        psi[0] += 1
        return t

    def MM(ps, out, lhsT, rhs, start, stop, reads):
        P.op("pe", lambda e: e.matmul(out, lhsT, rhs, start=start, stop=stop), reads=reads, writes=[ps.buf])

    def TR(ps, out, in_, ident, reads):
        P.op("pe", lambda e: e.transpose(out, in_, ident), reads=reads, writes=[ps.buf])

    def DMA(q, out, in_, reads, writes, slot):
        P.op(q, lambda e: e.dma_start(out=out, in_=in_), reads=reads, writes=writes, dma=slot)

    def ACT(out, in_, func, reads, writes, bias=None, scale=None, accum=None):
        kw = {}
        if bias is not None:
            kw["bias"] = bias
        if scale is not None:
            kw["scale"] = scale
        if accum is not None:
            kw["accum_out"] = accum
        P.op("act", lambda e: e.activation(out, in_, func, **kw), reads=reads, writes=writes)

    def TT(out, in0, in1, op, reads, writes, eng="dve"):
        P.op(eng, lambda e: e.tensor_tensor(out, in0, in1, op), reads=reads, writes=writes)

    def TS(out, in0, s1, s2, op0, op1, reads, writes, eng="dve"):
        if op1 is None:
            P.op(eng, lambda e: e.tensor_scalar(out, in0, s1, None, op0), reads=reads, writes=writes)
        else:
            P.op(eng, lambda e: e.tensor_scalar(out, in0, s1, s2, op0, op1), reads=reads, writes=writes)

    def STT(out, in0, scalar, in1, op0, op1, reads, writes, eng="dve"):
        P.op(eng, lambda e: e.scalar_tensor_tensor(out, in0, scalar, in1, op0, op1), reads=reads, writes=writes)

    def CP(out, in_, reads, writes, eng="dve"):
        P.op(eng, lambda e: e.tensor_copy(out, in_), reads=reads, writes=writes)

    def wblock(slot, src2d, kchunks, ncols):
        v = slot.ap[:, 0:kchunks, 0:ncols]
        DMA("pool", v, src2d.rearrange("(kc p) n -> p kc n", p=128), [], [slot.buf], slot.buf)
        return v

    def rms_to_T(xt, dstT, col0, gT, junk, small, xs):
        ssq = small.ap[:, 0:1]
        t1 = small.ap[:, 1:2]
        t2 = small.ap[:, 2:3]
        rstd = small.ap[:, 3:4]
        ACT(junk.ap, xt.ap, AF.Square, [xt.buf], [junk.buf, small.buf], accum=ssq)
        TS(t1, ssq, 1.0 / D, EPS, ALU.mult, ALU.add, [small.buf], [small.buf])
        ACT(t2, t1, AF.Sqrt, [small.buf], [small.buf])
        P.op("dve", lambda e: e.reciprocal(rstd, t2), reads=[small.buf], writes=[small.buf])
        TS(xs.ap, xt.ap, rstd, None, ALU.mult, None, [xt.buf, small.buf], [xs.buf])
        for kg in range(KC // 4):
            ps = PS()
            for j in range(4):
                kc = kg * 4 + j
                TR(ps, ps.ap[:, j * 128:(j + 1) * 128], xs.ap[:, kc * 128:(kc + 1) * 128], ident.ap, [xs.buf, ident.buf])
            gb = gT.ap[:, kg * 4:kg * 4 + 4].unsqueeze(2).to_broadcast([128, 4, 128])
            TT(dstT.ap[:, kg * 4:kg * 4 + 4, col0:col0 + 128], ps.ap.rearrange("p (a b) -> p a b", a=4), gb, ALU.mult,
               [ps.buf, gT.buf], [dstT.buf])

    ident = AR.alloc("ident", [128])
    g1T = AR.alloc("g1T", [KC])
    g2T = AR.alloc("g2T", [KC])
    DMA("sp", ident.ap, ident_d, [], [ident.buf], ident.buf)
    DMA("sp", g1T.ap, g1T_d, [], [g1T.buf], g1T.buf)
    DMA("sp", g2T.ap, g2T_d, [], [g2T.buf], g2T.buf)
    rm1 = AR.alloc("rm1", [NT, NE])
    rm2 = AR.alloc("rm2", [NT, NE])
    rc12 = AR.alloc("rc12", [NT, 2])
    rpos = AR.alloc("rpos", [NT, 2], I32)
    reid = AR.alloc("reid", [JMAX], I32)
    const_off = AR.off

    def phase_reset():
        P.barrier()
        AR.off = const_off

    def phase_A():
        phase_reset()
        hT = AR.alloc("hT", [KC, T], BF16)
        xts = [AR.alloc("xtA%d" % i, [D]) for i in range(2)]
        xs = AR.alloc("xsA", [D])
        junk = AR.alloc("junkA", [D], BF16)
        smalls = [AR.alloc("smallA%d" % i, [4]) for i in range(2)]
        wsl = [AR.alloc("wA%d" % i, [KC, 512], BF16) for i in range(2)]
        stg = [AR.alloc("stgA%d" % i, [512], BF16) for i in range(4)]
        wi = [0]
        si = [0]
        xi = [0]
        for p in parts:
            ntile = p["S"] // T
            for tt in range(ntile):
                t0 = tt * T
                if t0 < p["own"]:
                    kind = "own"
                elif t0 < p["ext"]:
                    kind = "halo"
                else:
                    kind = "far"
                for s4 in range(4):
                    xt = xts[xi[0] % 2]
                    sm = smalls[xi[0] % 2]
                    xi[0] += 1
                    DMA("sp", xt.ap, p["x"][t0 + s4 * 128:t0 + (s4 + 1) * 128, :], [], [xt.buf], xt.buf)
                    rms_to_T(xt, hT, s4 * 128, g1T, junk, sm, xs)
                secs = []
                if kind == "own":
                    secs.append((0, AW, "fm", p["qT"], p["b_qT"], 0, T))
                    secs.append((AW, AW, "fm", p["kT"], p["b_kT"], 0, T))
                    secs.append((2 * AW, AW, "tm", p["v"], p["b_v"], 0, T))
                elif kind == "halo":
                    if t0 < p["own"] + T:
                        nh = 64 * DIL[1]
                        secs.append((AW, 2 * GW, "fm", p["kT"], p["b_kT"], 0, nh))
                        secs.append((2 * AW, 2 * GW, "tm", p["v"], p["b_v"], 0, nh))
                    secs.append((AW + 2 * GW, GW, "fm", p["kT"], p["b_kT"], 2 * GW, T))
                    secs.append((2 * AW + 2 * GW, GW, "tm", p["v"], p["b_v"], 2 * GW, T))
                secs.append((3 * AW, FW, "tm", p["f"], p["b_f"], 0, T))
                for (c0, ncols, mode, dst, dstb, dc0, ntok) in secs:
                    bw = 512 if ncols % 512 == 0 else 128
                    for blk in range(ncols // bw):
                        ws = wsl[wi[0] % 2]
                        wi[0] += 1
                        wv = wblock(ws, w_in[:, c0 + blk * bw:c0 + (blk + 1) * bw], KC, bw)
                        if mode == "fm":
                            for j in range(bw // 128):
                                ps = PS()
                                for kc in range(KC):
                                    MM(ps, ps.ap[:, 0:ntok], wv[:, kc, j * 128:(j + 1) * 128], hT.ap[:, kc, 0:ntok], kc == 0, kc == KC - 1,
                                       [ws.buf, hT.buf])
                                sg = stg[si[0] % 4]
                                si[0] += 1
                                ACT(sg.ap[:, 0:ntok], ps.ap[:, 0:ntok], AF.Copy, [ps.buf], [sg.buf])
                                r0 = dc0 + blk * bw + j * 128
                                DMA("sp", dst[r0:r0 + 128, t0:t0 + ntok], sg.ap[:, 0:ntok], [sg.buf], [dstb], sg.buf)
                        else:
                            ns4 = ntok // 128
                            pst = [PS() for _ in range(ns4)]
                            for kc in range(KC):
                                for s4 in range(ns4):
                                    MM(pst[s4], pst[s4].ap[:, 0:bw], hT.ap[:, kc, s4 * 128:(s4 + 1) * 128], wv[:, kc, :],
                                       kc == 0, kc == KC - 1, [ws.buf, hT.buf])
                            for s4 in range(ns4):
                                sg = stg[si[0] % 4]
                                si[0] += 1
                                if s4 % 2 == 0:
                                    ACT(sg.ap[:, 0:bw], pst[s4].ap[:, 0:bw], AF.Copy, [pst[s4].buf], [sg.buf])
                                else:
                                    CP(sg.ap[:, 0:bw], pst[s4].ap[:, 0:bw], [pst[s4].buf], [sg.buf])
                                cc0 = dc0 + blk * bw
                                DMA("sp", dst[t0 + s4 * 128:t0 + (s4 + 1) * 128, cc0:cc0 + bw], sg.ap[:, 0:bw], [sg.buf], [dstb],
                                    sg.buf)

    def phase_B1():
        phase_reset()
        etA = AR.alloc("etA", [NGRP * HPG, 128])
        etB = AR.alloc("etB", [NGRP * HPG, 128])
        etA0 = AR.alloc("etA0", [NGRP * HPG, 128])
        ones = AR.alloc("ones", [128], BF16)
        DMA("sp", etA.ap, ea_d, [], [etA.buf], etA.buf)
        DMA("sp", etB.ap, eb_d, [], [etB.buf], etB.buf)
        DMA("sp", etA0.ap[0:64], ea0_d, [], [etA0.buf], etA0.buf)
        P.op("dve", lambda e: e.memset(ones.ap, 1.0), writes=[ones.buf])
        maxown = max(p["own"] for p in parts)
        ksz = [max(p["own"] + 64 * DIL[g] for p in parts) for g in range(NGRP)]
        vsz = [max((max(1, p["own"] // DIL[g] // 128) + 1) * DIL[g] * 128 for p in parts) for g in range(NGRP)]
        qs = [[AR.alloc("q%d_%d" % (i, g), [maxown], BF16) for g in range(NGRP)] for i in range(2)]
        ks = [[AR.alloc("k%d_%d" % (i, g), [ksz[g]], BF16) for g in range(NGRP)] for i in range(2)]
        vs = [[AR.alloc("v%d_%d" % (i, g), [vsz[g]], BF16) for g in range(NGRP)] for i in range(2)]
        oacc = [AR.alloc("oacc%d" % i, [maxown]) for i in range(2)]
        dacc = [AR.alloc("dacc%d" % i, [maxown]) for i in range(2)]
        ost = [AR.alloc("ost%d" % i, [maxown], BF16) for i in range(2)]
        pfs = [AR.alloc("pf%d" % i, [128]) for i in range(4)]
        pbs = [AR.alloc("pb%d" % i, [128], BF16) for i in range(4)]
        hi = [0]
        bi = [0]
        conv_state["ssem"] = [Buf("cvs%d" % i) for i in range(4)]
        per_head = (len(conv_jobs) // 2 + 2 * HPG - 1) // (2 * HPG)
        for p in parts:
            own, ext = p["own"], p["ext"]
            for h in range(HPG):
                if not cfg.get("dense_moe", False):
                    emit_conv(per_head)
                par = hi[0] % 2
                hi[0] += 1
                oa, da, os_ = oacc[par], dacc[par], ost[par]
                for g in range(NGRP):
                    d = DIL[g]
                    Lo = own // d
                    eg = own + 64 * d
                    row0 = (g * HPG + h) * 128
                    qt, kt, vt = qs[par][g], ks[par][g], vs[par][g]
                    DMA("sp", qt.ap[:, 0:own], p["qT"][row0:row0 + 128, 0:own], [p["b_qT"]], [qt.buf], qt.buf)
                    DMA("sp", kt.ap[:, 0:eg], p["kT"][row0:row0 + 128, 0:eg], [p["b_kT"]], [kt.buf], kt.buf)
                    nq = max(1, Lo // 128)
                    Q = min(128, Lo)
                    vcol = slice(row0, row0 + 128)
                    MT = nq + 1
                    vv = vt.ap[:, 0:MT * d * 128].rearrange("p (m r c) -> p m r c", m=MT, r=d)
                    DMA("sp", vv[0:64, 0], p["v"][0:64 * d, vcol].rearrange("(p r) c -> p r c", r=d), [p["b_v"]], [vt.buf], vt.buf)
                    if Lo >= 128:
                        for m in range(1, MT):
                            tok0 = (128 * m - 64) * d
                            DMA("sp", vv[:, m], p["v"][tok0:tok0 + 128 * d, vcol].rearrange("(p r) c -> p r c", r=d), [p["b_v"]],
                                [vt.buf], vt.buf)
                    else:
                        DMA("sp", vv[0:Q, 1], p["v"][64 * d:(64 + Q) * d, vcol].rearrange("(p r) c -> p r c", r=d), [p["b_v"]],
                            [vt.buf], vt.buf)
                    eidx = g * HPG + h
                    for r in range(d):
                        for iq in range(nq):
                            i0 = iq * 128
                            qv = qt.ap[:, i0 * d + r:(i0 + Q) * d:d]
                            psO = PS()
                            psD = PS()
                            tiles = []
                            if iq == 0:
                                tiles.append((0, 64, 0, etA0.ap[0:64, eidx, 0:Q], etA0.buf))
                            else:
                                tiles.append((i0 - 64, 128, iq, etA.ap[:, eidx, 0:Q], etA.buf))
                            KB = Q
                            tiles.append((i0 + 64, KB, iq + 1, etB.ap[0:KB, eidx, 0:Q], etB.buf))
                            for ti, (lo, K, m, E, Eb) in enumerate(tiles):
                                kv = kt.ap[:, lo * d + r:(lo + K) * d:d]
                                psS = PS()
                                MM(psS, psS.ap[0:K, 0:Q], kv, qv, True, True, [kt.buf, qt.buf])
                                pf = pfs[bi[0] % 4]
                                pb = pbs[bi[0] % 4]
                                bi[0] += 1
                                ACT(pf.ap[0:K, 0:Q], psS.ap[0:K, 0:Q], AF.Exp, [psS.buf], [pf.buf], scale=scale)
                                TT(pb.ap[0:K, 0:Q], pf.ap[0:K, 0:Q], E, ALU.mult, [pf.buf, Eb], [pb.buf])
                                MM(psO, psO.ap[:, 0:Q], vv[0:K, m, r, :], pb.ap[0:K, 0:Q], ti == 0, ti == 1, [vt.buf, pb.buf])
                                MM(psD, psD.ap[:, 0:Q], ones.ap[0:K, :], pb.ap[0:K, 0:Q], ti == 0, ti == 1, [ones.buf, pb.buf])
                            ov = oa.ap[:, i0 * d + r:(i0 + Q) * d:d]
                            dv = da.ap[:, i0 * d + r:(i0 + Q) * d:d]
                            if g == 0:
                                ACT(ov, psO.ap[:, 0:Q], AF.Copy, [psO.buf], [oa.buf])
                                CP(dv, psD.ap[:, 0:Q], [psD.buf], [da.buf])
                            else:
                                TT(ov, ov, psO.ap[:, 0:Q], ALU.add, [psO.buf, oa.buf], [oa.buf])
                                TT(dv, dv, psD.ap[:, 0:Q], ALU.add, [psD.buf, da.buf], [da.buf])
                P.op("dve", lambda e, a=da.ap[:, 0:own]: e.reciprocal(a, a), reads=[da.buf], writes=[da.buf])
                TT(os_.ap[:, 0:own], oa.ap[:, 0:own], da.ap[:, 0:own], ALU.mult, [oa.buf, da.buf], [os_.buf], eng="pool")
                DMA("pool", p["oT"][h * 128:(h + 1) * 128, 0:own], os_.ap[:, 0:own], [os_.buf], [p["b_oT"]], os_.buf)

    def phase_B2():
        phase_reset()
        ccs = AR.alloc("ccs", [FCG, FGW], BF16)
        scs = AR.alloc("scs", [FCG, FGW], BF16)
        wblock(ccs, cc_d, FCG, FGW)
        wblock(scs, sc_d, FCG, FGW)
        maxS = max(p["S"] for p in parts)
        csb = AR.alloc("csb", [maxS // 128, T], BF16)
        ssb = AR.alloc("ssb", [maxS // 128, T], BF16)
        xf = [AR.alloc("xf%d" % i, [maxS // 128, FGW], BF16) for i in range(2)]
        abT = [AR.alloc("abT%d" % i, [2 * FCG, T], BF16) for i in range(2)]
        ystg = [AR.alloc("ystg%d" % i, [T], BF16) for i in range(4)]
        xi = [0]
        yi = [0]
        n_it = sum(p["own"] // T for p in parts) * NFG
        per_it = (len(conv_jobs) - conv_state["i"] + n_it - 1) // n_it
        for p in parts:
            NC_ = p["S"] // 128
            for kb in range(p["own"] // T):
                k0 = kb * T
                csv = wblock(csb, p["cs"][:, k0:k0 + T], NC_, T)
                ssv = wblock(ssb, p["ss"][:, k0:k0 + T], NC_, T)
                for fg in range(NFG):
                    if not cfg.get("dense_moe", False):
                        emit_conv(per_it)
                    x_ = xf[xi[0] % 2]
                    ab = abT[xi[0] % 2]
                    xi[0] += 1
                    xv = x_.ap[:, 0:NC_, :]
                    DMA("sp", xv, p["f"][:, fg * FGW:(fg + 1) * FGW].rearrange("(n p) c -> p n c", p=128), [p["b_f"]], [x_.buf], x_.buf)
                    for fc in range(FCG):
                        for which, mat, matb in ((0, csv, csb.buf), (1, ssv, ssb.buf)):
                            ps = PS()
                            for n in range(NC_):
                                MM(ps, ps.ap, xv[:, n, fc * 128:(fc + 1) * 128], mat[:, n, :], n == 0, n == NC_ - 1, [x_.buf, matb])
                            if which == 0:
                                ACT(ab.ap[:, fc, :], ps.ap, AF.Copy, [ps.buf], [ab.buf])
                            else:
                                CP(ab.ap[:, FCG + fc, :], ps.ap, [ps.buf], [ab.buf])
                    for oc in range(FCG):
                        ps = PS()
                        for c in range(2 * FCG):
                            m_ = ccs if c < FCG else scs
                            MM(ps, ps.ap, m_.ap[:, c % FCG, oc * 128:(oc + 1) * 128], ab.ap[:, c, :], c == 0, c == 2 * FCG - 1,
                               [m_.buf, ab.buf])
                        ys = ystg[yi[0] % 4]
                        yi[0] += 1
                        ACT(ys.ap, ps.ap, AF.Copy, [ps.buf], [ys.buf])
                        r0 = fg * FGW + oc * 128
                        DMA("act", p["frT"][r0:r0 + 128, k0:k0 + T], ys.ap, [ys.buf], [p["b_frT"]], ys.buf)

    def phase_C1():
        phase_reset()
        bgT = AR.alloc("bgT", [2 * KC])
        DMA("sp", bgT.ap, bgT_d, [], [bgT.buf], bgT.buf)
        hT = AR.alloc("hTc", [KC, T], BF16)
        mT = AR.alloc("mTc", [KC, T], BF16)
        oTt = AR.alloc("oTt", [AO // 128, T], BF16)
        frt = AR.alloc("frt", [FW // 128, T], BF16)
        xts = [AR.alloc("xtC%d" % i, [D]) for i in range(1)]
        junk = AR.alloc("junkC", [D], BF16)
        smalls = [AR.alloc("smallC%d" % i, [4]) for i in range(2)]
        WB = 256
        WKC = max(KC, AO // 128 + FW // 128)
        wsl = [AR.alloc("wC%d" % i, [WKC, WB], BF16) for i in range(3)]
        gts = [AR.alloc("gt%d" % i, [T]) for i in range(4)]
        tmp = [AR.alloc("tmpC%d" % i, [T]) for i in range(4)]
        xr = [AR.alloc("xr%d" % i, [WB]) for i in range(4)]
        wi = [0]
        gi = [0]
        xi = [0]
        ri = [0]

        def nextw():
            w = wsl[wi[0] % 3]
            wi[0] += 1
            return w

        NBR = AO // 128 + FW // 128
        NCB = D // WB
        wc_ga = dscr("wc_ga", [NCB * 128, KC * WB])
        wc_gf = dscr("wc_gf", [NCB * 128, KC * WB])
        wc_br = dscr("wc_br", [NCB * 128, NBR * WB])
        wc_wo = dscr("wc_wo", [NCB * 128, KC * WB])
        b_wc = {k: Buf("wc_" + k, dram=True) for k in ("ga", "gf", "br", "wo")}

        def cache_store(kind, wc, cb, slot, nch):
            DMA("sp", wc[cb * 128:(cb + 1) * 128, :], slot.ap[:, 0:nch, :].rearrange("p a b -> p (a b)"), [slot.buf], [b_wc[kind]],
                slot.buf)

        def cache_load(kind, wc, cb, slot, nch):
            v = slot.ap[:, 0:nch, :]
            DMA("pool", v, wc[cb * 128:(cb + 1) * 128, :].rearrange("p (a b) -> p a b", b=WB), [b_wc[kind]], [slot.buf], slot.buf)
            return v

        first = [True]
        for p in parts:
            for tt in range(p["own"] // T):
                t0 = tt * T
                fill = first[0]
                first[0] = False
                DMA("sp", oTt.ap, p["oT"][:, t0:t0 + T].rearrange("(c p) t -> p c t", p=128), [p["b_oT"]], [oTt.buf], oTt.buf)
                DMA("sp", frt.ap, p["frT"][:, t0:t0 + T].rearrange("(c p) t -> p c t", p=128), [p["b_frT"]], [frt.buf], frt.buf)
                for s4 in range(4):
                    xt = xts[0]
                    sm = smalls[xi[0] % 2]
                    xi[0] += 1
                    DMA("sp", xt.ap, p["x"][t0 + s4 * 128:t0 + (s4 + 1) * 128, :], [], [xt.buf], xt.buf)
                    rms_to_T(xt, hT, s4 * 128, g1T, junk, sm, xt)
                for cb in range(D // WB):
                    wga = nextw()
                    wgf = nextw()
                    wbr = nextw()
                    wbav = wbr.ap[:, 0:AO // 128, 0:WB]
                    wbfv = wbr.ap[:, AO // 128:AO // 128 + FW // 128, 0:WB]
                    if fill:
                        wgav = wblock(wga, w_in[:, 3 * AW + FW + cb * WB:3 * AW + FW + (cb + 1) * WB], KC, WB)
                        wgfv = wblock(wgf, w_in[:, 3 * AW + FW + D + cb * WB:3 * AW + FW + D + (cb + 1) * WB], KC, WB)
                        DMA("pool", wbav, w_ba[:, cb * WB:(cb + 1) * WB].rearrange("(kc p) n -> p kc n", p=128), [], [wbr.buf], wbr.buf)
                        DMA("pool", wbfv, w_bf[:, cb * WB:(cb + 1) * WB].rearrange("(kc p) n -> p kc n", p=128), [], [wbr.buf], wbr.buf)
                    else:
                        wgav = cache_load("ga", wc_ga, cb, wga, KC)
                        wgfv = cache_load("gf", wc_gf, cb, wgf, KC)
                        cache_load("br", wc_br, cb, wbr, NBR)
                    for j in range(WB // 128):
                        c = cb * (WB // 128) + j
                        cs_ = slice(j * 128, (j + 1) * 128)
                        psA, psF, psa, psf = PS(), PS(), PS(), PS()
                        for kc in range(KC):
                            MM(psA, psA.ap, wgav[:, kc, cs_], hT.ap[:, kc, :], kc == 0, kc == KC - 1, [wga.buf, hT.buf])
                        for kc in range(KC):
                            MM(psF, psF.ap, wgfv[:, kc, cs_], hT.ap[:, kc, :], kc == 0, kc == KC - 1, [wgf.buf, hT.buf])
                        na = AO // 128
                        for kc in range(na):
                            MM(psa, psa.ap, wbav[:, kc, cs_], oTt.ap[:, kc, :], kc == 0, kc == na - 1, [wbr.buf, oTt.buf])
                        nf = FW // 128
                        for kc in range(nf):
                            MM(psf, psf.ap, wbfv[:, kc, cs_], frt.ap[:, kc, :], kc == 0, kc == nf - 1, [wbr.buf, frt.buf])
                        gA = gts[gi[0] % 4]
                        gF = gts[(gi[0] + 1) % 4]
                        t1 = tmp[gi[0] % 4]
                        t2 = tmp[(gi[0] + 1) % 4]
                        gi[0] += 2
                        ACT(gA.ap, psA.ap, AF.Sigmoid, [psA.buf, bgT.buf], [gA.buf], bias=bgT.ap[:, c:c + 1])
                        ACT(gF.ap, psF.ap, AF.Sigmoid, [psF.buf, bgT.buf], [gF.buf], bias=bgT.ap[:, KC + c:KC + c + 1])
                        TT(t1.ap, gA.ap, psa.ap, ALU.mult, [gA.buf, psa.buf], [t1.buf])
                        TT(t2.ap, gF.ap, psf.ap, ALU.mult, [gF.buf, psf.buf], [t2.buf])
                        TT(mT.ap[:, c, :], t1.ap, t2.ap, ALU.add, [t1.buf, t2.buf], [mT.buf], eng="pool")
                    if fill:
                        cache_store("ga", wc_ga, cb, wga, KC)
                        cache_store("gf", wc_gf, cb, wgf, KC)
                        cache_store("br", wc_br, cb, wbr, NBR)
                for cb in range(D // WB):
                    ws = nextw()
                    if fill:
                        wv = wblock(ws, w_out[:, cb * WB:(cb + 1) * WB], KC, WB)
                    else:
                        wv = cache_load("wo", wc_wo, cb, ws, KC)
                    pst = [PS() for _ in range(4)]
                    for kc in range(KC):
                        for s4 in range(4):
                            MM(pst[s4], pst[s4].ap[:, 0:WB], mT.ap[:, kc, s4 * 128:(s4 + 1) * 128], wv[:, kc, :], kc == 0, kc == KC - 1,
                               [ws.buf, mT.buf])
                    for s4 in range(4):
                        x_ = xr[ri[0] % 4]
                        ri[0] += 1
                        rows = slice(t0 + s4 * 128, t0 + (s4 + 1) * 128)
                        DMA("sp", x_.ap, p["x"][rows, cb * WB:(cb + 1) * WB], [], [x_.buf], x_.buf)
                        TT(x_.ap, x_.ap, pst[s4].ap[:, 0:WB], ALU.add, [x_.buf, pst[s4].buf], [x_.buf])
                        DMA("sp", p["x1"][rows, cb * WB:(cb + 1) * WB], x_.ap, [x_.buf], [p["b_x1"]], x_.buf)
                    if fill:
                        cache_store("wo", wc_wo, cb, ws, KC)

    def phase_C2():
        phase_reset()
        gfb = AR.alloc("gfb", [D])
        brb = AR.alloc("brb", [NR])
        wrs = AR.alloc("wrs", [KC, NR], BF16)
        DMA("sp", gfb.ap, gf_d, [], [gfb.buf], gfb.buf)
        DMA("sp", brb.ap, br_d, [], [brb.buf], brb.buf)
        wblock(wrs, w_r, KC, NR)
        xt = AR.alloc("x1t", [4, D])
        hT = AR.alloc("h2T", [KC, T], BF16)
        xs = AR.alloc("xsD", [D])
        junk = xs
        smalls = [AR.alloc("smallD%d" % i, [4]) for i in range(2)]
        comb = AR.alloc("comb", [4, NE])
        rt = AR.alloc("rt", [4, 64])
        WB = 128
        DB = min(512, D)
        WKC = max(KC, (FFC * DB + WB - 1) // WB)
        NWS = 4
        wsl = [AR.alloc("wD%d" % i, [WKC, WB], BF16) for i in range(NWS)]
        aT = [AR.alloc("aT%d" % i, [FFC, T], BF16) for i in range(2)]
        sil = [AR.alloc("sil%d" % i, [T]) for i in range(3)]
        wi = [0]
        ai = [0]
        li = [0]
        xi = [0]

        def nextw():
            w = wsl[wi[0] % NWS]
            wi[0] += 1
            return w

        for p in parts:
            for tt in range(p["own"] // T):
                t0 = tt * T
                for s4 in range(4):
                    xv = Tile(xt.ap[:, s4, :], xt.buf)
                    sm = smalls[xi[0] % 2]
                    xi[0] += 1
                    DMA("sp", xv.ap, p["x1"][t0 + s4 * 128:t0 + (s4 + 1) * 128, :], [p["b_x1"]], [xt.buf], xt.buf)
                    rms_to_T(xv, hT, s4 * 128, g2T, junk, sm, xs)
                for s4 in range(4):
                    ps = PS()
                    for kc in range(KC):
                        MM(ps, ps.ap[:, 0:NR], hT.ap[:, kc, s4 * 128:(s4 + 1) * 128], wrs.ap[:, kc, :], kc == 0, kc == KC - 1,
                           [hT.buf, wrs.buf])
                    R = rt.ap[:, s4, :]
                    rb = [rt.buf]
                    lg = R[:, 0:NR]
                    TT(lg, ps.ap[:, 0:NR], brb.ap, ALU.add, [ps.buf, brb.buf], rb)
                    gmax = R[:, 20:21]
                    P.op("dve", lambda e, o=gmax, i=lg[:, 0:NEG]: e.reduce_max(o, i, AX.X), reads=rb, writes=rb)
                    oh = R[:, 21:25]
                    TS(oh, lg[:, 0:NEG], gmax, None, ALU.is_equal, None, rb, rb)
                    ngm = R[:, 25:26]
                    TS(ngm, gmax, -1.0, None, ALU.mult, None, rb, rb)
                    eg_ = R[:, 26:30]
                    sg_ = R[:, 30:31]
                    ACT(eg_, lg[:, 0:NEG], AF.Exp, rb, rb, bias=ngm, accum=sg_)
                    pg = R[:, 31:32]
                    P.op("dve", lambda e, o=pg, i=sg_: e.reciprocal(o, i), reads=rb, writes=rb)
                    les = R[:, 32:36]
                    TS(les, lg[:, NEG:NEG + EPG], oh[:, 0:1], None, ALU.mult, None, rb, rb)
                    for g in range(1, NEG):
                        STT(les, lg[:, NEG + g * EPG:NEG + (g + 1) * EPG], oh[:, g:g + 1], les, ALU.mult, ALU.add, rb, rb)
                    m1 = R[:, 36:37]
                    P.op("dve", lambda e, o=m1, i=les: e.reduce_max(o, i, AX.X), reads=rb, writes=rb)
                    k1 = R[:, 37:41]
                    TS(k1, les, m1, None, ALU.is_equal, None, rb, rb)
                    le2 = R[:, 41:45]
                    STT(le2, k1, -1e30, les, ALU.mult, ALU.add, rb, rb)
                    m2 = R[:, 45:46]
                    P.op("dve", lambda e, o=m2, i=le2: e.reduce_max(o, i, AX.X), reads=rb, writes=rb)
                    k2 = R[:, 46:50]
                    TS(k2, le2, m2, None, ALU.is_equal, None, rb, rb)
                    dm = R[:, 50:51]
                    TT(dm, m2, m1, ALU.subtract, rb, rb)
                    ex = R[:, 51:52]
                    ACT(ex, dm, AF.Exp, rb, rb)
                    den = R[:, 52:53]
                    TS(den, ex, 1.0, None, ALU.add, None, rb, rb)
                    p1 = R[:, 53:54]
                    P.op("dve", lambda e, o=p1, i=den: e.reciprocal(o, i), reads=rb, writes=rb)
                    p2 = R[:, 54:55]
                    TT(p2, ex, p1, ALU.mult, rb, rb)
                    TT(p1, p1, pg, ALU.mult, rb, rb)
                    TT(p2, p2, pg, ALU.mult, rb, rb)
                    cw = R[:, 55:59]
                    TS(cw, k1, p1, None, ALU.mult, None, rb, rb)
                    STT(cw, k2, p2, cw, ALU.mult, ALU.add, rb, rb)
                    for g in range(NEG):
                        TS(comb.ap[:, s4, g * EPG:(g + 1) * EPG], cw, oh[:, g:g + 1], None, ALU.mult, None, rb, [comb.buf])
                for ex_ in range(NE):
                    a_ = aT[ai[0] % 2]
                    ai[0] += 1
                    for fb in range(DFF // WB):
                        wg = nextw()
                        wgv = wblock(wg, w_eg[ex_, :, fb * WB:(fb + 1) * WB], KC, WB)
                        wu = nextw()
                        wuv = wblock(wu, w_eu[ex_, :, fb * WB:(fb + 1) * WB], KC, WB)
                        for j in range(WB // 128):
                            fc = fb * (WB // 128) + j
                            cs_ = slice(j * 128, (j + 1) * 128)
                            psG, psU = PS(), PS()
                            for kc in range(KC):
                                MM(psG, psG.ap, wgv[:, kc, cs_], hT.ap[:, kc, :], kc == 0, kc == KC - 1, [wg.buf, hT.buf])
                            for kc in range(KC):
                                MM(psU, psU.ap, wuv[:, kc, cs_], hT.ap[:, kc, :], kc == 0, kc == KC - 1, [wu.buf, hT.buf])
                            s_ = sil[li[0] % 3]
                            li[0] += 1
                            ACT(s_.ap, psG.ap, AF.Silu, [psG.buf], [s_.buf])
                            TT(a_.ap[:, fc, :], s_.ap, psU.ap, ALU.mult, [s_.buf, psU.buf], [a_.buf])
                    for cb in range(D // DB):
                        wd = nextw()
                        wdv = wd.ap.rearrange("p a b -> p (a b)")[:, 0:FFC * DB].rearrange("p (a b) -> p a b", a=FFC)
                        DMA("pool", wdv, w_ed[ex_, :, cb * DB:(cb + 1) * DB].rearrange("(kc p) n -> p kc n", p=128), [], [wd.buf], wd.buf)
                        pst = [PS() for _ in range(4)]
                        for kc in range(FFC):
                            for s4 in range(4):
                                MM(pst[s4], pst[s4].ap[:, 0:DB], a_.ap[:, kc, s4 * 128:(s4 + 1) * 128], wdv[:, kc, :], kc == 0, kc == FFC - 1,
                                   [wd.buf, a_.buf])
                        for s4 in range(4):
                            xv = xt.ap[:, s4, cb * DB:(cb + 1) * DB]
                            STT(xv, pst[s4].ap[:, 0:DB], comb.ap[:, s4, ex_:ex_ + 1], xv, ALU.mult, ALU.add, [pst[s4].buf, comb.buf, xt.buf], [xt.buf])
                for s4 in range(4):
                    sm = smalls[xi[0] % 2]
                    xi[0] += 1
                    xv = xt.ap[:, s4, :]
                    ssq, t1, t2, rstd = sm.ap[:, 0:1], sm.ap[:, 1:2], sm.ap[:, 2:3], sm.ap[:, 3:4]
                    ACT(junk.ap, xv, AF.Square, [xt.buf], [junk.buf, sm.buf], accum=ssq)
                    TS(t1, ssq, 1.0 / D, EPS, ALU.mult, ALU.add, [sm.buf], [sm.buf])
                    ACT(t2, t1, AF.Sqrt, [sm.buf], [sm.buf])
                    P.op("dve", lambda e, o=rstd, i=t2: e.reciprocal(o, i), reads=[sm.buf], writes=[sm.buf])
                    STT(xs.ap, xv, rstd, gfb.ap, ALU.mult, ALU.mult, [xt.buf, sm.buf, gfb.buf], [xs.buf])
                    DMA("sp", p["y"][t0 + s4 * 128:t0 + (s4 + 1) * 128, :], xs.ap, [xs.buf], [p["b_y"]], xs.buf)


    def IDMA(out, out_off, in_, in_off, reads, writes, slot, eoff=0):
        def emit(e):
            oo = bass.IndirectOffsetOnAxis(ap=out_off, axis=0) if out_off is not None else None
            io = bass.IndirectOffsetOnAxis(ap=in_off, axis=0) if in_off is not None else None
            return e.indirect_dma_start(out=out, out_offset=oo, in_=in_, in_offset=io, element_offset=eoff)
        P.op("pool", emit, reads=reads, writes=writes, dma=slot)

    def phase_C2_sparse():
        subt = [(p, t0) for p in parts for t0 in range(0, p["own"], 128)]
        phase_reset()
        g2b = AR.alloc("g2b", [D])
        brb = AR.alloc("brb", [NR])
        wrs = AR.alloc("wrs", [KC, NR], BF16)
        DMA("sp", g2b.ap, g2b_d, [], [g2b.buf], g2b.buf)
        DMA("sp", brb.ap, br_d, [], [brb.buf], brb.buf)
        wblock(wrs, w_r, KC, NR)
        hT = AR.alloc("h2T", [KC, T], BF16)
        xts = [AR.alloc("x1a%d" % i, [D]) for i in range(2)]
        xs = AR.alloc("xsD", [D])
        htm = [AR.alloc("htm%d" % i, [D], BF16) for i in range(2)]
        smalls = [AR.alloc("smallD%d" % i, [4]) for i in range(2)]
        rt = AR.alloc("rt", [4, 64])
        xi = [0]
        for tb_ in range(NT // 4):
            for s4 in range(4):
                i = tb_ * 4 + s4
                p, t0 = subt[i]
                xt = xts[xi[0] % 2]
                sm = smalls[xi[0] % 2]
                hm = htm[xi[0] % 2]
                xi[0] += 1
                DMA("sp", xt.ap, p["x1"][t0:t0 + 128, :], [p["b_x1"]], [xt.buf], xt.buf)
                rms_to_T(xt, hT, s4 * 128, g2T, xs, sm, xs)
                TT(hm.ap, xs.ap, g2b.ap, ALU.mult, [xs.buf, g2b.buf], [hm.buf], eng="pool")
                DMA("sp", H_d[i * 128:(i + 1) * 128, :], hm.ap, [hm.buf], [b_H], hm.buf)
            for s4 in range(4):
                i = tb_ * 4 + s4
                ps = PS()
                for kc in range(KC):
                    MM(ps, ps.ap[:, 0:NR], hT.ap[:, kc, s4 * 128:(s4 + 1) * 128], wrs.ap[:, kc, :], kc == 0, kc == KC - 1,
                       [hT.buf, wrs.buf])
                R = rt.ap[:, s4, :]
                rb = [rt.buf]
                lg = R[:, 0:NR]
                TT(lg, ps.ap[:, 0:NR], brb.ap, ALU.add, [ps.buf, brb.buf], rb)
                gmax = R[:, 20:21]
                P.op("dve", lambda e, o=gmax, i_=lg[:, 0:NEG]: e.reduce_max(o, i_, AX.X), reads=rb, writes=rb)
                oh = R[:, 21:25]
                TS(oh, lg[:, 0:NEG], gmax, None, ALU.is_equal, None, rb, rb)
                ngm = R[:, 25:26]
                TS(ngm, gmax, -1.0, None, ALU.mult, None, rb, rb)
                eg_ = R[:, 26:30]
                sg_ = R[:, 30:31]
                ACT(eg_, lg[:, 0:NEG], AF.Exp, rb, rb, bias=ngm, accum=sg_)
                pg = R[:, 31:32]
                P.op("dve", lambda e, o=pg, i_=sg_: e.reciprocal(o, i_), reads=rb, writes=rb)
                les = R[:, 32:36]
                TS(les, lg[:, NEG:NEG + EPG], oh[:, 0:1], None, ALU.mult, None, rb, rb)
                for g in range(1, NEG):
                    STT(les, lg[:, NEG + g * EPG:NEG + (g + 1) * EPG], oh[:, g:g + 1], les, ALU.mult, ALU.add, rb, rb)
                m1 = R[:, 36:37]
                P.op("dve", lambda e, o=m1, i_=les: e.reduce_max(o, i_, AX.X), reads=rb, writes=rb)
                k1 = R[:, 37:41]
                TS(k1, les, m1, None, ALU.is_equal, None, rb, rb)
                le2 = R[:, 41:45]
                STT(le2, k1, -1e30, les, ALU.mult, ALU.add, rb, rb)
                m2 = R[:, 45:46]
                P.op("dve", lambda e, o=m2, i_=le2: e.reduce_max(o, i_, AX.X), reads=rb, writes=rb)
                k2 = R[:, 46:50]
                TS(k2, le2, m2, None, ALU.is_equal, None, rb, rb)
                dm = R[:, 50:51]
                TT(dm, m2, m1, ALU.subtract, rb, rb)
                ex = R[:, 51:52]
                ACT(ex, dm, AF.Exp, rb, rb)
                den = R[:, 52:53]
                TS(den, ex, 1.0, None, ALU.add, None, rb, rb)
                p1 = R[:, 53:54]
                P.op("dve", lambda e, o=p1, i_=den: e.reciprocal(o, i_), reads=rb, writes=rb)
                p2 = R[:, 54:55]
                TT(p2, ex, p1, ALU.mult, rb, rb)
                TT(rc12.ap[:, i, 0:1], p1, pg, ALU.mult, rb, [rc12.buf])
                TT(rc12.ap[:, i, 1:2], p2, pg, ALU.mult, rb, [rc12.buf])
                for g in range(NEG):
                    TS(rm1.ap[:, i, g * EPG:(g + 1) * EPG], k1, oh[:, g:g + 1], None, ALU.mult, None, rb, [rm1.buf])
                    TS(rm2.ap[:, i, g * EPG:(g + 1) * EPG], k2, oh[:, g:g + 1], None, ALU.mult, None, rb, [rm2.buf])
        Mbf = AR.alloc("Mbf", [NT, NE], BF16)
        onesb = AR.alloc("onesb", [128], BF16)
        trib = AR.alloc("trib", [128], BF16)
        Cs = AR.alloc("Cs", [NT, NE])
        posf = AR.alloc("posf", [NT, NE])
        prod = AR.alloc("prod", [NT, NE])
        pf12 = AR.alloc("pf12", [2, NT])
        tb = AR.alloc("tb", [96])
        jid = AR.alloc("jid", [JMAX])
        eacc = AR.alloc("eacc", [JMAX])
        DMA("pool", trib.ap, tri_d, [], [trib.buf], trib.buf)
        DMA("sp", jid.ap, jidx_d, [], [jid.buf], jid.buf)
        P.op("dve", lambda e: e.memset(onesb.ap, 1.0), writes=[onesb.buf])
        TT(Mbf.ap, rm1.ap, rm2.ap, ALU.add, [rm1.buf, rm2.buf], [Mbf.buf])
        for i in range(NT):
            ps = PS()
            for i2 in range(i):
                MM(ps, ps.ap[:, 0:NE], onesb.ap, Mbf.ap[:, i2, :], i2 == 0, False, [onesb.buf, Mbf.buf])
            MM(ps, ps.ap[:, 0:NE], trib.ap, Mbf.ap[:, i, :], i == 0, True, [trib.buf, Mbf.buf])
            CP(Cs.ap[:, i, :], ps.ap[:, 0:NE], [ps.buf], [Cs.buf])
        ps = PS()
        for i in range(NT):
            MM(ps, ps.ap[:, 0:NE], onesb.ap, Mbf.ap[:, i, :], i == 0, i == NT - 1, [onesb.buf, Mbf.buf])
        tbb = [tb.buf]
        n_ = tb.ap[:, 0:16]
        ntl = tb.ap[:, 16:32]
        cum = tb.ap[:, 32:48]
        bm1 = tb.ap[:, 48:64]
        tmp_ = tb.ap[:, 64:80]
        CP(n_, ps.ap[:, 0:NE], [ps.buf], tbb)
        P.op("dve", lambda e: e.memset(ntl, 0.0), reads=tbb, writes=tbb)
        for j in range(NTOK // TB):
            STT(ntl, n_, float(j * TB), ntl, ALU.is_gt, ALU.add, tbb, tbb)
        CP(cum[:, 0:1], ntl[:, 0:1], tbb, tbb)
        for e_ in range(1, NE):
            TT(cum[:, e_:e_ + 1], cum[:, e_ - 1:e_], ntl[:, e_:e_ + 1], ALU.add, tbb, tbb)
        TT(tmp_, cum, ntl, ALU.subtract, tbb, tbb)
        TS(bm1, tmp_, float(TB), -1.0, ALU.mult, ALU.add, tbb, tbb)
        TT(posf.ap, Cs.ap, bm1.unsqueeze(1).to_broadcast([128, NT, NE]), ALU.add, [Cs.buf] + tbb, [posf.buf])
        for k, rm in ((0, rm1), (1, rm2)):
            TT(prod.ap, posf.ap, rm.ap, ALU.mult, [posf.buf, rm.buf], [prod.buf])
            P.op("dve", lambda e, o=pf12.ap[:, k, :], i_=prod.ap: e.reduce_sum(o, i_, AX.X), reads=[prod.buf], writes=[pf12.buf])
            CP(rpos.ap[:, :, k], pf12.ap[:, k, :], [pf12.buf], [rpos.buf])
        P.op("dve", lambda e: e.memset(eacc.ap, 0.0), writes=[eacc.buf])
        for e_ in range(NE):
            STT(eacc.ap, jid.ap, cum[:, e_:e_ + 1], eacc.ap, ALU.is_ge, ALU.add, [jid.buf, eacc.buf] + tbb, [eacc.buf])
        TS(eacc.ap, eacc.ap, float(NE - 1), None, ALU.min, None, [eacc.buf], [eacc.buf])
        pidx = AR.alloc("pidx", [1])
        DMA("sp", pidx.ap, pidx_d, [], [pidx.buf], pidx.buf)
        TS(eacc.ap, eacc.ap, 128.0, pidx.ap[:, 0:1], ALU.mult, ALU.add, [eacc.buf, pidx.buf], [eacc.buf])
        CP(reid.ap, eacc.ap, [eacc.buf], [reid.buf])
        phase_reset()
        hb = [AR.alloc("hb%d" % i, [D], BF16) for i in range(2)]
        hbsem = [Buf("hbsem%d" % i) for i in range(2)]
        zt = AR.alloc("zt", [D], BF16)
        b_Hs0 = Buf("Hs0", dram=True)
        P.op("dve", lambda e: e.memset(zt.ap, 0.0), writes=[zt.buf])
        for r in range(NS // 128):
            DMA("sp", Hs_d[r * 128:(r + 1) * 128, :], zt.ap, [zt.buf], [b_Hs0], zt.buf)
        for i in range(NT):
            h = hb[i % 2]
            DMA("sp", h.ap, H_d[i * 128:(i + 1) * 128, :], [b_H], [h.buf], h.buf)
            for k in range(2):
                IDMA(Hs_d[:, :], rpos.ap[:, i, k:k + 1], h.ap, None, [h.buf, rpos.buf, b_Hs0], [b_Hs], hbsem[i % 2])
        NSUB = TB // 128
        hstm = [AR.alloc("hstm%d" % i, [NSUB, D], BF16) for i in range(2)]
        hsT = [AR.alloc("hsT%d" % i, [KC, TB], BF16) for i in range(2)]
        aT = [AR.alloc("aTs%d" % i, [FFC, TB], BF16) for i in range(2)]
        sil = [AR.alloc("sils%d" % i, [TB]) for i in range(3)]
        DB = min(512, D)
        WB = 128
        WKC = max(KC, (FFC * DB + WB - 1) // WB)
        NWS = 6
        wsl = [AR.alloc("wS%d" % i, [WKC, WB], BF16) for i in range(NWS)]
        ystg = [AR.alloc("ystg%d" % i, [DB]) for i in range(4)]
        identb = AR.alloc("identb", [128], BF16)
        CP(identb.ap, ident.ap, [ident.buf], [identb.buf])
        wi = [0]
        li = [0]
        yi = [0]
        regs = {}

        def wdyn(j, wb2, blk, n, view, slot):
            rowlen = wb2.shape[1]
            v2 = slot.ap.rearrange("p a b -> p (a b)")[:, 0:rowlen]
            IDMA(v2, None, wb2, reid.ap[:, j:j + 1], [reid.buf, b_wb], [slot.buf], slot.buf, eoff=blk * NE * 128 * rowlen)

        for j in range(JMAX):
            hm = hstm[j % 2]
            hT_ = hsT[j % 2]
            a_ = aT[j % 2]
            DMA("sp", hm.ap, Hs_d[j * TB:(j + 1) * TB, :].rearrange("(s p) d -> p s d", p=128), [b_Hs], [hm.buf], hm.buf)
            for st_ in range(NSUB):
                for kg in range(KC // 4):
                    ps = PS()
                    psb = ps.ap.bitcast(BF16)
                    for jj in range(4):
                        kc = kg * 4 + jj
                        TR(ps, psb[:, jj * 128:(jj + 1) * 128], hm.ap[:, st_, kc * 128:(kc + 1) * 128], identb.ap, [hm.buf, identb.buf])
                    src = psb[:, 0:512].rearrange("p (a b) -> p a b", a=4)
                    dst = hT_.ap[:, kg * 4:kg * 4 + 4, st_ * 128:(st_ + 1) * 128]
                    if kg % 2 == 0:
                        CP(dst, src, [ps.buf], [hT_.buf])
                    else:
                        ACT(dst, src, AF.Copy, [ps.buf], [hT_.buf])
            for fc in range(FFC):
                wg = wsl[wi[0] % NWS]
                wi[0] += 1
                wgv = wg.ap.rearrange("p a b -> p (a b)")[:, 0:KC * 128].rearrange("p (a b) -> p a b", a=KC)
                wdyn(j, wb_eg, fc, 128, wgv, wg)
                wu = wsl[wi[0] % NWS]
                wi[0] += 1
                wuv = wu.ap.rearrange("p a b -> p (a b)")[:, 0:KC * 128].rearrange("p (a b) -> p a b", a=KC)
                wdyn(j, wb_eu, fc, 128, wuv, wu)
                psG, psU = PS(), PS()
                for kc in range(KC):
                    MM(psG, psG.ap[:, 0:TB], wgv[:, kc, :], hT_.ap[:, kc, :], kc == 0, kc == KC - 1, [wg.buf, hT_.buf])
                for kc in range(KC):
                    MM(psU, psU.ap[:, 0:TB], wuv[:, kc, :], hT_.ap[:, kc, :], kc == 0, kc == KC - 1, [wu.buf, hT_.buf])
                s_ = sil[li[0] % 3]
                li[0] += 1
                ACT(s_.ap, psG.ap[:, 0:TB], AF.Silu, [psG.buf], [s_.buf])
                TT(a_.ap[:, fc, :], s_.ap, psU.ap[:, 0:TB], ALU.mult, [s_.buf, psU.buf], [a_.buf])
            for cb in range(D // DB):
                wd = wsl[wi[0] % NWS]
                wi[0] += 1
                wdv = wd.ap.rearrange("p a b -> p (a b)")[:, 0:FFC * DB].rearrange("p (a b) -> p a b", a=FFC)
                wdyn(j, wb_ed, cb, DB, wdv, wd)
                pst = [PS() for _ in range(NSUB)]
                for kc in range(FFC):
                    for st_ in range(NSUB):
                        MM(pst[st_], pst[st_].ap[:, 0:DB], a_.ap[:, kc, st_ * 128:(st_ + 1) * 128], wdv[:, kc, :], kc == 0, kc == FFC - 1,
                           [wd.buf, a_.buf])
                for st_ in range(NSUB):
                    ys = ystg[yi[0] % 4]
                    yi[0] += 1
                    if st_ % 2 == 0:
                        ACT(ys.ap, pst[st_].ap[:, 0:DB], AF.Copy, [pst[st_].buf], [ys.buf])
                    else:
                        CP(ys.ap, pst[st_].ap[:, 0:DB], [pst[st_].buf], [ys.buf])
                    r0 = j * TB + st_ * 128
                    DMA("sp", Ys_d[r0:r0 + 128, cb * DB:(cb + 1) * DB], ys.ap, [ys.buf], [b_Ys], ys.buf)
        phase_reset()
        gfb = AR.alloc("gfb", [D])
        DMA("sp", gfb.ap, gf_d, [], [gfb.buf], gfb.buf)
        x1b = [AR.alloc("x1e%d" % i, [D]) for i in range(2)]
        yab = [AR.alloc("ya%d" % i, [D]) for i in range(2)]
        ybb = [AR.alloc("yb%d" % i, [D]) for i in range(2)]
        xo = [AR.alloc("xo%d" % i, [D]) for i in range(2)]
        smalls = [AR.alloc("smallE%d" % i, [4]) for i in range(2)]
        for i in range(NT):
            p, t0 = subt[i]
            x = x1b[i % 2]
            ya, yb, o_ = yab[i % 2], ybb[i % 2], xo[i % 2]
            sm = smalls[i % 2]
            DMA("sp", x.ap, p["x1"][t0:t0 + 128, :], [p["b_x1"]], [x.buf], x.buf)
            IDMA(ya.ap, None, Ys_d[:, :], rpos.ap[:, i, 0:1], [b_Ys, rpos.buf], [ya.buf], ya.buf)
            IDMA(yb.ap, None, Ys_d[:, :], rpos.ap[:, i, 1:2], [b_Ys, rpos.buf], [yb.buf], yb.buf)
            STT(x.ap, ya.ap, rc12.ap[:, i, 0:1], x.ap, ALU.mult, ALU.add, [ya.buf, rc12.buf, x.buf], [x.buf])
            STT(x.ap, yb.ap, rc12.ap[:, i, 1:2], x.ap, ALU.mult, ALU.add, [yb.buf, rc12.buf, x.buf], [x.buf])
            ssq, t1, t2, rstd = sm.ap[:, 0:1], sm.ap[:, 1:2], sm.ap[:, 2:3], sm.ap[:, 3:4]
            ACT(o_.ap, x.ap, AF.Square, [x.buf], [o_.buf, sm.buf], accum=ssq)
            TS(t1, ssq, 1.0 / D, EPS, ALU.mult, ALU.add, [sm.buf], [sm.buf])
            ACT(t2, t1, AF.Sqrt, [sm.buf], [sm.buf])
            P.op("dve", lambda e, o=rstd, i_=t2: e.reciprocal(o, i_), reads=[sm.buf], writes=[sm.buf])
            STT(o_.ap, x.ap, rstd, gfb.ap, ALU.mult, ALU.mult, [x.buf, sm.buf, gfb.buf], [o_.buf])
            DMA("sp", p["y"][t0:t0 + 128, :], o_.ap, [o_.buf], [p["b_y"]], o_.buf)

    phase_A()
    phase_B1()
    phase_B2()
    phase_C1()
    if cfg.get("dense_moe", False):
        phase_C2()
    else:
        phase_C2_sparse()
    P.op("sp", lambda e: e.nop(), reads=[p["b_y"] for p in parts])
    P.emit_all(nc)
    st.close()
    return nc


def _slopes(HPG):
    nh = NGRP * HPG
    s = 2.0 ** (-8.0 * np.arange(1, nh + 1) / nh)
    return s.astype(np.float32).reshape(NGRP, HPG)


def _const_tables(cfg):
    HPG, FGW = cfg["HPG"], cfg["FGW"]
    sl = _slopes(HPG).astype(np.float64)
    a = np.arange(128)[:, None]
    b = np.arange(128)[None, :]
    etA = np.zeros((128, NGRP * HPG, 128), np.float32)
    etB = np.zeros((128, NGRP * HPG, 128), np.float32)
    for g in range(NGRP):
        for h in range(HPG):
            s = np.float64(np.float32(sl[g, h])) * DIL[g]
            ea = np.where(a >= b, np.exp(-s * np.abs(a - b - 64)), 0.0)
            eb = np.where(a <= b, np.exp(-s * np.abs(a - b + 64)), 0.0)
            etA[:, g * HPG + h, :] = ea
            etB[:, g * HPG + h, :] = eb
    etA0 = np.ascontiguousarray(etA[64:128])
    c = np.arange(FGW)
    ang = 2.0 * np.pi * np.outer(c, c) / FGW
    cc = (np.cos(ang) / np.sqrt(FGW)).astype(np.float32)
    scn = (-np.sin(ang) / np.sqrt(FGW)).astype(np.float32)
    return dict(etA=etA, etB=etB, etA0=etA0, cc=cc, scn=scn, ident=np.eye(128, dtype=np.float32))


def _dft_local(S, own, half):
    loc = np.arange(S, dtype=np.int64)
    glob = loc if half == 0 else S - 1 - loc
    prod = np.outer(glob, glob[:own]) % S
    ang = 2.0 * np.pi * prod / S
    return (np.cos(ang) / np.sqrt(S)).astype(np.float32), (np.sin(ang) / np.sqrt(S)).astype(np.float32)


def _relayout_up(w):
    ne, d, dff = w.shape
    kc, ffc = d // 128, dff // 128
    r = w.reshape(ne, kc, 128, ffc, 128).transpose(3, 0, 2, 1, 4)
    return np.ascontiguousarray(r).reshape(ffc, ne * 128, kc * 128)


def _relayout_down(w):
    ne, dff, d = w.shape
    ffc = dff // 128
    db = min(512, d)
    r = w.reshape(ne, ffc, 128, d // db, db).transpose(3, 0, 2, 1, 4)
    return np.ascontiguousarray(r).reshape(d // db, ne * 128, ffc * db)


def _run(cfg, x_prompt, x_sample, attn_norm_g, w_in, w_branch_attn, w_branch_fourier, b_gate, w_out, ffn_norm_g,
         w_router_group, b_router_group, w_router_expert, b_router_expert, w_expert_gate, w_expert_up, w_expert_down,
         final_norm_g, n_cores=8):
    D = cfg["D"]
    KC = D // 128
    f = lambda a: np.ascontiguousarray(np.asarray(a, dtype=np.float32))
    x_prompt, x_sample = f(x_prompt), f(x_sample)
    shared = dict(
        w_in=f(w_in)[0], w_ba=f(w_branch_attn)[0], w_bf=f(w_branch_fourier)[0], w_out=f(w_out)[0],
        w_r=np.ascontiguousarray(np.concatenate(
            [f(w_router_group)[0], f(w_router_expert)[0].transpose(1, 0, 2).reshape(D, NE)], axis=1)),
        w_eg=_relayout_up(f(w_expert_gate)[0]), w_eu=_relayout_up(f(w_expert_up)[0]),
        w_ed=_relayout_down(f(w_expert_down)[0]),
        pidx=np.arange(128, dtype=np.float32).reshape(128, 1),
        g1T=np.ascontiguousarray(f(attn_norm_g)[0].reshape(KC, 128).T),
        g2T=np.ascontiguousarray(f(ffn_norm_g)[0].reshape(KC, 128).T),
        bgT=np.ascontiguousarray(f(b_gate)[0].reshape(2 * KC, 128).T),
        gf_b=np.ascontiguousarray(np.broadcast_to(f(final_norm_g)[None, :], (128, D))),
        br_b=np.ascontiguousarray(np.broadcast_to(
            np.concatenate([f(b_router_group)[0], f(b_router_expert)[0].reshape(NE)])[None, :], (128, NR))),
    )
    shared.update(_const_tables(cfg))
    TB = cfg.get("TB", 256)
    ntok = (cfg["S_S"] + cfg["S_P"]) // 2
    jmax = 2 * ntok // TB + NE
    shared["g2_b"] = np.ascontiguousarray(np.broadcast_to(f(ffn_norm_g)[0][None, :], (128, D)))
    shared["jidx"] = np.ascontiguousarray(np.broadcast_to(np.arange(jmax, dtype=np.float32)[None, :], (128, jmax)))
    shared["tri"] = np.triu(np.ones((128, 128), np.float32))
    dft = {}
    for nm, S in (("s", cfg["S_S"]), ("p", cfg["S_P"])):
        for half in (0, 1):
            dft[(nm, half)] = _dft_local(S, S // 2, half)
    in_maps = []
    for c in range(n_cores):
        b, half = c // 2, c % 2
        m = dict(shared)
        xs = x_sample[b] if half == 0 else x_sample[b, ::-1]
        xp = x_prompt[b] if half == 0 else x_prompt[b, ::-1]
        m["x_s"] = np.ascontiguousarray(xs)
        m["x_p"] = np.ascontiguousarray(xp)
        m["cs_s"], m["ss_s"] = dft[("s", half)]
        m["cs_p"], m["ss_p"] = dft[("p", half)]
        in_maps.append(m)
    nc = build_nc(cfg)
    res = run_bass_kernel_spmd(nc, in_maps, core_ids=list(range(n_cores)))
    nb = n_cores // 2
    y_p = np.zeros((nb, cfg["S_P"], D), np.float32)
    y_s = np.zeros((nb, cfg["S_S"], D), np.float32)
    for c in range(n_cores):
        b, half = c // 2, c % 2
        r = res.results[c]
        for nm, S, dst in (("p", cfg["S_P"], y_p), ("s", cfg["S_S"], y_s)):
            o = np.asarray(r["y_" + nm], dtype=np.float32)
            if half == 0:
                dst[b, :S // 2] = o
            else:
                dst[b, S // 2:] = o[::-1]
    return y_p, y_s


def kernel(**inputs):
    return _run(FULL_CFG, **inputs)
```

```python
import math
from contextlib import ExitStack

import numpy as np
import concourse.bass as bass
import concourse.mybir as mybir
from concourse.bass_utils import run_bass_kernel_spmd

F32 = mybir.dt.float32
BF16 = mybir.dt.bfloat16
I32 = mybir.dt.int32
AF = mybir.ActivationFunctionType
ALU = mybir.AluOpType
AX = mybir.AxisListType

FULL_CFG = dict(D=4096, HPG=8, FGW=512, DFF=1024, S_S=4096, S_P=2048)
NGRP = 3
DIL = (1, 4, 16)
NFG = 4
NEG = 4
EPG = 4
NE = NEG * EPG
NR = NEG + NE
T = 512
EPS = 1e-6
ARENA_WORDS = 48 * 1024


class Buf:
    __slots__ = ("name", "dram", "w", "r", "cnt", "sid")

    def __init__(self, name, dram=False):
        self.name = name
        self.dram = dram
        self.w = {}
        self.r = {}
        self.cnt = 0
        self.sid = None


def _merge(dst, src):
    for k, v in src.items():
        if dst.get(k, -1) < v:
            dst[k] = v


class Op:
    __slots__ = ("emit", "deps", "seq", "dma")


class Prog:
    ENGS = ("pe", "act", "dve", "pool", "sp")

    def __init__(self):
        self.q = {e: [] for e in self.ENGS}
        self.needed = {e: set() for e in self.ENGS}
        self.barrier_deps = {}
        self.slots = []
        self.last_compute = {}

    def op(self, eng, emit, reads=(), writes=(), dma=None):
        o = Op()
        o.emit = emit
        deps = dict(self.barrier_deps)
        for b in reads:
            _merge(deps, b.w)
        for b in writes:
            _merge(deps, b.w)
            _merge(deps, b.r)
        o.seq = len(self.q[eng])
        if dma is not None:
            if dma.sid is None:
                dma.sid = len(self.slots)
                self.slots.append(dma)
            dma.cnt += 1
            key, val = ("dma", dma.sid), dma.cnt * 16
            o.dma = dma.sid
        else:
            key, val = ("eng", eng), o.seq
            o.dma = None
            self.last_compute[eng] = o.seq
        for k, v in deps.items():
            if k[0] == "eng" and not (k[1] == "pe" and eng == "pe"):
                self.needed[k[1]].add(v)
        for b in reads:
            if not b.dram and b.r.get(key, -1) < val:
                b.r[key] = val
        for b in writes:
            if b.dram:
                if b.w.get(key, -1) < val:
                    b.w[key] = val
            elif b.r:
                b.w = {key: val}
                b.r = {}
            else:
                if b.w.get(key, -1) < val:
                    b.w[key] = val
        o.deps = deps
        self.q[eng].append(o)
        return o

    def barrier(self):
        d = {}
        for e, v in self.last_compute.items():
            d[("eng", e)] = v
        for s in self.slots:
            d[("dma", s.sid)] = s.cnt * 16
        self.barrier_deps = d

    def emit_all(self, nc):
        ranks = {}
        for e in self.ENGS:
            ranks[e] = {s: i + 1 for i, s in enumerate(sorted(self.needed[e]))}
        with ExitStack() as st:
            esem = {e: st.enter_context(nc.semaphore("sem_" + e)) for e in self.ENGS}
            dsem = [st.enter_context(nc.semaphore("dsem%d" % i)) for i in range(len(self.slots))]
            block = st.enter_context(nc.Block())

            def run(ename, eng):
                known = {}
                for o in self.q[ename]:
                    for k, v in o.deps.items():
                        if k[0] == "eng":
                            if k[1] == "pe" and ename == "pe":
                                continue
                            sem, val = esem[k[1]], ranks[k[1]][v]
                        else:
                            sem, val = dsem[k[1]], v
                        if known.get(k, -1) >= val:
                            continue
                        eng.wait_ge(sem, val)
                        known[k] = val
                    inst = o.emit(eng)
                    if o.dma is not None:
                        inst.then_inc(dsem[o.dma], 16)
                    elif o.seq in ranks[ename]:
                        inst.then_inc(esem[ename], 1)

            @block.tensor
            def _(e):
                run("pe", e)

            @block.scalar
            def _(e):
                run("act", e)

            @block.vector
            def _(e):
                run("dve", e)

            @block.gpsimd
            def _(e):
                run("pool", e)

            @block.sync
            def _(e):
                run("sp", e)


class Tile:
    __slots__ = ("ap", "buf")

    def __init__(self, ap, buf):
        self.ap = ap
        self.buf = buf


class Arena:
    def __init__(self, ap, nwords):
        self.base = ap
        self.n = nwords
        self.off = 0

    def reset(self):
        self.off = 0

    def alloc(self, name, free_shape, dtype=F32):
        n = 1
        for s in free_shape:
            n *= s
        words = n if dtype in (F32, I32) else (n + 1) // 2
        words = (words + 7) // 8 * 8
        assert self.off + words <= self.n, "SBUF arena overflow at %s: %d + %d > %d" % (name, self.off, words, self.n)
        a = self.base[:, self.off:self.off + words]
        self.off += words
        if dtype != F32:
            a = a.bitcast(dtype)
        a = a[:, 0:n]
        if len(free_shape) == 2:
            a = a.rearrange("p (a b) -> p a b", a=free_shape[0])
        elif len(free_shape) == 3:
            a = a.rearrange("p (a b c) -> p a b c", a=free_shape[0], b=free_shape[1])
        return Tile(a, Buf(name))


def build_nc(cfg):
    D, HPG, FGW, DFF = cfg["D"], cfg["HPG"], cfg["FGW"], cfg["DFF"]
    KC = D // 128
    AW = NGRP * HPG * 128
    GW = HPG * 128
    AO = HPG * 128
    FW = NFG * FGW
    FCG = FGW // 128
    IN_W = 3 * AW + FW + 2 * D
    FFC = DFF // 128
    parts = [dict(name="s", S=cfg["S_S"], own=cfg["S_S"] // 2), dict(name="p", S=cfg["S_P"], own=cfg["S_P"] // 2)]
    for p in parts:
        p["ext"] = p["own"] + 1024
        assert p["ext"] <= p["S"]
    scale = 1.0 / math.sqrt(128.0)

    nc = bass.Bass("TRN2", target_bir_lowering=False)

    def din(name, shape, dt=F32):
        return nc.dram_tensor(name, list(shape), dt, kind="ExternalInput").ap()

    def dscr(name, shape, dt=BF16):
        return nc.dram_tensor(name, list(shape), dt, kind="Internal").ap()

    for p in parts:
        n = p["name"]
        p["x"] = din("x_" + n, [p["S"], D])
        p["cs"] = din("cs_" + n, [p["S"], p["own"]])
        p["ss"] = din("ss_" + n, [p["S"], p["own"]])
        p["y"] = nc.dram_tensor("y_" + n, [p["own"], D], F32, kind="ExternalOutput").ap()
        p["qT"] = dscr("qT_" + n, [AW, p["own"]])
        p["kT"] = dscr("kT_" + n, [AW, p["ext"]])
        p["v"] = dscr("v_" + n, [p["ext"], AW])
        p["f"] = dscr("f_" + n, [p["S"], FW])
        p["oT"] = dscr("oT_" + n, [AO, p["own"]])
        p["frT"] = dscr("frT_" + n, [FW, p["own"]])
        p["x1"] = dscr("x1_" + n, [p["own"], D], F32)
        for k in ("qT", "kT", "v", "f", "oT", "frT", "x1", "y"):
            p["b_" + k] = Buf(k + "_" + n, dram=True)
    w_in = din("w_in", [D, IN_W])
    w_ba = din("w_ba", [AO, D])
    w_bf = din("w_bf", [FW, D])
    w_out = din("w_out", [D, D])
    w_r = din("w_r", [D, NR])
    DBX = min(512, D)
    w_eg = din("w_eg", [FFC, NE * 128, KC * 128])
    w_eu = din("w_eu", [FFC, NE * 128, KC * 128])
    w_ed = din("w_ed", [D // DBX, NE * 128, FFC * DBX])
    pidx_d = din("pidx", [128, 1])
    wb_eg = dscr("wb_eg", [FFC * NE * 128, KC * 128])
    wb_eu = dscr("wb_eu", [FFC * NE * 128, KC * 128])
    wb_ed = dscr("wb_ed", [(D // DBX) * NE * 128, FFC * DBX])
    b_wb = Buf("wb", dram=True)
    conv_jobs = []
    for (src3, dst2) in ((w_eg, wb_eg), (w_eu, wb_eu), (w_ed, wb_ed)):
        src2 = src3.rearrange("f r n -> (f r) n")
        for r0 in range(0, dst2.shape[0], 128):
            conv_jobs.append((src2, dst2, r0))
    conv_state = dict(i=0, slots=None)

    def emit_conv(n):
        for _ in range(n):
            if conv_state["i"] >= len(conv_jobs):
                return
            src2, dst2, r0 = conv_jobs[conv_state["i"]]
            sem = conv_state["ssem"][conv_state["i"] % len(conv_state["ssem"])]
            conv_state["i"] += 1
            DMA("pool", dst2[r0:r0 + 128, :], src2[r0:r0 + 128, :], [], [b_wb], sem)
    g1T_d = din("g1T", [128, KC])
    g2T_d = din("g2T", [128, KC])
    bgT_d = din("bgT", [128, 2 * KC])
    gf_d = din("gf_b", [128, D])
    br_d = din("br_b", [128, NR])
    ident_d = din("ident", [128, 128])
    cc_d = din("cc", [FGW, FGW])
    sc_d = din("scn", [FGW, FGW])
    ea_d = din("etA", [128, NGRP * HPG, 128])
    eb_d = din("etB", [128, NGRP * HPG, 128])
    ea0_d = din("etA0", [64, NGRP * HPG, 128])
    TB = cfg.get("TB", 256)
    NTOK = sum(p["own"] for p in parts)
    NT = NTOK // 128
    JMAX = 2 * NTOK // TB + NE
    NS = JMAX * TB
    g2b_d = din("g2_b", [128, D])
    jidx_d = din("jidx", [128, JMAX])
    tri_d = din("tri", [128, 128])
    H_d = dscr("H_scr", [NTOK, D])
    Hs_d = dscr("Hs_scr", [NS, D])
    Ys_d = dscr("Ys_scr", [NS, D], F32)
    b_H, b_Hs, b_Ys = Buf("H", dram=True), Buf("Hs", dram=True), Buf("Ys", dram=True)

    P = Prog()
    st = ExitStack()
    arena_t = st.enter_context(nc.sbuf_tensor("arena", [128, ARENA_WORDS], F32))
    AR = Arena(arena_t[:], ARENA_WORDS)
    pss = []
    for i in range(8):
        pt = st.enter_context(nc.psum_tensor("ps%d" % i, [128, 512], F32))
        pss.append(Tile(pt[:], Buf("ps%d" % i)))
    psi = [0]

    def PS():
        t = pss[psi[0] % 8]
        psi[0] += 1
        return t

    def MM(ps, out, lhsT, rhs, start, stop, reads):
        P.op("pe", lambda e: e.matmul(out, lhsT, rhs, start=start, stop=stop), reads=reads, writes=[ps.buf])

    def TR(ps, out, in_, ident, reads):
        P.op("pe", lambda e: e.transpose(out, in_, ident), reads=reads, writes=[ps.buf])

    def DMA(q, out, in_, reads, writes, slot):
        P.op(q, lambda e: e.dma_start(out=out, in_=in_), reads=reads, writes=writes, dma=slot)

    def ACT(out, in_, func, reads, writes, bias=None, scale=None, accum=None):
        kw = {}
        if bias is not None:
            kw["bias"] = bias
        if scale is not None:
            kw["scale"] = scale
        if accum is not None:
            kw["accum_out"] = accum
        P.op("act", lambda e: e.activation(out, in_, func, **kw), reads=reads, writes=writes)

    def TT(out, in0, in1, op, reads, writes, eng="dve"):
        P.op(eng, lambda e: e.tensor_tensor(out, in0, in1, op), reads=reads, writes=writes)

    def TS(out, in0, s1, s2, op0, op1, reads, writes, eng="dve"):
        if op1 is None:
            P.op(eng, lambda e: e.tensor_scalar(out, in0, s1, None, op0), reads=reads, writes=writes)
        else:
            P.op(eng, lambda e: e.tensor_scalar(out, in0, s1, s2, op0, op1), reads=reads, writes=writes)

    def STT(out, in0, scalar, in1, op0, op1, reads, writes, eng="dve"):
        P.op(eng, lambda e: e.scalar_tensor_tensor(out, in0, scalar, in1, op0, op1), reads=reads, writes=writes)

    def CP(out, in_, reads, writes, eng="dve"):
        P.op(eng, lambda e: e.tensor_copy(out, in_), reads=reads, writes=writes)

    def wblock(slot, src2d, kchunks, ncols):
        v = slot.ap[:, 0:kchunks, 0:ncols]
        DMA("pool", v, src2d.rearrange("(kc p) n -> p kc n", p=128), [], [slot.buf], slot.buf)
        return v

    def rms_to_T(xt, dstT, col0, gT, junk, small, xs):
        ssq = small.ap[:, 0:1]
        t1 = small.ap[:, 1:2]
        t2 = small.ap[:, 2:3]
        rstd = small.ap[:, 3:4]
        ACT(junk.ap, xt.ap, AF.Square, [xt.buf], [junk.buf, small.buf], accum=ssq)
        TS(t1, ssq, 1.0 / D, EPS, ALU.mult, ALU.add, [small.buf], [small.buf])
        ACT(t2, t1, AF.Sqrt, [small.buf], [small.buf])
        P.op("dve", lambda e: e.reciprocal(rstd, t2), reads=[small.buf], writes=[small.buf])
        TS(xs.ap, xt.ap, rstd, None, ALU.mult, None, [xt.buf, small.buf], [xs.buf])
        for kg in range(KC // 4):
            ps = PS()
            for j in range(4):
                kc = kg * 4 + j
                TR(ps, ps.ap[:, j * 128:(j + 1) * 128], xs.ap[:, kc * 128:(kc + 1) * 128], ident.ap, [xs.buf, ident.buf])
            gb = gT.ap[:, kg * 4:kg * 4 + 4].unsqueeze(2).to_broadcast([128, 4, 128])
            TT(dstT.ap[:, kg * 4:kg * 4 + 4, col0:col0 + 128], ps.ap.rearrange("p (a b) -> p a b", a=4), gb, ALU.mult,
               [ps.buf, gT.buf], [dstT.buf])

    ident = AR.alloc("ident", [128])
    g1T = AR.alloc("g1T", [KC])
    g2T = AR.alloc("g2T", [KC])
    DMA("sp", ident.ap, ident_d, [], [ident.buf], ident.buf)
    DMA("sp", g1T.ap, g1T_d, [], [g1T.buf], g1T.buf)
    DMA("sp", g2T.ap, g2T_d, [], [g2T.buf], g2T.buf)
    rm1 = AR.alloc("rm1", [NT, NE])
    rm2 = AR.alloc("rm2", [NT, NE])
    rc12 = AR.alloc("rc12", [NT, 2])
    rpos = AR.alloc("rpos", [NT, 2], I32)
    reid = AR.alloc("reid", [JMAX], I32)
    const_off = AR.off

    def phase_reset():
        P.barrier()
        AR.off = const_off

    def phase_A():
        phase_reset()
        hT = AR.alloc("hT", [KC, T], BF16)
        xts = [AR.alloc("xtA%d" % i, [D]) for i in range(2)]
        xs = AR.alloc("xsA", [D])
        junk = AR.alloc("junkA", [D], BF16)
        smalls = [AR.alloc("smallA%d" % i, [4]) for i in range(2)]
        wsl = [AR.alloc("wA%d" % i, [KC, 512], BF16) for i in range(2)]
        stg = [AR.alloc("stgA%d" % i, [512], BF16) for i in range(4)]
        wi = [0]
        si = [0]
        xi = [0]
        cacheA = {}
        b_wcA = Buf("wcA", dram=True)
        conv_state["ssem"] = [Buf("cvs%d" % i) for i in range(4)]
        nconvA = 0 if cfg.get("dense_moe", False) else len(conv_jobs) // 4
        bcount = [0]
        for p in parts:
            ntile = p["S"] // T
            for tt in range(ntile):
                t0 = tt * T
                if t0 < p["own"]:
                    kind = "own"
                elif t0 < p["ext"]:
                    kind = "halo"
                else:
                    kind = "far"
                for s4 in range(4):
                    xt = xts[xi[0] % 2]
                    sm = smalls[xi[0] % 2]
                    xi[0] += 1
                    DMA("sp", xt.ap, p["x"][t0 + s4 * 128:t0 + (s4 + 1) * 128, :], [], [xt.buf], xt.buf)
                    rms_to_T(xt, hT, s4 * 128, g1T, junk, sm, xs)
                secs = []
                if kind == "own":
                    secs.append((0, AW, "fm", p["qT"], p["b_qT"], 0, T))
                    secs.append((AW, AW, "fm", p["kT"], p["b_kT"], 0, T))
                    secs.append((2 * AW, AW, "tm", p["v"], p["b_v"], 0, T))
                elif kind == "halo":
                    if t0 < p["own"] + T:
                        nh = 64 * DIL[1]
                        secs.append((AW, 2 * GW, "fm", p["kT"], p["b_kT"], 0, nh))
                        secs.append((2 * AW, 2 * GW, "tm", p["v"], p["b_v"], 0, nh))
                    secs.append((AW + 2 * GW, GW, "fm", p["kT"], p["b_kT"], 2 * GW, T))
                    secs.append((2 * AW + 2 * GW, GW, "tm", p["v"], p["b_v"], 2 * GW, T))
                secs.append((3 * AW, FW, "tm", p["f"], p["b_f"], 0, T))
                for (c0, ncols, mode, dst, dstb, dc0, ntok) in secs:
                    bw = 512 if ncols % 512 == 0 else 128
                    for blk in range(ncols // bw):
                        ws = wsl[wi[0] % 2]
                        wi[0] += 1
                        ckey = (c0 + blk * bw, bw)
                        fillA = ckey not in cacheA
                        if fillA:
                            wv = wblock(ws, w_in[:, c0 + blk * bw:c0 + (blk + 1) * bw], KC, bw)
                        else:
                            wv = ws.ap[:, 0:KC, 0:bw]
                            DMA("pool", wv, cacheA[ckey].rearrange("p (a b) -> p a b", b=bw), [b_wcA], [ws.buf], ws.buf)
                            bcount[0] += 1
                            if bcount[0] % 3 == 0 and conv_state["i"] < nconvA:
                                emit_conv(1)
                        if mode == "fm":
                            for j in range(bw // 128):
                                ps = PS()
                                for kc in range(KC):
                                    MM(ps, ps.ap[:, 0:ntok], wv[:, kc, j * 128:(j + 1) * 128], hT.ap[:, kc, 0:ntok], kc == 0, kc == KC - 1,
                                       [ws.buf, hT.buf])
                                sg = stg[si[0] % 4]
                                si[0] += 1
                                ACT(sg.ap[:, 0:ntok], ps.ap[:, 0:ntok], AF.Copy, [ps.buf], [sg.buf])
                                r0 = dc0 + blk * bw + j * 128
                                DMA("sp", dst[r0:r0 + 128, t0:t0 + ntok], sg.ap[:, 0:ntok], [sg.buf], [dstb], sg.buf)
                        else:
                            ns4 = ntok // 128
                            pst = [PS() for _ in range(ns4)]
                            for kc in range(KC):
                                for s4 in range(ns4):
                                    MM(pst[s4], pst[s4].ap[:, 0:bw], hT.ap[:, kc, s4 * 128:(s4 + 1) * 128], wv[:, kc, :],
                                       kc == 0, kc == KC - 1, [ws.buf, hT.buf])
                            for s4 in range(ns4):
                                sg = stg[si[0] % 4]
                                si[0] += 1
                                if s4 % 2 == 0:
                                    ACT(sg.ap[:, 0:bw], pst[s4].ap[:, 0:bw], AF.Copy, [pst[s4].buf], [sg.buf])
                                else:
                                    CP(sg.ap[:, 0:bw], pst[s4].ap[:, 0:bw], [pst[s4].buf], [sg.buf])
                                cc0 = dc0 + blk * bw
                                DMA("sp", dst[t0 + s4 * 128:t0 + (s4 + 1) * 128, cc0:cc0 + bw], sg.ap[:, 0:bw], [sg.buf], [dstb],
                                    sg.buf)
                        if fillA:
                            cacheA[ckey] = dscr("wcA_%d" % ckey[0], [128, KC * bw])
                            DMA("sp", cacheA[ckey].rearrange("p (a b) -> p a b", b=bw), wv, [ws.buf], [b_wcA], ws.buf)

    def phase_B1():
        phase_reset()
        etA = AR.alloc("etA", [NGRP * HPG, 128])
        etB = AR.alloc("etB", [NGRP * HPG, 128])
        etA0 = AR.alloc("etA0", [NGRP * HPG, 128])
        ones = AR.alloc("ones", [128], BF16)
        DMA("sp", etA.ap, ea_d, [], [etA.buf], etA.buf)
        DMA("sp", etB.ap, eb_d, [], [etB.buf], etB.buf)
        DMA("sp", etA0.ap[0:64], ea0_d, [], [etA0.buf], etA0.buf)
        P.op("dve", lambda e: e.memset(ones.ap, 1.0), writes=[ones.buf])
        maxown = max(p["own"] for p in parts)
        ksz = [max(p["own"] + 64 * DIL[g] for p in parts) for g in range(NGRP)]
        vsz = [max((max(1, p["own"] // DIL[g] // 128) + 1) * DIL[g] * 128 for p in parts) for g in range(NGRP)]
        qs = [[AR.alloc("q%d_%d" % (i, g), [maxown], BF16) for g in range(NGRP)] for i in range(2)]
        ks = [[AR.alloc("k%d_%d" % (i, g), [ksz[g]], BF16) for g in range(NGRP)] for i in range(2)]
        vs = [[AR.alloc("v%d_%d" % (i, g), [vsz[g]], BF16) for g in range(NGRP)] for i in range(2)]
        oacc = [AR.alloc("oacc%d" % i, [maxown]) for i in range(2)]
        dacc = [AR.alloc("dacc%d" % i, [maxown]) for i in range(2)]
        ost = [AR.alloc("ost%d" % i, [maxown], BF16) for i in range(2)]
        pfs = [AR.alloc("pf%d" % i, [128]) for i in range(4)]
        pbs = [AR.alloc("pb%d" % i, [128], BF16) for i in range(4)]
        hi = [0]
        bi = [0]
        per_head = (len(conv_jobs) // 4 + 2 * HPG - 1) // (2 * HPG)
        for p in parts:
            own, ext = p["own"], p["ext"]
            for h in range(HPG):
                if not cfg.get("dense_moe", False):
                    emit_conv(per_head)
                par = hi[0] % 2
                hi[0] += 1
                oa, da, os_ = oacc[par], dacc[par], ost[par]
                for g in range(NGRP):
                    d = DIL[g]
                    Lo = own // d
                    eg = own + 64 * d
                    row0 = (g * HPG + h) * 128
                    qt, kt, vt = qs[par][g], ks[par][g], vs[par][g]
                    DMA("sp", qt.ap[:, 0:own], p["qT"][row0:row0 + 128, 0:own], [p["b_qT"]], [qt.buf], qt.buf)
                    DMA("sp", kt.ap[:, 0:eg], p["kT"][row0:row0 + 128, 0:eg], [p["b_kT"]], [kt.buf], kt.buf)
                    nq = max(1, Lo // 128)
                    Q = min(128, Lo)
                    vcol = slice(row0, row0 + 128)
                    MT = nq + 1
                    vv = vt.ap[:, 0:MT * d * 128].rearrange("p (m r c) -> p m r c", m=MT, r=d)
                    DMA("sp", vv[0:64, 0], p["v"][0:64 * d, vcol].rearrange("(p r) c -> p r c", r=d), [p["b_v"]], [vt.buf], vt.buf)
                    if Lo >= 128:
                        for m in range(1, MT):
                            tok0 = (128 * m - 64) * d
                            DMA("sp", vv[:, m], p["v"][tok0:tok0 + 128 * d, vcol].rearrange("(p r) c -> p r c", r=d), [p["b_v"]],
                                [vt.buf], vt.buf)
                    else:
                        DMA("sp", vv[0:Q, 1], p["v"][64 * d:(64 + Q) * d, vcol].rearrange("(p r) c -> p r c", r=d), [p["b_v"]],
                            [vt.buf], vt.buf)
                    eidx = g * HPG + h
                    for r in range(d):
                        for iq in range(nq):
                            i0 = iq * 128
                            qv = qt.ap[:, i0 * d + r:(i0 + Q) * d:d]
                            psO = PS()
                            psD = PS()
                            tiles = []
                            if iq == 0:
                                tiles.append((0, 64, 0, etA0.ap[0:64, eidx, 0:Q], etA0.buf))
                            else:
                                tiles.append((i0 - 64, 128, iq, etA.ap[:, eidx, 0:Q], etA.buf))
                            KB = Q
                            tiles.append((i0 + 64, KB, iq + 1, etB.ap[0:KB, eidx, 0:Q], etB.buf))
                            for ti, (lo, K, m, E, Eb) in enumerate(tiles):
                                kv = kt.ap[:, lo * d + r:(lo + K) * d:d]
                                psS = PS()
                                MM(psS, psS.ap[0:K, 0:Q], kv, qv, True, True, [kt.buf, qt.buf])
                                pf = pfs[bi[0] % 4]
                                pb = pbs[bi[0] % 4]
                                bi[0] += 1
                                ACT(pf.ap[0:K, 0:Q], psS.ap[0:K, 0:Q], AF.Exp, [psS.buf], [pf.buf], scale=scale)
                                TT(pb.ap[0:K, 0:Q], pf.ap[0:K, 0:Q], E, ALU.mult, [pf.buf, Eb], [pb.buf])
                                MM(psO, psO.ap[:, 0:Q], vv[0:K, m, r, :], pb.ap[0:K, 0:Q], ti == 0, ti == 1, [vt.buf, pb.buf])
                                MM(psD, psD.ap[:, 0:Q], ones.ap[0:K, :], pb.ap[0:K, 0:Q], ti == 0, ti == 1, [ones.buf, pb.buf])
                            ov = oa.ap[:, i0 * d + r:(i0 + Q) * d:d]
                            dv = da.ap[:, i0 * d + r:(i0 + Q) * d:d]
                            if g == 0:
                                ACT(ov, psO.ap[:, 0:Q], AF.Copy, [psO.buf], [oa.buf])
                                CP(dv, psD.ap[:, 0:Q], [psD.buf], [da.buf])
                            else:
                                TT(ov, ov, psO.ap[:, 0:Q], ALU.add, [psO.buf, oa.buf], [oa.buf])
                                TT(dv, dv, psD.ap[:, 0:Q], ALU.add, [psD.buf, da.buf], [da.buf])
                P.op("dve", lambda e, a=da.ap[:, 0:own]: e.reciprocal(a, a), reads=[da.buf], writes=[da.buf])
                TT(os_.ap[:, 0:own], oa.ap[:, 0:own], da.ap[:, 0:own], ALU.mult, [oa.buf, da.buf], [os_.buf], eng="pool")
                DMA("pool", p["oT"][h * 128:(h + 1) * 128, 0:own], os_.ap[:, 0:own], [os_.buf], [p["b_oT"]], os_.buf)

    def phase_B2():
        phase_reset()
        ccs = AR.alloc("ccs", [FCG, FGW], BF16)
        scs = AR.alloc("scs", [FCG, FGW], BF16)
        wblock(ccs, cc_d, FCG, FGW)
        wblock(scs, sc_d, FCG, FGW)
        maxS = max(p["S"] for p in parts)
        csb = AR.alloc("csb", [maxS // 128, T], BF16)
        ssb = AR.alloc("ssb", [maxS // 128, T], BF16)
        xf = [AR.alloc("xf%d" % i, [maxS // 128, FGW], BF16) for i in range(2)]
        abT = [AR.alloc("abT%d" % i, [2 * FCG, T], BF16) for i in range(2)]
        ystg = [AR.alloc("ystg%d" % i, [T], BF16) for i in range(4)]
        xi = [0]
        yi = [0]
        n_it = sum(p["own"] // T for p in parts) * NFG
        per_it = (len(conv_jobs) // 4 + n_it - 1) // n_it
        for p in parts:
            NC_ = p["S"] // 128
            for kb in range(p["own"] // T):
                k0 = kb * T
                csv = wblock(csb, p["cs"][:, k0:k0 + T], NC_, T)
                ssv = wblock(ssb, p["ss"][:, k0:k0 + T], NC_, T)
                for fg in range(NFG):
                    if not cfg.get("dense_moe", False):
                        emit_conv(per_it)
                    x_ = xf[xi[0] % 2]
                    ab = abT[xi[0] % 2]
                    xi[0] += 1
                    xv = x_.ap[:, 0:NC_, :]
                    DMA("sp", xv, p["f"][:, fg * FGW:(fg + 1) * FGW].rearrange("(n p) c -> p n c", p=128), [p["b_f"]], [x_.buf], x_.buf)
                    for fc in range(FCG):
                        for which, mat, matb in ((0, csv, csb.buf), (1, ssv, ssb.buf)):
                            ps = PS()
                            for n in range(NC_):
                                MM(ps, ps.ap, xv[:, n, fc * 128:(fc + 1) * 128], mat[:, n, :], n == 0, n == NC_ - 1, [x_.buf, matb])
                            if which == 0:
                                ACT(ab.ap[:, fc, :], ps.ap, AF.Copy, [ps.buf], [ab.buf])
                            else:
                                CP(ab.ap[:, FCG + fc, :], ps.ap, [ps.buf], [ab.buf])
                    for oc in range(FCG):
                        ps = PS()
                        for c in range(2 * FCG):
                            m_ = ccs if c < FCG else scs
                            MM(ps, ps.ap, m_.ap[:, c % FCG, oc * 128:(oc + 1) * 128], ab.ap[:, c, :], c == 0, c == 2 * FCG - 1,
                               [m_.buf, ab.buf])
                        ys = ystg[yi[0] % 4]
                        yi[0] += 1
                        ACT(ys.ap, ps.ap, AF.Copy, [ps.buf], [ys.buf])
                        r0 = fg * FGW + oc * 128
                        DMA("act", p["frT"][r0:r0 + 128, k0:k0 + T], ys.ap, [ys.buf], [p["b_frT"]], ys.buf)

    def phase_C1():
        phase_reset()
        bgT = AR.alloc("bgT", [2 * KC])
        DMA("sp", bgT.ap, bgT_d, [], [bgT.buf], bgT.buf)
        hT = AR.alloc("hTc", [KC, T], BF16)
        mT = AR.alloc("mTc", [KC, T], BF16)
        oTt = AR.alloc("oTt", [AO // 128, T], BF16)
        frt = AR.alloc("frt", [FW // 128, T], BF16)
        xts = [AR.alloc("xtC%d" % i, [D]) for i in range(1)]
        junk = AR.alloc("junkC", [D], BF16)
        smalls = [AR.alloc("smallC%d" % i, [4]) for i in range(2)]
        WB = 256
        WKC = max(KC, AO // 128 + FW // 128)
        wsl = [AR.alloc("wC%d" % i, [WKC, WB], BF16) for i in range(3)]
        gts = [AR.alloc("gt%d" % i, [T]) for i in range(4)]
        tmp = [AR.alloc("tmpC%d" % i, [T]) for i in range(4)]
        xr = [AR.alloc("xr%d" % i, [WB]) for i in range(4)]
        wi = [0]
        gi = [0]
        xi = [0]
        ri = [0]

        def nextw():
            w = wsl[wi[0] % 3]
            wi[0] += 1
            return w

        NBR = AO // 128 + FW // 128
        NCB = D // WB
        wc_ga = dscr("wc_ga", [NCB * 128, KC * WB])
        wc_gf = dscr("wc_gf", [NCB * 128, KC * WB])
        wc_br = dscr("wc_br", [NCB * 128, NBR * WB])
        wc_wo = dscr("wc_wo", [NCB * 128, KC * WB])
        b_wc = {k: Buf("wc_" + k, dram=True) for k in ("ga", "gf", "br", "wo")}

        def cache_store(kind, wc, cb, slot, nch):
            DMA("sp", wc[cb * 128:(cb + 1) * 128, :], slot.ap[:, 0:nch, :].rearrange("p a b -> p (a b)"), [slot.buf], [b_wc[kind]],
                slot.buf)

        def cache_load(kind, wc, cb, slot, nch):
            v = slot.ap[:, 0:nch, :]
            DMA("pool", v, wc[cb * 128:(cb + 1) * 128, :].rearrange("p (a b) -> p a b", b=WB), [b_wc[kind]], [slot.buf], slot.buf)
            return v

        first = [True]
        for p in parts:
            for tt in range(p["own"] // T):
                t0 = tt * T
                fill = first[0]
                first[0] = False
                DMA("sp", oTt.ap, p["oT"][:, t0:t0 + T].rearrange("(c p) t -> p c t", p=128), [p["b_oT"]], [oTt.buf], oTt.buf)
                DMA("sp", frt.ap, p["frT"][:, t0:t0 + T].rearrange("(c p) t -> p c t", p=128), [p["b_frT"]], [frt.buf], frt.buf)
                for s4 in range(4):
                    xt = xts[0]
                    sm = smalls[xi[0] % 2]
                    xi[0] += 1
                    DMA("sp", xt.ap, p["x"][t0 + s4 * 128:t0 + (s4 + 1) * 128, :], [], [xt.buf], xt.buf)
                    rms_to_T(xt, hT, s4 * 128, g1T, junk, sm, xt)
                for cb in range(D // WB):
                    wga = nextw()
                    wgf = nextw()
                    wbr = nextw()
                    wbav = wbr.ap[:, 0:AO // 128, 0:WB]
                    wbfv = wbr.ap[:, AO // 128:AO // 128 + FW // 128, 0:WB]
                    if fill:
                        wgav = wblock(wga, w_in[:, 3 * AW + FW + cb * WB:3 * AW + FW + (cb + 1) * WB], KC, WB)
                        wgfv = wblock(wgf, w_in[:, 3 * AW + FW + D + cb * WB:3 * AW + FW + D + (cb + 1) * WB], KC, WB)
                        DMA("pool", wbav, w_ba[:, cb * WB:(cb + 1) * WB].rearrange("(kc p) n -> p kc n", p=128), [], [wbr.buf], wbr.buf)
                        DMA("pool", wbfv, w_bf[:, cb * WB:(cb + 1) * WB].rearrange("(kc p) n -> p kc n", p=128), [], [wbr.buf], wbr.buf)
                    else:
                        if not cfg.get("dense_moe", False):
                            emit_conv(2)
                        wgav = cache_load("ga", wc_ga, cb, wga, KC)
                        wgfv = cache_load("gf", wc_gf, cb, wgf, KC)
                        cache_load("br", wc_br, cb, wbr, NBR)
                    for j in range(WB // 128):
                        c = cb * (WB // 128) + j
                        cs_ = slice(j * 128, (j + 1) * 128)
                        psA, psF, psa, psf = PS(), PS(), PS(), PS()
                        for kc in range(KC):
                            MM(psA, psA.ap, wgav[:, kc, cs_], hT.ap[:, kc, :], kc == 0, kc == KC - 1, [wga.buf, hT.buf])
                        for kc in range(KC):
                            MM(psF, psF.ap, wgfv[:, kc, cs_], hT.ap[:, kc, :], kc == 0, kc == KC - 1, [wgf.buf, hT.buf])
                        na = AO // 128
                        for kc in range(na):
                            MM(psa, psa.ap, wbav[:, kc, cs_], oTt.ap[:, kc, :], kc == 0, kc == na - 1, [wbr.buf, oTt.buf])
                        nf = FW // 128
                        for kc in range(nf):
                            MM(psf, psf.ap, wbfv[:, kc, cs_], frt.ap[:, kc, :], kc == 0, kc == nf - 1, [wbr.buf, frt.buf])
                        gA = gts[gi[0] % 4]
                        gF = gts[(gi[0] + 1) % 4]
                        t1 = tmp[gi[0] % 4]
                        t2 = tmp[(gi[0] + 1) % 4]
                        gi[0] += 2
                        ACT(gA.ap, psA.ap, AF.Sigmoid, [psA.buf, bgT.buf], [gA.buf], bias=bgT.ap[:, c:c + 1])
                        ACT(gF.ap, psF.ap, AF.Sigmoid, [psF.buf, bgT.buf], [gF.buf], bias=bgT.ap[:, KC + c:KC + c + 1])
                        TT(t1.ap, gA.ap, psa.ap, ALU.mult, [gA.buf, psa.buf], [t1.buf])
                        TT(t2.ap, gF.ap, psf.ap, ALU.mult, [gF.buf, psf.buf], [t2.buf])
                        TT(mT.ap[:, c, :], t1.ap, t2.ap, ALU.add, [t1.buf, t2.buf], [mT.buf], eng="pool")
                    if fill:
                        cache_store("ga", wc_ga, cb, wga, KC)
                        cache_store("gf", wc_gf, cb, wgf, KC)
                        cache_store("br", wc_br, cb, wbr, NBR)
                for cb in range(D // WB):
                    ws = nextw()
                    if fill:
                        wv = wblock(ws, w_out[:, cb * WB:(cb + 1) * WB], KC, WB)
                    else:
                        wv = cache_load("wo", wc_wo, cb, ws, KC)
                    pst = [PS() for _ in range(4)]
                    for kc in range(KC):
                        for s4 in range(4):
                            MM(pst[s4], pst[s4].ap[:, 0:WB], mT.ap[:, kc, s4 * 128:(s4 + 1) * 128], wv[:, kc, :], kc == 0, kc == KC - 1,
                               [ws.buf, mT.buf])
                    for s4 in range(4):
                        x_ = xr[ri[0] % 4]
                        ri[0] += 1
                        rows = slice(t0 + s4 * 128, t0 + (s4 + 1) * 128)
                        DMA("sp", x_.ap, p["x"][rows, cb * WB:(cb + 1) * WB], [], [x_.buf], x_.buf)
                        TT(x_.ap, x_.ap, pst[s4].ap[:, 0:WB], ALU.add, [x_.buf, pst[s4].buf], [x_.buf])
                        DMA("sp", p["x1"][rows, cb * WB:(cb + 1) * WB], x_.ap, [x_.buf], [p["b_x1"]], x_.buf)
                    if fill:
                        cache_store("wo", wc_wo, cb, ws, KC)

    def phase_C2():
        phase_reset()
        gfb = AR.alloc("gfb", [D])
        brb = AR.alloc("brb", [NR])
        wrs = AR.alloc("wrs", [KC, NR], BF16)
        DMA("sp", gfb.ap, gf_d, [], [gfb.buf], gfb.buf)
        DMA("sp", brb.ap, br_d, [], [brb.buf], brb.buf)
        wblock(wrs, w_r, KC, NR)
        xt = AR.alloc("x1t", [4, D])
        hT = AR.alloc("h2T", [KC, T], BF16)
        xs = AR.alloc("xsD", [D])
        junk = xs
        smalls = [AR.alloc("smallD%d" % i, [4]) for i in range(2)]
        comb = AR.alloc("comb", [4, NE])
        rt = AR.alloc("rt", [4, 64])
        WB = 128
        DB = min(512, D)
        WKC = max(KC, (FFC * DB + WB - 1) // WB)
        NWS = 4
        wsl = [AR.alloc("wD%d" % i, [WKC, WB], BF16) for i in range(NWS)]
        aT = [AR.alloc("aT%d" % i, [FFC, T], BF16) for i in range(2)]
        sil = [AR.alloc("sil%d" % i, [T]) for i in range(3)]
        wi = [0]
        ai = [0]
        li = [0]
        xi = [0]

        def nextw():
            w = wsl[wi[0] % NWS]
            wi[0] += 1
            return w

        for p in parts:
            for tt in range(p["own"] // T):
                t0 = tt * T
                for s4 in range(4):
                    xv = Tile(xt.ap[:, s4, :], xt.buf)
                    sm = smalls[xi[0] % 2]
                    xi[0] += 1
                    DMA("sp", xv.ap, p["x1"][t0 + s4 * 128:t0 + (s4 + 1) * 128, :], [p["b_x1"]], [xt.buf], xt.buf)
                    rms_to_T(xv, hT, s4 * 128, g2T, junk, sm, xs)
                for s4 in range(4):
                    ps = PS()
                    for kc in range(KC):
                        MM(ps, ps.ap[:, 0:NR], hT.ap[:, kc, s4 * 128:(s4 + 1) * 128], wrs.ap[:, kc, :], kc == 0, kc == KC - 1,
                           [hT.buf, wrs.buf])
                    R = rt.ap[:, s4, :]
                    rb = [rt.buf]
                    lg = R[:, 0:NR]
                    TT(lg, ps.ap[:, 0:NR], brb.ap, ALU.add, [ps.buf, brb.buf], rb)
                    gmax = R[:, 20:21]
                    P.op("dve", lambda e, o=gmax, i=lg[:, 0:NEG]: e.reduce_max(o, i, AX.X), reads=rb, writes=rb)
                    oh = R[:, 21:25]
                    TS(oh, lg[:, 0:NEG], gmax, None, ALU.is_equal, None, rb, rb)
                    ngm = R[:, 25:26]
                    TS(ngm, gmax, -1.0, None, ALU.mult, None, rb, rb)
                    eg_ = R[:, 26:30]
                    sg_ = R[:, 30:31]
                    ACT(eg_, lg[:, 0:NEG], AF.Exp, rb, rb, bias=ngm, accum=sg_)
                    pg = R[:, 31:32]
                    P.op("dve", lambda e, o=pg, i=sg_: e.reciprocal(o, i), reads=rb, writes=rb)
                    les = R[:, 32:36]
                    TS(les, lg[:, NEG:NEG + EPG], oh[:, 0:1], None, ALU.mult, None, rb, rb)
                    for g in range(1, NEG):
                        STT(les, lg[:, NEG + g * EPG:NEG + (g + 1) * EPG], oh[:, g:g + 1], les, ALU.mult, ALU.add, rb, rb)
                    m1 = R[:, 36:37]
                    P.op("dve", lambda e, o=m1, i=les: e.reduce_max(o, i, AX.X), reads=rb, writes=rb)
                    k1 = R[:, 37:41]
                    TS(k1, les, m1, None, ALU.is_equal, None, rb, rb)
                    le2 = R[:, 41:45]
                    STT(le2, k1, -1e30, les, ALU.mult, ALU.add, rb, rb)
                    m2 = R[:, 45:46]
                    P.op("dve", lambda e, o=m2, i=le2: e.reduce_max(o, i, AX.X), reads=rb, writes=rb)
                    k2 = R[:, 46:50]
                    TS(k2, le2, m2, None, ALU.is_equal, None, rb, rb)
                    dm = R[:, 50:51]
                    TT(dm, m2, m1, ALU.subtract, rb, rb)
                    ex = R[:, 51:52]
                    ACT(ex, dm, AF.Exp, rb, rb)
                    den = R[:, 52:53]
                    TS(den, ex, 1.0, None, ALU.add, None, rb, rb)
                    p1 = R[:, 53:54]
                    P.op("dve", lambda e, o=p1, i=den: e.reciprocal(o, i), reads=rb, writes=rb)
                    p2 = R[:, 54:55]
                    TT(p2, ex, p1, ALU.mult, rb, rb)
                    TT(p1, p1, pg, ALU.mult, rb, rb)
                    TT(p2, p2, pg, ALU.mult, rb, rb)
                    cw = R[:, 55:59]
                    TS(cw, k1, p1, None, ALU.mult, None, rb, rb)
                    STT(cw, k2, p2, cw, ALU.mult, ALU.add, rb, rb)
                    for g in range(NEG):
                        TS(comb.ap[:, s4, g * EPG:(g + 1) * EPG], cw, oh[:, g:g + 1], None, ALU.mult, None, rb, [comb.buf])
                for ex_ in range(NE):
                    a_ = aT[ai[0] % 2]
                    ai[0] += 1
                    for fb in range(DFF // WB):
                        wg = nextw()
                        wgv = wblock(wg, w_eg[ex_, :, fb * WB:(fb + 1) * WB], KC, WB)
                        wu = nextw()
                        wuv = wblock(wu, w_eu[ex_, :, fb * WB:(fb + 1) * WB], KC, WB)
                        for j in range(WB // 128):
                            fc = fb * (WB // 128) + j
                            cs_ = slice(j * 128, (j + 1) * 128)
                            psG, psU = PS(), PS()
                            for kc in range(KC):
                                MM(psG, psG.ap, wgv[:, kc, cs_], hT.ap[:, kc, :], kc == 0, kc == KC - 1, [wg.buf, hT.buf])
                            for kc in range(KC):
                                MM(psU, psU.ap, wuv[:, kc, cs_], hT.ap[:, kc, :], kc == 0, kc == KC - 1, [wu.buf, hT.buf])
                            s_ = sil[li[0] % 3]
                            li[0] += 1
                            ACT(s_.ap, psG.ap, AF.Silu, [psG.buf], [s_.buf])
                            TT(a_.ap[:, fc, :], s_.ap, psU.ap, ALU.mult, [s_.buf, psU.buf], [a_.buf])
                    for cb in range(D // DB):
                        wd = nextw()
                        wdv = wd.ap.rearrange("p a b -> p (a b)")[:, 0:FFC * DB].rearrange("p (a b) -> p a b", a=FFC)
                        DMA("pool", wdv, w_ed[ex_, :, cb * DB:(cb + 1) * DB].rearrange("(kc p) n -> p kc n", p=128), [], [wd.buf], wd.buf)
                        pst = [PS() for _ in range(4)]
                        for kc in range(FFC):
                            for s4 in range(4):
                                MM(pst[s4], pst[s4].ap[:, 0:DB], a_.ap[:, kc, s4 * 128:(s4 + 1) * 128], wdv[:, kc, :], kc == 0, kc == FFC - 1,
                                   [wd.buf, a_.buf])
                        for s4 in range(4):
                            xv = xt.ap[:, s4, cb * DB:(cb + 1) * DB]
                            STT(xv, pst[s4].ap[:, 0:DB], comb.ap[:, s4, ex_:ex_ + 1], xv, ALU.mult, ALU.add, [pst[s4].buf, comb.buf, xt.buf], [xt.buf])
                for s4 in range(4):
                    sm = smalls[xi[0] % 2]
                    xi[0] += 1
                    xv = xt.ap[:, s4, :]
                    ssq, t1, t2, rstd = sm.ap[:, 0:1], sm.ap[:, 1:2], sm.ap[:, 2:3], sm.ap[:, 3:4]
                    ACT(junk.ap, xv, AF.Square, [xt.buf], [junk.buf, sm.buf], accum=ssq)
                    TS(t1, ssq, 1.0 / D, EPS, ALU.mult, ALU.add, [sm.buf], [sm.buf])
                    ACT(t2, t1, AF.Sqrt, [sm.buf], [sm.buf])
                    P.op("dve", lambda e, o=rstd, i=t2: e.reciprocal(o, i), reads=[sm.buf], writes=[sm.buf])
                    STT(xs.ap, xv, rstd, gfb.ap, ALU.mult, ALU.mult, [xt.buf, sm.buf, gfb.buf], [xs.buf])
                    DMA("sp", p["y"][t0 + s4 * 128:t0 + (s4 + 1) * 128, :], xs.ap, [xs.buf], [p["b_y"]], xs.buf)


    def IDMA(out, out_off, in_, in_off, reads, writes, slot, eoff=0):
        def emit(e):
            oo = bass.IndirectOffsetOnAxis(ap=out_off, axis=0) if out_off is not None else None
            io = bass.IndirectOffsetOnAxis(ap=in_off, axis=0) if in_off is not None else None
            return e.indirect_dma_start(out=out, out_offset=oo, in_=in_, in_offset=io, element_offset=eoff)
        P.op("pool", emit, reads=reads, writes=writes, dma=slot)

    def phase_C2_sparse():
        emit_conv(len(conv_jobs))
        subt = [(p, t0) for p in parts for t0 in range(0, p["own"], 128)]
        phase_reset()
        g2b = AR.alloc("g2b", [D])
        brb = AR.alloc("brb", [NR])
        wrs = AR.alloc("wrs", [KC, NR], BF16)
        DMA("sp", g2b.ap, g2b_d, [], [g2b.buf], g2b.buf)
        DMA("sp", brb.ap, br_d, [], [brb.buf], brb.buf)
        wblock(wrs, w_r, KC, NR)
        hT = AR.alloc("h2T", [KC, T], BF16)
        xts = [AR.alloc("x1a%d" % i, [D]) for i in range(2)]
        xs = AR.alloc("xsD", [D])
        htm = [AR.alloc("htm%d" % i, [D], BF16) for i in range(2)]
        smalls = [AR.alloc("smallD%d" % i, [4]) for i in range(2)]
        rt = AR.alloc("rt", [4, 64])
        xi = [0]
        for tb_ in range(NT // 4):
            for s4 in range(4):
                i = tb_ * 4 + s4
                p, t0 = subt[i]
                xt = xts[xi[0] % 2]
                sm = smalls[xi[0] % 2]
                hm = htm[xi[0] % 2]
                xi[0] += 1
                DMA("sp", xt.ap, p["x1"][t0:t0 + 128, :], [p["b_x1"]], [xt.buf], xt.buf)
                rms_to_T(xt, hT, s4 * 128, g2T, xs, sm, xs)
                TT(hm.ap, xs.ap, g2b.ap, ALU.mult, [xs.buf, g2b.buf], [hm.buf], eng="pool")
                DMA("sp", H_d[i * 128:(i + 1) * 128, :], hm.ap, [hm.buf], [b_H], hm.buf)
            for s4 in range(4):
                i = tb_ * 4 + s4
                ps = PS()
                for kc in range(KC):
                    MM(ps, ps.ap[:, 0:NR], hT.ap[:, kc, s4 * 128:(s4 + 1) * 128], wrs.ap[:, kc, :], kc == 0, kc == KC - 1,
                       [hT.buf, wrs.buf])
                R = rt.ap[:, s4, :]
                rb = [rt.buf]
                lg = R[:, 0:NR]
                TT(lg, ps.ap[:, 0:NR], brb.ap, ALU.add, [ps.buf, brb.buf], rb)
                gmax = R[:, 20:21]
                P.op("dve", lambda e, o=gmax, i_=lg[:, 0:NEG]: e.reduce_max(o, i_, AX.X), reads=rb, writes=rb)
                oh = R[:, 21:25]
                TS(oh, lg[:, 0:NEG], gmax, None, ALU.is_equal, None, rb, rb)
                ngm = R[:, 25:26]
                TS(ngm, gmax, -1.0, None, ALU.mult, None, rb, rb)
                eg_ = R[:, 26:30]
                sg_ = R[:, 30:31]
                ACT(eg_, lg[:, 0:NEG], AF.Exp, rb, rb, bias=ngm, accum=sg_)
                pg = R[:, 31:32]
                P.op("dve", lambda e, o=pg, i_=sg_: e.reciprocal(o, i_), reads=rb, writes=rb)
                les = R[:, 32:36]
                TS(les, lg[:, NEG:NEG + EPG], oh[:, 0:1], None, ALU.mult, None, rb, rb)
                for g in range(1, NEG):
                    STT(les, lg[:, NEG + g * EPG:NEG + (g + 1) * EPG], oh[:, g:g + 1], les, ALU.mult, ALU.add, rb, rb)
                m1 = R[:, 36:37]
                P.op("dve", lambda e, o=m1, i_=les: e.reduce_max(o, i_, AX.X), reads=rb, writes=rb)
                k1 = R[:, 37:41]
                TS(k1, les, m1, None, ALU.is_equal, None, rb, rb)
                le2 = R[:, 41:45]
                STT(le2, k1, -1e30, les, ALU.mult, ALU.add, rb, rb)
                m2 = R[:, 45:46]
                P.op("dve", lambda e, o=m2, i_=le2: e.reduce_max(o, i_, AX.X), reads=rb, writes=rb)
                k2 = R[:, 46:50]
                TS(k2, le2, m2, None, ALU.is_equal, None, rb, rb)
                dm = R[:, 50:51]
                TT(dm, m2, m1, ALU.subtract, rb, rb)
                ex = R[:, 51:52]
                ACT(ex, dm, AF.Exp, rb, rb)
                den = R[:, 52:53]
                TS(den, ex, 1.0, None, ALU.add, None, rb, rb)
                p1 = R[:, 53:54]
                P.op("dve", lambda e, o=p1, i_=den: e.reciprocal(o, i_), reads=rb, writes=rb)
                p2 = R[:, 54:55]
                TT(p2, ex, p1, ALU.mult, rb, rb)
                TT(rc12.ap[:, i, 0:1], p1, pg, ALU.mult, rb, [rc12.buf])
                TT(rc12.ap[:, i, 1:2], p2, pg, ALU.mult, rb, [rc12.buf])
                for g in range(NEG):
                    TS(rm1.ap[:, i, g * EPG:(g + 1) * EPG], k1, oh[:, g:g + 1], None, ALU.mult, None, rb, [rm1.buf])
                    TS(rm2.ap[:, i, g * EPG:(g + 1) * EPG], k2, oh[:, g:g + 1], None, ALU.mult, None, rb, [rm2.buf])
        Mbf = AR.alloc("Mbf", [NT, NE], BF16)
        onesb = AR.alloc("onesb", [128], BF16)
        trib = AR.alloc("trib", [128], BF16)
        Cs = AR.alloc("Cs", [NT, NE])
        posf = AR.alloc("posf", [NT, NE])
        prod = AR.alloc("prod", [NT, NE])
        pf12 = AR.alloc("pf12", [2, NT])
        tb = AR.alloc("tb", [96])
        jid = AR.alloc("jid", [JMAX])
        eacc = AR.alloc("eacc", [JMAX])
        DMA("pool", trib.ap, tri_d, [], [trib.buf], trib.buf)
        DMA("sp", jid.ap, jidx_d, [], [jid.buf], jid.buf)
        P.op("dve", lambda e: e.memset(onesb.ap, 1.0), writes=[onesb.buf])
        TT(Mbf.ap, rm1.ap, rm2.ap, ALU.add, [rm1.buf, rm2.buf], [Mbf.buf])
        for i in range(NT):
            ps = PS()
            for i2 in range(i):
                MM(ps, ps.ap[:, 0:NE], onesb.ap, Mbf.ap[:, i2, :], i2 == 0, False, [onesb.buf, Mbf.buf])
            MM(ps, ps.ap[:, 0:NE], trib.ap, Mbf.ap[:, i, :], i == 0, True, [trib.buf, Mbf.buf])
            CP(Cs.ap[:, i, :], ps.ap[:, 0:NE], [ps.buf], [Cs.buf])
        ps = PS()
        for i in range(NT):
            MM(ps, ps.ap[:, 0:NE], onesb.ap, Mbf.ap[:, i, :], i == 0, i == NT - 1, [onesb.buf, Mbf.buf])
        tbb = [tb.buf]
        n_ = tb.ap[:, 0:16]
        ntl = tb.ap[:, 16:32]
        cum = tb.ap[:, 32:48]
        bm1 = tb.ap[:, 48:64]
        tmp_ = tb.ap[:, 64:80]
        CP(n_, ps.ap[:, 0:NE], [ps.buf], tbb)
        P.op("dve", lambda e: e.memset(ntl, 0.0), reads=tbb, writes=tbb)
        for j in range(NTOK // TB):
            STT(ntl, n_, float(j * TB), ntl, ALU.is_gt, ALU.add, tbb, tbb)
        CP(cum[:, 0:1], ntl[:, 0:1], tbb, tbb)
        for e_ in range(1, NE):
            TT(cum[:, e_:e_ + 1], cum[:, e_ - 1:e_], ntl[:, e_:e_ + 1], ALU.add, tbb, tbb)
        TT(tmp_, cum, ntl, ALU.subtract, tbb, tbb)
        TS(bm1, tmp_, float(TB), -1.0, ALU.mult, ALU.add, tbb, tbb)
        TT(posf.ap, Cs.ap, bm1.unsqueeze(1).to_broadcast([128, NT, NE]), ALU.add, [Cs.buf] + tbb, [posf.buf])
        for k, rm in ((0, rm1), (1, rm2)):
            TT(prod.ap, posf.ap, rm.ap, ALU.mult, [posf.buf, rm.buf], [prod.buf])
            P.op("dve", lambda e, o=pf12.ap[:, k, :], i_=prod.ap: e.reduce_sum(o, i_, AX.X), reads=[prod.buf], writes=[pf12.buf])
            CP(rpos.ap[:, :, k], pf12.ap[:, k, :], [pf12.buf], [rpos.buf])
        P.op("dve", lambda e: e.memset(eacc.ap, 0.0), writes=[eacc.buf])
        for e_ in range(NE):
            STT(eacc.ap, jid.ap, cum[:, e_:e_ + 1], eacc.ap, ALU.is_ge, ALU.add, [jid.buf, eacc.buf] + tbb, [eacc.buf])
        TS(eacc.ap, eacc.ap, float(NE - 1), None, ALU.min, None, [eacc.buf], [eacc.buf])
        pidx = AR.alloc("pidx", [1])
        DMA("sp", pidx.ap, pidx_d, [], [pidx.buf], pidx.buf)
        TS(eacc.ap, eacc.ap, 128.0, pidx.ap[:, 0:1], ALU.mult, ALU.add, [eacc.buf, pidx.buf], [eacc.buf])
        CP(reid.ap, eacc.ap, [eacc.buf], [reid.buf])
        phase_reset()
        hb = [AR.alloc("hb%d" % i, [D], BF16) for i in range(2)]
        hbsem = [Buf("hbsem%d" % i) for i in range(2)]
        zt = AR.alloc("zt", [D], BF16)
        b_Hs0 = Buf("Hs0", dram=True)
        P.op("dve", lambda e: e.memset(zt.ap, 0.0), writes=[zt.buf])
        for r in range(NS // 128):
            DMA("sp", Hs_d[r * 128:(r + 1) * 128, :], zt.ap, [zt.buf], [b_Hs0], zt.buf)
        for i in range(NT):
            h = hb[i % 2]
            DMA("sp", h.ap, H_d[i * 128:(i + 1) * 128, :], [b_H], [h.buf], h.buf)
            for k in range(2):
                IDMA(Hs_d[:, :], rpos.ap[:, i, k:k + 1], h.ap, None, [h.buf, rpos.buf, b_Hs0], [b_Hs], hbsem[i % 2])
        NSUB = TB // 128
        hstm = [AR.alloc("hstm%d" % i, [NSUB, D], BF16) for i in range(2)]
        hsT = [AR.alloc("hsT%d" % i, [KC, TB], BF16) for i in range(2)]
        aT = [AR.alloc("aTs%d" % i, [FFC, TB], BF16) for i in range(2)]
        sil = [AR.alloc("sils%d" % i, [TB]) for i in range(3)]
        DB = min(512, D)
        WB = 128
        WKC = max(KC, (FFC * DB + WB - 1) // WB)
        NWS = 6
        wsl = [AR.alloc("wS%d" % i, [WKC, WB], BF16) for i in range(NWS)]
        ystg = [AR.alloc("ystg%d" % i, [DB]) for i in range(4)]
        identb = AR.alloc("identb", [128], BF16)
        CP(identb.ap, ident.ap, [ident.buf], [identb.buf])
        wi = [0]
        li = [0]
        yi = [0]
        regs = {}

        def wdyn(j, wb2, blk, n, view, slot):
            rowlen = wb2.shape[1]
            v2 = slot.ap.rearrange("p a b -> p (a b)")[:, 0:rowlen]
            IDMA(v2, None, wb2, reid.ap[:, j:j + 1], [reid.buf, b_wb], [slot.buf], slot.buf, eoff=blk * NE * 128 * rowlen)

        for j in range(JMAX):
            hm = hstm[j % 2]
            hT_ = hsT[j % 2]
            a_ = aT[j % 2]
            DMA("sp", hm.ap, Hs_d[j * TB:(j + 1) * TB, :].rearrange("(s p) d -> p s d", p=128), [b_Hs], [hm.buf], hm.buf)
            for st_ in range(NSUB):
                for kg in range(KC // 4):
                    ps = PS()
                    psb = ps.ap.bitcast(BF16)
                    for jj in range(4):
                        kc = kg * 4 + jj
                        TR(ps, psb[:, jj * 128:(jj + 1) * 128], hm.ap[:, st_, kc * 128:(kc + 1) * 128], identb.ap, [hm.buf, identb.buf])
                    src = psb[:, 0:512].rearrange("p (a b) -> p a b", a=4)
                    dst = hT_.ap[:, kg * 4:kg * 4 + 4, st_ * 128:(st_ + 1) * 128]
                    if kg % 2 == 0:
                        CP(dst, src, [ps.buf], [hT_.buf])
                    else:
                        ACT(dst, src, AF.Copy, [ps.buf], [hT_.buf])
            for fc in range(FFC):
                wg = wsl[wi[0] % NWS]
                wi[0] += 1
                wgv = wg.ap.rearrange("p a b -> p (a b)")[:, 0:KC * 128].rearrange("p (a b) -> p a b", a=KC)
                wdyn(j, wb_eg, fc, 128, wgv, wg)
                wu = wsl[wi[0] % NWS]
                wi[0] += 1
                wuv = wu.ap.rearrange("p a b -> p (a b)")[:, 0:KC * 128].rearrange("p (a b) -> p a b", a=KC)
                wdyn(j, wb_eu, fc, 128, wuv, wu)
                psG, psU = PS(), PS()
                for kc in range(KC):
                    MM(psG, psG.ap[:, 0:TB], wgv[:, kc, :], hT_.ap[:, kc, :], kc == 0, kc == KC - 1, [wg.buf, hT_.buf])
                for kc in range(KC):
                    MM(psU, psU.ap[:, 0:TB], wuv[:, kc, :], hT_.ap[:, kc, :], kc == 0, kc == KC - 1, [wu.buf, hT_.buf])
                s_ = sil[li[0] % 3]
                li[0] += 1
                ACT(s_.ap, psG.ap[:, 0:TB], AF.Silu, [psG.buf], [s_.buf])
                TT(a_.ap[:, fc, :], s_.ap, psU.ap[:, 0:TB], ALU.mult, [s_.buf, psU.buf], [a_.buf])
            for cb in range(D // DB):
                wd = wsl[wi[0] % NWS]
                wi[0] += 1
                wdv = wd.ap.rearrange("p a b -> p (a b)")[:, 0:FFC * DB].rearrange("p (a b) -> p a b", a=FFC)
                wdyn(j, wb_ed, cb, DB, wdv, wd)
                pst = [PS() for _ in range(NSUB)]
                for kc in range(FFC):
                    for st_ in range(NSUB):
                        MM(pst[st_], pst[st_].ap[:, 0:DB], a_.ap[:, kc, st_ * 128:(st_ + 1) * 128], wdv[:, kc, :], kc == 0, kc == FFC - 1,
                           [wd.buf, a_.buf])
                for st_ in range(NSUB):
                    ys = ystg[yi[0] % 4]
                    yi[0] += 1
                    if st_ % 2 == 0:
                        ACT(ys.ap, pst[st_].ap[:, 0:DB], AF.Copy, [pst[st_].buf], [ys.buf])
                    else:
                        CP(ys.ap, pst[st_].ap[:, 0:DB], [pst[st_].buf], [ys.buf])
                    r0 = j * TB + st_ * 128
                    DMA("sp", Ys_d[r0:r0 + 128, cb * DB:(cb + 1) * DB], ys.ap, [ys.buf], [b_Ys], ys.buf)
        phase_reset()
        gfb = AR.alloc("gfb", [D])
        DMA("sp", gfb.ap, gf_d, [], [gfb.buf], gfb.buf)
        x1b = [AR.alloc("x1e%d" % i, [D]) for i in range(2)]
        yab = [AR.alloc("ya%d" % i, [D]) for i in range(2)]
        ybb = [AR.alloc("yb%d" % i, [D]) for i in range(2)]
        xo = [AR.alloc("xo%d" % i, [D]) for i in range(2)]
        smalls = [AR.alloc("smallE%d" % i, [4]) for i in range(2)]
        for i in range(NT):
            p, t0 = subt[i]
            x = x1b[i % 2]
            ya, yb, o_ = yab[i % 2], ybb[i % 2], xo[i % 2]
            sm = smalls[i % 2]
            DMA("sp", x.ap, p["x1"][t0:t0 + 128, :], [p["b_x1"]], [x.buf], x.buf)
            IDMA(ya.ap, None, Ys_d[:, :], rpos.ap[:, i, 0:1], [b_Ys, rpos.buf], [ya.buf], ya.buf)
            IDMA(yb.ap, None, Ys_d[:, :], rpos.ap[:, i, 1:2], [b_Ys, rpos.buf], [yb.buf], yb.buf)
            STT(x.ap, ya.ap, rc12.ap[:, i, 0:1], x.ap, ALU.mult, ALU.add, [ya.buf, rc12.buf, x.buf], [x.buf])
            STT(x.ap, yb.ap, rc12.ap[:, i, 1:2], x.ap, ALU.mult, ALU.add, [yb.buf, rc12.buf, x.buf], [x.buf])
            ssq, t1, t2, rstd = sm.ap[:, 0:1], sm.ap[:, 1:2], sm.ap[:, 2:3], sm.ap[:, 3:4]
            ACT(o_.ap, x.ap, AF.Square, [x.buf], [o_.buf, sm.buf], accum=ssq)
            TS(t1, ssq, 1.0 / D, EPS, ALU.mult, ALU.add, [sm.buf], [sm.buf])
            ACT(t2, t1, AF.Sqrt, [sm.buf], [sm.buf])
            P.op("dve", lambda e, o=rstd, i_=t2: e.reciprocal(o, i_), reads=[sm.buf], writes=[sm.buf])
            STT(o_.ap, x.ap, rstd, gfb.ap, ALU.mult, ALU.mult, [x.buf, sm.buf, gfb.buf], [o_.buf])
            DMA("sp", p["y"][t0:t0 + 128, :], o_.ap, [o_.buf], [p["b_y"]], o_.buf)

    phase_A()
    phase_B1()
    phase_B2()
    phase_C1()
    if cfg.get("dense_moe", False):
        phase_C2()
    else:
        phase_C2_sparse()
    P.op("sp", lambda e: e.nop(), reads=[p["b_y"] for p in parts])
    P.emit_all(nc)
    st.close()
    return nc


def _slopes(HPG):
    nh = NGRP * HPG
    s = 2.0 ** (-8.0 * np.arange(1, nh + 1) / nh)
    return s.astype(np.float32).reshape(NGRP, HPG)


def _const_tables(cfg):
    HPG, FGW = cfg["HPG"], cfg["FGW"]
    sl = _slopes(HPG).astype(np.float64)
    a = np.arange(128)[:, None]
    b = np.arange(128)[None, :]
    etA = np.zeros((128, NGRP * HPG, 128), np.float32)
    etB = np.zeros((128, NGRP * HPG, 128), np.float32)
    for g in range(NGRP):
        for h in range(HPG):
            s = np.float64(np.float32(sl[g, h])) * DIL[g]
            ea = np.where(a >= b, np.exp(-s * np.abs(a - b - 64)), 0.0)
            eb = np.where(a <= b, np.exp(-s * np.abs(a - b + 64)), 0.0)
            etA[:, g * HPG + h, :] = ea
            etB[:, g * HPG + h, :] = eb
    etA0 = np.ascontiguousarray(etA[64:128])
    c = np.arange(FGW)
    ang = 2.0 * np.pi * np.outer(c, c) / FGW
    cc = (np.cos(ang) / np.sqrt(FGW)).astype(np.float32)
    scn = (-np.sin(ang) / np.sqrt(FGW)).astype(np.float32)
    return dict(etA=etA, etB=etB, etA0=etA0, cc=cc, scn=scn, ident=np.eye(128, dtype=np.float32))


def _dft_local(S, own, half):
    loc = np.arange(S, dtype=np.int64)
    glob = loc if half == 0 else S - 1 - loc
    prod = np.outer(glob, glob[:own]) % S
    ang = 2.0 * np.pi * prod / S
    return (np.cos(ang) / np.sqrt(S)).astype(np.float32), (np.sin(ang) / np.sqrt(S)).astype(np.float32)


def _relayout_up(w):
    ne, d, dff = w.shape
    kc, ffc = d // 128, dff // 128
    r = w.reshape(ne, kc, 128, ffc, 128).transpose(3, 0, 2, 1, 4)
    return np.ascontiguousarray(r).reshape(ffc, ne * 128, kc * 128)


def _relayout_down(w):
    ne, dff, d = w.shape
    ffc = dff // 128
    db = min(512, d)
    r = w.reshape(ne, ffc, 128, d // db, db).transpose(3, 0, 2, 1, 4)
    return np.ascontiguousarray(r).reshape(d // db, ne * 128, ffc * db)


def _run(cfg, x_prompt, x_sample, attn_norm_g, w_in, w_branch_attn, w_branch_fourier, b_gate, w_out, ffn_norm_g,
         w_router_group, b_router_group, w_router_expert, b_router_expert, w_expert_gate, w_expert_up, w_expert_down,
         final_norm_g, n_cores=8):
    D = cfg["D"]
    KC = D // 128
    f = lambda a: np.ascontiguousarray(np.asarray(a, dtype=np.float32))
    x_prompt, x_sample = f(x_prompt), f(x_sample)
    shared = dict(
        w_in=f(w_in)[0], w_ba=f(w_branch_attn)[0], w_bf=f(w_branch_fourier)[0], w_out=f(w_out)[0],
        w_r=np.ascontiguousarray(np.concatenate(
            [f(w_router_group)[0], f(w_router_expert)[0].transpose(1, 0, 2).reshape(D, NE)], axis=1)),
        w_eg=_relayout_up(f(w_expert_gate)[0]), w_eu=_relayout_up(f(w_expert_up)[0]),
        w_ed=_relayout_down(f(w_expert_down)[0]),
        pidx=np.arange(128, dtype=np.float32).reshape(128, 1),
        g1T=np.ascontiguousarray(f(attn_norm_g)[0].reshape(KC, 128).T),
        g2T=np.ascontiguousarray(f(ffn_norm_g)[0].reshape(KC, 128).T),
        bgT=np.ascontiguousarray(f(b_gate)[0].reshape(2 * KC, 128).T),
        gf_b=np.ascontiguousarray(np.broadcast_to(f(final_norm_g)[None, :], (128, D))),
        br_b=np.ascontiguousarray(np.broadcast_to(
            np.concatenate([f(b_router_group)[0], f(b_router_expert)[0].reshape(NE)])[None, :], (128, NR))),
    )
    shared.update(_const_tables(cfg))
    TB = cfg.get("TB", 256)
    ntok = (cfg["S_S"] + cfg["S_P"]) // 2
    jmax = 2 * ntok // TB + NE
    shared["g2_b"] = np.ascontiguousarray(np.broadcast_to(f(ffn_norm_g)[0][None, :], (128, D)))
    shared["jidx"] = np.ascontiguousarray(np.broadcast_to(np.arange(jmax, dtype=np.float32)[None, :], (128, jmax)))
    shared["tri"] = np.triu(np.ones((128, 128), np.float32))
    dft = {}
    for nm, S in (("s", cfg["S_S"]), ("p", cfg["S_P"])):
        for half in (0, 1):
            dft[(nm, half)] = _dft_local(S, S // 2, half)
    in_maps = []
    for c in range(n_cores):
        b, half = c // 2, c % 2
        m = dict(shared)
        xs = x_sample[b] if half == 0 else x_sample[b, ::-1]
        xp = x_prompt[b] if half == 0 else x_prompt[b, ::-1]
        m["x_s"] = np.ascontiguousarray(xs)
        m["x_p"] = np.ascontiguousarray(xp)
        m["cs_s"], m["ss_s"] = dft[("s", half)]
        m["cs_p"], m["ss_p"] = dft[("p", half)]
        in_maps.append(m)
    nc = build_nc(cfg)
    res = run_bass_kernel_spmd(nc, in_maps, core_ids=list(range(n_cores)))
    nb = n_cores // 2
    y_p = np.zeros((nb, cfg["S_P"], D), np.float32)
    y_s = np.zeros((nb, cfg["S_S"], D), np.float32)
    for c in range(n_cores):
        b, half = c // 2, c % 2
        r = res.results[c]
        for nm, S, dst in (("p", cfg["S_P"], y_p), ("s", cfg["S_S"], y_s)):
            o = np.asarray(r["y_" + nm], dtype=np.float32)
            if half == 0:
                dst[b, :S // 2] = o
            else:
                dst[b, S // 2:] = o[::-1]
    return y_p, y_s


def kernel(**inputs):
    return _run(FULL_CFG, **inputs)
```
